# Optimizing a Trainium2 kernel written in Bass

```python
import math
import jax, jax.numpy as jnp
from jax import lax
import numpy as np

D_MODEL = 2048
BATCH = 2
SEQ = 4096
DEPTH = 1

HG_HEADS = 8
HG_DK = 128
HG_DV = 128
HG_CHUNK = 64
ATT_HEADS = 8
ATT_KV_HEADS = 2
HEAD_DIM = 128
WINDOW = 128
ATT_BLOCK = 128
ROPE_THETA = 10000.0
N_EXPERTS = 16
EXPERT_FF = 1024
CAPACITY_FACTOR = 2
NORM_EPS = 1e-6

HG_QK = HG_HEADS * HG_DK
HG_V = HG_HEADS * HG_DV
ATT_Q = ATT_HEADS * HEAD_DIM
ATT_KV = ATT_KV_HEADS * HEAD_DIM
IN_SPLITS = (HG_QK, HG_QK, HG_QK, HG_V, HG_V, ATT_Q, ATT_KV, ATT_KV, D_MODEL, D_MODEL)
IN_WIDTH = 5 * 1024 + 1024 + 256 + 256 + 2 * D_MODEL

kernel_name = "hybrid_hgrn2_swa_ecmoe_block"


def rmsnorm(x, g):
    xf = x.astype(jnp.float32)
    y = xf * lax.rsqrt(jnp.mean(xf * xf, axis=-1, keepdims=True) + NORM_EPS)
    return y.astype(x.dtype) * g


def to_heads(t, n):
    b, s, _ = t.shape
    return t.reshape(b, s, n, -1).transpose(0, 2, 1, 3)


def hgrn2_scan(q, k, v, log_f):
    B, H, S, DK = q.shape
    DV = v.shape[-1]
    L = HG_CHUNK
    N = S // L
    rs = lambda t: t.astype(jnp.float32).reshape(B, H, N, L, t.shape[-1])
    q, k, v, log_f = rs(q), rs(k), rs(v), rs(log_f)
    b = jnp.cumsum(log_f, axis=3)
    b_ref = b[..., L // 2:L // 2 + 1, :]
    q_in = q * jnp.exp(b - b_ref)
    k_in = k * jnp.exp(b_ref - b)
    causal_in_chunk = jnp.tril(jnp.ones((L, L), dtype=bool))
    a = jnp.einsum('bhnld,bhnmd->bhnlm', q_in, k_in)
    a = jnp.where(causal_in_chunk, a, 0.0)
    o_intra = jnp.einsum('bhnlm,bhnmv->bhnlv', a, v)
    b_last = b[..., -1:, :]
    chunk_state = jnp.einsum('bhnld,bhnlv->bhndv', k * jnp.exp(b_last - b), v)
    chunk_decay = jnp.exp(b_last[..., 0, :])

    def step(s_prev, inp):
        dec, cs = inp
        return dec[..., None] * s_prev + cs, s_prev

    _, s_prevs = lax.scan(step, jnp.zeros((B, H, DK, DV), jnp.float32),
                          (jnp.moveaxis(chunk_decay, 2, 0), jnp.moveaxis(chunk_state, 2, 0)))
    s_prevs = jnp.moveaxis(s_prevs, 0, 2)
    o_inter = jnp.einsum('bhnld,bhndv->bhnlv', q * jnp.exp(b), s_prevs)
    return (o_intra + o_inter).reshape(B, H, S, DV)


def rope(x, pos):
    half = x.shape[-1] // 2
    inv = ROPE_THETA ** (-jnp.arange(half, dtype=jnp.float32) / half)
    ang = pos[:, None, :, None].astype(jnp.float32) * inv
    cos, sin = jnp.cos(ang), jnp.sin(ang)
    xf = x.astype(jnp.float32)
    x1, x2 = xf[..., :half], xf[..., half:]
    return jnp.concatenate([x1 * cos - x2 * sin, x1 * sin + x2 * cos], axis=-1).astype(x.dtype)


def window_attention(q, k, v, sink):
    B, HQ, S, Dh = q.shape
    HKV = k.shape[1]
    G = HQ // HKV
    Bk = ATT_BLOCK
    NB = S // Bk
    qb = q.reshape(B, HKV, G, NB, Bk, Dh).astype(jnp.float32)
    pad = ((0, 0), (0, 0), (Bk, Bk), (0, 0))
    kp = jnp.pad(k, pad).reshape(B, HKV, NB + 2, Bk, Dh)
    vp = jnp.pad(v, pad).reshape(B, HKV, NB + 2, Bk, Dh)
    kb = jnp.concatenate([kp[:, :, :-2], kp[:, :, 1:-1], kp[:, :, 2:]], axis=3)
    vb = jnp.concatenate([vp[:, :, :-2], vp[:, :, 1:-1], vp[:, :, 2:]], axis=3)
    s = jnp.einsum('bhgnqd,bhnkd->bhgnqk', qb, kb.astype(jnp.float32)) * (Dh ** -0.5)
    qi = jnp.arange(Bk)[:, None]
    kj = jnp.arange(3 * Bk)[None, :]
    band = jnp.abs(kj - Bk - qi) <= WINDOW
    key_pos = jnp.arange(NB)[:, None] * Bk - Bk + jnp.arange(3 * Bk)[None, :]
    in_range = (key_pos >= 0) & (key_pos < S)
    mask = band[None] & in_range[:, None, :]
    s = jnp.where(mask, s, -jnp.inf)
    sk = sink.astype(jnp.float32).reshape(1, HKV, G, 1, 1, 1)
    m = jnp.maximum(jnp.max(s, axis=-1, keepdims=True), sk)
    p = jnp.exp(s - m)
    denom = jnp.sum(p, axis=-1, keepdims=True) + jnp.exp(sk - m)
    o = jnp.einsum('bhgnqk,bhnkd->bhgnqd', p, vb.astype(jnp.float32)) / denom
    o = o.reshape(B, HQ, S, Dh).astype(q.dtype)
    return o.transpose(0, 2, 1, 3).reshape(B, S, HQ * Dh)


def hybrid_mixer(h, positions, w_in, lb, hg_out_norm, attn_sink, w_branch_a, w_branch_b, w_out):
    B, S, _ = h.shape
    proj = h @ w_in
    offsets = [int(o) for o in np.cumsum(IN_SPLITS)[:-1]]
    hq, hf_f, hf_b, hi, hg, aq, ak, av, gate_a, gate_b = jnp.split(proj, offsets, axis=-1)

    q = jax.nn.silu(to_heads(hq, HG_HEADS))
    v = to_heads(hi, HG_HEADS)

    def forget(z, lb_d):
        lb_h = lb_d.reshape(HG_HEADS, 1, HG_DK)
        return lb_h + (1.0 - lb_h) * jax.nn.sigmoid(to_heads(z, HG_HEADS).astype(jnp.float32))

    f_fwd = forget(hf_f, lb[0])
    f_bwd = forget(hf_b, lb[1])
    o_f = hgrn2_scan(q, 1.0 - f_fwd, v, jnp.log(f_fwd))
    flip = lambda t: jnp.flip(t, axis=2)
    o_b = flip(hgrn2_scan(flip(q), flip(1.0 - f_bwd), flip(v), flip(jnp.log(f_bwd))))
    o = (o_f + o_b).astype(h.dtype)
    o = rmsnorm(o, hg_out_norm[:, None, :]) * jax.nn.silu(to_heads(hg, HG_HEADS))
    y_a = o.transpose(0, 2, 1, 3).reshape(B, S, HG_V) @ w_branch_a

    qa = rope(to_heads(aq, ATT_HEADS), positions)
    ka = rope(to_heads(ak, ATT_KV_HEADS), positions)
    va = to_heads(av, ATT_KV_HEADS)
    y_b = window_attention(qa, ka, va, attn_sink) @ w_branch_b

    merged = jax.nn.sigmoid(gate_a) * y_a + jax.nn.sigmoid(gate_b) * y_b
    return merged @ w_out


def expert_choice_ffn(h, w_router, w_gate, w_up, w_down):
    B, T, D = h.shape
    C = CAPACITY_FACTOR * T // N_EXPERTS
    logits = jnp.einsum('btd,de->bte', h.astype(jnp.float32), w_router.astype(jnp.float32))
    aff = jax.nn.softmax(logits, axis=-1)
    top_aff, top_idx = lax.top_k(jnp.swapaxes(aff, 1, 2), C)
    xe = jax.vmap(lambda hb, ib: hb[ib])(h, top_idx)
    g = jnp.einsum('becd,edf->becf', xe, w_gate)
    u = jnp.einsum('becd,edf->becf', xe, w_up)
    y = jnp.einsum('becf,efd->becd', jax.nn.silu(g) * u, w_down)
    y = y * top_aff[..., None].astype(y.dtype)
    return jax.vmap(lambda ib, yb: jnp.zeros((T, D), yb.dtype).at[ib.reshape(-1)].add(yb.reshape(-1, D)))(top_idx, y)


def setup_inputs(seed: int = 0) -> dict:
    key = jax.random.key(seed)
    ks = jax.random.split(key, 24)
    f32 = jnp.float32
    nrm = lambda k, shape, scale: jax.random.normal(k, shape, f32) * scale
    gain = lambda k, shape: 1.0 + 0.02 * jax.random.normal(k, shape, f32)
    D = D_MODEL
    return {
        "x": nrm(ks[0], (BATCH, SEQ, D), 1.0),
        "c": nrm(ks[1], (BATCH, D), 1.0),
        "positions": jnp.broadcast_to(jnp.arange(SEQ, dtype=jnp.int32), (BATCH, SEQ)),
        "w_ada": nrm(ks[2], (DEPTH, D, 6 * D), 0.5 * D ** -0.5),
        "b_ada": nrm(ks[3], (DEPTH, 6 * D), 0.02),
        "g_pre_mix": gain(ks[4], (DEPTH, D)),
        "g_post_mix": gain(ks[5], (DEPTH, D)),
        "g_pre_ffn": gain(ks[6], (DEPTH, D)),
        "g_post_ffn": gain(ks[7], (DEPTH, D)),
        "w_in": nrm(ks[8], (DEPTH, D, IN_WIDTH), D ** -0.5),
        "hg_lb_logits": nrm(ks[9], (2, DEPTH + 1, HG_QK), 0.1),
        "hg_out_norm": gain(ks[10], (DEPTH, HG_HEADS, HG_DV)),
        "attn_sink": nrm(ks[11], (DEPTH, ATT_HEADS), 0.5),
        "w_branch_a": nrm(ks[12], (DEPTH, HG_V, D), HG_V ** -0.5),
        "w_branch_b": nrm(ks[13], (DEPTH, ATT_Q, D), ATT_Q ** -0.5),
        "w_out": nrm(ks[14], (DEPTH, D, D), D ** -0.5),
        "w_router": nrm(ks[15], (DEPTH, D, N_EXPERTS), D ** -0.5),
        "w_exp_gate": nrm(ks[16], (DEPTH, N_EXPERTS, D, EXPERT_FF), D ** -0.5),
        "w_exp_up": nrm(ks[17], (DEPTH, N_EXPERTS, D, EXPERT_FF), D ** -0.5),
        "w_exp_down": nrm(ks[18], (DEPTH, N_EXPERTS, EXPERT_FF, D), EXPERT_FF ** -0.5),
    }


def reference(x, c, positions, w_ada, b_ada, g_pre_mix, g_post_mix, g_pre_ffn, g_post_ffn,
              w_in, hg_lb_logits, hg_out_norm, attn_sink, w_branch_a, w_branch_b, w_out,
              w_router, w_exp_gate, w_exp_up, w_exp_down):
    lb_all = jnp.cumsum(jax.nn.softmax(hg_lb_logits.astype(jnp.float32), axis=1), axis=1)
    for layer in range(DEPTH):
        mod = jax.nn.silu(c) @ w_ada[layer] + b_ada[layer]
        sh1, sc1, gt1, sh2, sc2, gt2 = jnp.split(mod[:, None, :], 6, axis=-1)
        h = rmsnorm(x, g_pre_mix[layer]) * (1.0 + sc1) + sh1
        y = hybrid_mixer(h, positions, w_in[layer], lb_all[:, layer], hg_out_norm[layer],
                         attn_sink[layer], w_branch_a[layer], w_branch_b[layer], w_out[layer])
        x = x + gt1 * rmsnorm(y, g_post_mix[layer])
        h = rmsnorm(x, g_pre_ffn[layer]) * (1.0 + sc2) + sh2
        y = expert_choice_ffn(h, w_router[layer], w_exp_gate[layer], w_exp_up[layer], w_exp_down[layer])
        x = x + gt2 * rmsnorm(y, g_post_ffn[layer])
    return x
```

```python
import os
import math
import types
from contextlib import ExitStack
import numpy as np
import ml_dtypes
import concourse.bass as bass
import concourse.mybir as mybir
from concourse.bass_utils import run_bass_kernel_spmd

F32 = mybir.dt.float32
BF16 = mybir.dt.bfloat16
I32 = mybir.dt.int32
U8 = mybir.dt.uint8
AF = mybir.ActivationFunctionType
ALU = mybir.AluOpType
AX = mybir.AxisListType
DSZ = {F32: 4, BF16: 2, I32: 4, U8: 1}

DEBUG = bool(int(os.environ.get("KDEBUG", "0")))
KSTOP = int(os.environ.get("KSTOP", "99"))
KVAR = int(os.environ.get("KVAR", "0"))


class _Stop(Exception):
    pass
D = 2048
S = 4096
KC = 16
EPS = 1e-6
BIG = 1.0e6
G4 = [[0, 1, 2, 3], [4, 5, 6, 7]]
G8 = [list(range(8))]


class Tok:
    __slots__ = ("lw", "rd")

    def __init__(self):
        self.lw = None
        self.rd = []


class Op:
    __slots__ = ("eng", "fn", "deps", "dma", "ms", "msidx", "dsem", "dcount", "dprev", "seq")

    def __init__(self, eng, fn, dma):
        self.eng = eng
        self.fn = fn
        self.deps = []
        self.dma = dma
        self.ms = False
        self.msidx = None
        self.dsem = None
        self.dcount = None
        self.dprev = None


ENGS = ("pe", "act", "dve", "pool", "sp")


def _freeze(f):
    if getattr(f, "__closure__", None) is None:
        return f
    cells = []
    for c in f.__closure__:
        try:
            cells.append(types.CellType(c.cell_contents))
        except ValueError:
            cells.append(c)
    return types.FunctionType(f.__code__, f.__globals__, f.__name__, f.__defaults__, tuple(cells))


class Prog:
    def __init__(self, n_dma_sems=40):
        self.ops = {e: [] for e in ENGS}
        self.n_dma_sems = n_dma_sems
        self.all = []
        self.final_waits = []

    def op(self, eng, fn, reads=(), writes=(), dma=False):
        o = Op(eng, _freeze(fn), dma)
        deps = []
        for t in reads:
            if t.lw is not None:
                deps.append(t.lw)
        for t in writes:
            if t.lw is not None:
                deps.append(t.lw)
            deps.extend(t.rd)
        o.seq = len(self.all)
        seen = set()
        last = {}
        for d in deps:
            if id(d) in seen:
                continue
            seen.add(id(d))
            if d.dma:
                o.deps.append(d)
                continue
            if (not dma) and d.eng == "pe" and eng == "pe":
                continue
            if d.eng not in last or last[d.eng].seq < d.seq:
                last[d.eng] = d
        for d in last.values():
            o.deps.append(d)
            d.ms = True
        for t in reads:
            if not dma:
                t.rd = [x for x in t.rd if x.dma or x.eng != eng]
            t.rd.append(o)
        for t in writes:
            t.lw = o
            t.rd = []
        self.ops[eng].append(o)
        self.all.append(o)
        return o

    def get_dyn(self, eng, key):
        if getattr(self, "dyn", None) is None:
            pid = eng.partition_id()
            self.dyn = {"q1024": eng.snap((pid % 4) * 1024), "q4": eng.snap((pid % 4) * 4)}
        return self.dyn[key]

    def emit(self, nc, stack):
        for e in ENGS:
            k = 0
            for o in self.ops[e]:
                if (not o.dma) and o.ms:
                    k += 1
                    o.msidx = k
        esem = {e: stack.enter_context(nc.semaphore("s_" + e)) for e in ENGS}
        dsems = [stack.enter_context(nc.semaphore("d%d" % i)) for i in range(self.n_dma_sems)]
        dcum = [0] * self.n_dma_sems
        k = 0
        ksw = 0
        n_sw = 12
        n_hw = self.n_dma_sems - n_sw
        for o in self.all:
            if o.dma == "cc":
                dsems.append(stack.enter_context(nc.semaphore("cc%d" % len(dsems))))
                o.dsem = len(dsems) - 1
                o.dprev = 0
                o.dcount = 1
            elif o.dma:
                if o.eng == "pool":
                    i = n_hw + (ksw % n_sw)
                    ksw += 1
                else:
                    i = k % n_hw
                    k += 1
                o.dsem = i
                o.dprev = dcum[i]
                dcum[i] += 16
                o.dcount = dcum[i]
        finals = self.final_waits
        LIMIT = 2400
        segs = [[]]
        cnt = {e: 0 for e in ENGS}
        for o in self.all:
            c = 2 + len(o.deps)
            if cnt[o.eng] + c > LIMIT:
                segs.append([])
                cnt = {e: 0 for e in ENGS}
            cnt[o.eng] += c
            segs[-1].append(o)
        waited_all = {e: {} for e in ENGS}

        def run(ename, eng, seg, last):
            waited = waited_all[ename]

            def w(key, sem, val):
                if val <= 0 or waited.get(key, 0) >= val:
                    return
                waited[key] = val
                eng.wait_ge(sem, val)

            for o in seg:
                if o.eng != ename:
                    continue
                for d in o.deps:
                    if d.dma:
                        w(("d", d.dsem), dsems[d.dsem], d.dcount)
                    else:
                        w(("e", d.eng), esem[d.eng], d.msidx)
                if o.dma:
                    w(("d", o.dsem), dsems[o.dsem], o.dprev)
                    ins = o.fn(eng)
                    ins.then_inc(dsems[o.dsem], 1 if o.dma == "cc" else 16)
                else:
                    ins = o.fn(eng)
                    if o.ms:
                        ins.then_inc(esem[ename], 1)
            if last and ename == "sp":
                for d in finals:
                    if d.dma:
                        w(("d", d.dsem), dsems[d.dsem], d.dcount)
                    else:
                        w(("e", d.eng), esem[d.eng], d.msidx)

        for si, seg in enumerate(segs):
            last = si == len(segs) - 1
            with nc.Block() as block:
                @block.sync
                def _(eng):
                    run("sp", eng, seg, last)

                @block.tensor
                def _(eng):
                    run("pe", eng, seg, last)

                @block.scalar
                def _(eng):
                    run("act", eng, seg, last)

                @block.vector
                def _(eng):
                    run("dve", eng, seg, last)

                @block.gpsimd
                def _(eng):
                    run("pool", eng, seg, last)


class Arena:
    def __init__(self, ap_u8, nbytes):
        self.ap = ap_u8
        self.n = nbytes
        self.top = 0
        self.hist = []

    def mark(self):
        return self.top

    def release(self, m):
        self.top = m

    def alloc(self, shape, dt):
        n = DSZ[dt]
        for s in shape:
            n *= s
        start = (self.top + 31) // 32 * 32
        end = start + n
        assert end <= self.n, ("SBUF arena overflow", end, self.n)
        self.top = end
        tok = Tok()
        live = []
        for (s0, e0, t0) in self.hist:
            if s0 < end and start < e0:
                if t0.lw is not None:
                    tok.rd.append(t0.lw)
                tok.rd.extend(t0.rd)
            if t0.lw is not None or t0.rd:
                live.append((s0, e0, t0))
        self.hist = [h for h in self.hist if not (h[0] >= start and h[1] <= end)]
        self.hist.append((start, end, tok))
        v = self.ap[:, start:end].bitcast(dt)
        if len(shape) == 2:
            v = v.rearrange("p (a b) -> p a b", b=shape[1])
        elif len(shape) == 3:
            v = v.rearrange("p (a b c) -> p a b c", b=shape[1], c=shape[2])
        return v, tok


def build_program():
    nc = bass.Bass("TRN2", target_bir_lowering=False)
    P = Prog()

    def din(name, shape, dt):
        return nc.dram_tensor(name, shape, dt, kind="ExternalInput").ap()

    def dscr(name, shape, dt):
        return nc.dram_tensor(name, shape, dt, kind="Internal").ap()

    x_own = din("x_own", [1024, D], F32)
    c_b = din("c_b", [128, KC], F32)
    pos_b = din("pos_b", [1, S], I32)
    w_ada_q = din("w_ada_q", [D, 3072], F32)
    b_ada_q = din("b_ada_q", [1, 3072], F32)
    gvecs = din("gvecs", [4, D], F32)
    w_fm = din("w_fm", [D, 1152], F32)
    w_tm = din("w_tm", [D, 640], F32)
    w_gab = din("w_gab", [D, 4096], F32)
    lbl = din("lbl", [128, 4, 2], F32)
    hgn = din("hgn", [1, 256], F32)
    sink = din("sink", [1, 2], F32)
    w_ba = din("w_ba", [1024, D], F32)
    w_bb = din("w_bb", [1024, D], F32)
    w_o = din("w_o", [D, D], F32)
    w_r = din("w_r", [D, 16], F32)
    wg = din("wg", [4, D, 1024], F32)
    wu = din("wu", [4, D, 1024], F32)
    wd = din("wd", [4, 1024, D], F32)
    c_identb = din("c_identb", [128, 128], BF16)
    c_identf = din("c_identf", [128, 128], F32)
    c_maskF = din("c_maskF", [128, 128], F32)
    c_maskB = din("c_maskB", [128, 128], F32)
    c_pswap = din("c_pswap", [128, 128], BF16)
    c_small = din("c_small", [128, 4], F32)
    c_band = din("c_band", [128, 3, 384], F32)
    c_rmask = din("c_rmask", [128, 512], F32)
    c_iota = din("c_iota", [128, 512], F32)
    c_tval = din("c_tval", [128, 32, 2], BF16)
    c_rowbase = din("c_rowbase", [128, 16], F32)
    c_idx = din("c_idx", [128, 40], I32)
    out = nc.dram_tensor("out", [1024, D], F32, kind="ExternalOutput").ap()

    src_mod = dscr("src_mod", [1, 3072], F32)
    dst_mod = dscr("dst_mod", [4, 3072], F32)
    src_hT = dscr("src_hT", [4 * KC * 128, 256], BF16)
    dst_hT = dscr("dst_hT", [4 * KC * 128, 1024], BF16)
    hgT = dscr("hgT", [2, 2, 3, 128, S], BF16)
    koutM = dscr("koutM", [2, 2, S, 128], BF16)
    vg = dscr("vg", [S, 512], BF16)
    src_o = dscr("src_o", [S, 512], BF16)
    dst_o = dscr("dst_o", [4 * S, 512], BF16)
    x1_d = dscr("x1_d", [1024, D], F32)
    src_h2 = dscr("src_h2", [1024, D], BF16)
    dst_h2 = dscr("dst_h2", [4096, D], BF16)
    src_aff = dscr("src_aff", [16, 1024], F32)
    dst_aff = dscr("dst_aff", [64, 1024], F32)
    route_d = dscr("route_d", [16, S], F32)
    route_d2 = dscr("route_d2", [64, 1024], F32)
    src_Y = dscr("src_Y", [2048, D], BF16)
    dst_Y = dscr("dst_Y", [8192, D], BF16)
    t_o_ch = [Tok() for _ in range(4)]
    t_h2_ch = [Tok() for _ in range(4)]
    t_Y_ch = [Tok() for _ in range(8)]
    t_dram = {n: Tok() for n in ("src_mod", "dst_mod", "src_hT", "dst_hT", "hgT", "koutM", "vg", "src_o", "dst_o",
                                 "x1_d", "src_h2", "dst_h2", "src_aff", "dst_aff", "route_d", "src_Y", "dst_Y")}

    with ExitStack() as st:
        ARENA_BYTES = 204 * 1024
        arena_t = st.enter_context(nc.sbuf_tensor("arena", [128, ARENA_BYTES], U8))
        A = Arena(arena_t, ARENA_BYTES)
        psb = [st.enter_context(nc.psum_tensor("ps%d" % i, [128, 512], F32)) for i in range(8)]
        pst = [Tok() for _ in range(8)]

        def psf(i):
            return psb[i][:, :]

        def psbf(i):
            return psb[i][:, :].bitcast(BF16)

        OP = P.op

        def stop_here(n):
            if KSTOP == n:
                raise _Stop()

        def dma(eng, out_ap, in_ap, r=(), w=()):
            return OP(eng, lambda e: e.dma_start(out=out_ap, in_=in_ap), reads=r, writes=w, dma=True)

        agc = [0]

        def ag_start(src, rows, ci, tsrc, dst=None):
            ttmp = Tok()
            if dst is not None:
                allgather(src[ci * rows:(ci + 1) * rows, :], dst[ci * 4 * rows:(ci + 1) * 4 * rows, :], G4, tsrc, ttmp)
                return None, ttmp
            agc[0] += 1
            tmp = dscr("agtmp%d" % agc[0], [4 * rows, src.shape[1]], src.dtype)
            allgather(src[ci * rows:(ci + 1) * rows, :], tmp, G4, tsrc, ttmp)
            return tmp, ttmp

        def ag_finish(dst, nch, pend, tdst, only=None):
            dv = dst.rearrange("(r c m) x -> c r m x", r=4, c=nch)
            for ci, (tmp, ttmp) in enumerate(pend):
                if only is not None and ci != only:
                    continue
                dma("sp", dv[ci], tmp.rearrange("(r m) x -> r m x", r=4), r=[ttmp], w=[tdst])

        def ag_chunked(src, dst, rows, nch, tsrc, tdst):
            C = src.shape[1]
            dv = dst.rearrange("(r c m) x -> c r m x", r=4, c=nch)
            for ci in range(nch):
                agc[0] += 1
                tmp = dscr("agtmp%d" % agc[0], [4 * rows, C], src.dtype)
                ttmp = Tok()
                allgather(src[ci * rows:(ci + 1) * rows, :], tmp, G4, tsrc[ci] if isinstance(tsrc, list) else tsrc, ttmp)
                dma("sp", dv[ci], tmp.rearrange("(r m) x -> r m x", r=4), r=[ttmp], w=[tdst])

        def allgather(src, dst, groups, tsrc, tdst):
            return OP("pool", lambda e: e.collective_compute("AllGather", ALU.bypass, replica_groups=groups,
                                                             ins=[src.opt()], outs=[dst.opt()]),
                      reads=[tsrc], writes=[tdst], dma="cc")

        identb, t_identb = A.alloc([128], BF16)
        identf, t_identf = A.alloc([128], F32)
        maskF, t_maskF = A.alloc([128], F32)
        maskB, t_maskB = A.alloc([128], F32)
        pswap, t_pswap = A.alloc([128], BF16)
        small, t_small = A.alloc([4], F32)
        band, t_band = A.alloc([3, 384], F32)
        rmask, t_rmask = A.alloc([512], F32)
        iota, t_iota = A.alloc([512], F32)
        tval, t_tval = A.alloc([32, 2], BF16)
        rowbase, t_rowbase = A.alloc([16], F32)
        cidx, t_cidx = A.alloc([40], I32)
        lbt, t_lbt = A.alloc([4, 2], F32)
        lbv, t_lbv = A.alloc([4, 3], F32)
        hgnb, t_hgnb = A.alloc([256], F32)
        sinkb, t_sinkb = A.alloc([2], F32)
        for (sb_, src_, tk) in ((identb, c_identb, t_identb), (identf, c_identf, t_identf), (maskF, c_maskF, t_maskF),
                                (maskB, c_maskB, t_maskB), (pswap, c_pswap, t_pswap), (small, c_small, t_small),
                                (band, c_band, t_band), (rmask, c_rmask, t_rmask), (iota, c_iota, t_iota),
                                (tval, c_tval, t_tval), (rowbase, c_rowbase, t_rowbase), (lbt, lbl, t_lbt), (cidx, c_idx, t_cidx)):
            dma("sp", sb_, src_, w=[tk])
        dma("sp", hgnb, hgn.partition_broadcast(128), w=[t_hgnb])
        dma("sp", sinkb, sink.partition_broadcast(128), w=[t_sinkb])
        OP("dve", lambda e: e.tensor_tensor(out=lbv[:, :, 0], in0=lbt[:, :, 0], in1=lbt[:, :, 1], op=ALU.subtract),
           reads=[t_lbt], writes=[t_lbv])
        OP("act", lambda e: e.activation(out=lbv[:, :, 0], in_=lbv[:, :, 0], func=AF.Sigmoid), reads=[t_lbv], writes=[t_lbv])
        OP("dve", lambda e: e.tensor_scalar(out=lbv[:, :, 1], in0=lbv[:, :, 0], scalar1=-1.0, scalar2=1.0, op0=ALU.mult, op1=ALU.add),
           reads=[t_lbv], writes=[t_lbv])
        OP("dve", lambda e: e.tensor_scalar(out=lbv[:, :, 2], in0=lbv[:, :, 1], scalar1=-1.0, scalar2=None, op0=ALU.mult),
           reads=[t_lbv], writes=[t_lbv])
        invf = small[:, 0:1]
        sinsign = small[:, 1:2]
        negpis = small[:, 2:3]
        negpi = small[:, 3:4]

        def rms_rstd(eng_unused, ss, tss, rstd, trstd, n):
            OP("dve", lambda e: e.tensor_scalar(out=rstd, in0=ss, scalar1=1.0 / n, scalar2=EPS, op0=ALU.mult, op1=ALU.add),
               reads=[tss], writes=[trstd])
            OP("act", lambda e: e.activation(out=rstd, in_=rstd, func=AF.Sqrt), reads=[trstd], writes=[trstd])
            OP("dve", lambda e: e.reciprocal(out=rstd, in_=rstd), reads=[trstd], writes=[trstd])

        modflat = dst_mod.rearrange("r n -> (r n)").rearrange("(s d) -> s d", d=D)

        try:
            m1 = A.mark()
            cs, t_cs = A.alloc([KC], F32)
            csb, t_csb = A.alloc([KC], BF16)
            wada, t_wada = A.alloc([KC, 3072], BF16)
            bada, t_bada = A.alloc([3072], F32)
            modrow, t_modrow = A.alloc([3072], F32)
            dma("sp", cs, c_b, w=[t_cs])
            dma("sp", bada[0:1, :], b_ada_q, w=[t_bada])
            wsrc = w_ada_q.rearrange("(k p) n -> p k n", p=128)
            for g in range(4):
                dma("pool", wada[:, 4 * g:4 * g + 4, :], wsrc[:, 4 * g:4 * g + 4, :], w=[t_wada])
            OP("act", lambda e: e.activation(out=csb, in_=cs, func=AF.Silu), reads=[t_cs], writes=[t_csb])
            for g in range(6):
                bk = g % 4
                for kc in range(KC):
                    OP("pe", lambda e, g=g, kc=kc, bk=bk: e.matmul(psf(bk)[0:1, :], lhsT=csb[:, kc:kc + 1],
                                                                   rhs=wada[:, kc, g * 512:(g + 1) * 512],
                                                                   start=(kc == 0), stop=(kc == KC - 1)),
                       reads=[t_csb, t_wada], writes=[pst[bk]])
                OP("dve", lambda e, g=g, bk=bk: e.tensor_tensor(out=modrow[0:1, g * 512:(g + 1) * 512], in0=psf(bk)[0:1, :],
                                                                in1=bada[0:1, g * 512:(g + 1) * 512], op=ALU.add),
                   reads=[pst[bk], t_bada], writes=[t_modrow])
            dma("sp", src_mod, modrow[0:1, :], r=[t_modrow], w=[t_dram["src_mod"]])
            allgather(src_mod, dst_mod, G4, t_dram["src_mod"], t_dram["dst_mod"])
            A.release(m1)

            def load_modvec(dst_ap, tok, sidx, gidx, mode):
                if mode == "b":
                    dma("sp", dst_ap, modflat[sidx:sidx + 1, :].partition_broadcast(128), r=[t_dram["dst_mod"]], w=[tok])
                    return
                mk = A.mark()
                tmp, t_tmp = A.alloc([D], F32)
                dma("sp", dst_ap, modflat[sidx:sidx + 1, :].partition_broadcast(128), r=[t_dram["dst_mod"]], w=[tok])
                dma("sp", tmp, gvecs[gidx:gidx + 1, :].partition_broadcast(128), w=[t_tmp])
                if mode == "a":
                    OP("dve", lambda e: e.scalar_tensor_tensor(out=dst_ap, in0=dst_ap, scalar=1.0, in1=tmp, op0=ALU.add, op1=ALU.mult),
                       reads=[tok, t_tmp], writes=[tok])
                else:
                    OP("dve", lambda e: e.tensor_tensor(out=dst_ap, in0=dst_ap, in1=tmp, op=ALU.mult), reads=[tok, t_tmp], writes=[tok])
                A.release(mk)

            stop_here(1)
            m2 = A.mark()
            hTown, t_hTown = A.alloc([KC, 1024], BF16)
            A1, t_A1 = A.alloc([D], F32)
            B1, t_B1 = A.alloc([D], F32)
            load_modvec(A1, t_A1, 1, 0, "a")
            load_modvec(B1, t_B1, 0, 0, "b")
            xts = [A.alloc([D], F32) for _ in range(2)]
            junk, t_junk = A.alloc([D], BF16)
            hfs = [A.alloc([D], F32) for _ in range(2)]
            hbs = [A.alloc([D], BF16) for _ in range(2)]
            ss8, t_ss8 = A.alloc([8], F32)
            rs8, t_rs8 = A.alloc([8], F32)
            hT_tmp = []
            for j in range(8):
                xt, t_xt = xts[j % 2]
                hb, t_hb = hbs[j % 2]
                hf, t_hf = hfs[j % 2]
                dma("sp", xt, x_own[j * 128:(j + 1) * 128, :], w=[t_xt])
                OP("act", lambda e, xt=xt, j=j: e.activation(out=junk, in_=xt, func=AF.Square, accum_out=ss8[:, j:j + 1]),
                   reads=[t_xt], writes=[t_junk, t_ss8])
                rms_rstd(None, ss8[:, j:j + 1], t_ss8, rs8[:, j:j + 1], t_rs8, D)
                OP("dve", lambda e, xt=xt, j=j: e.scalar_tensor_tensor(out=hf, in0=xt, scalar=rs8[:, j:j + 1], in1=A1,
                                                                       op0=ALU.mult, op1=ALU.mult),
                   reads=[t_xt, t_rs8, t_A1], writes=[t_hf])
                OP("pool", lambda e, hb=hb: e.tensor_tensor(out=hb, in0=hf, in1=B1, op=ALU.add), reads=[t_hf, t_B1], writes=[t_hb])
                for half in range(2):
                    bk = 6 + half
                    for k8 in range(8):
                        kc = half * 8 + k8
                        OP("pe", lambda e, hb=hb, kc=kc, k8=k8, bk=bk: e.transpose(out=psbf(bk)[:, k8 * 128:(k8 + 1) * 128],
                                                                                   in_=hb[:, kc * 128:(kc + 1) * 128], identity=identb),
                           reads=[t_hb, t_identb], writes=[pst[bk]])
                    OP("act", lambda e, half=half, bk=bk, j=j: e.copy(out=hTown[:, half * 8:half * 8 + 8, j * 128:(j + 1) * 128],
                                                                      in_=psbf(bk).rearrange("p (a b) -> p a b", b=128)),
                       reads=[pst[bk]], writes=[t_hTown])
                if j % 2 == 1:
                    ci = j // 2
                    tch = Tok()
                    dma("sp", src_hT[ci * 2048:(ci + 1) * 2048, :].rearrange("(k p) t -> p k t", p=128), hTown[:, :, ci * 256:(ci + 1) * 256],
                        r=[t_hTown], w=[tch, t_dram["src_hT"]])
                    hT_tmp.append(ag_start(src_hT, 2048, ci, tch))

            A.release(m2)

            stop_here(2)
            m3 = A.mark()
            qTa = [A.alloc([S], BF16) for _ in range(2)]
            kTa, t_kTa = A.alloc([S + 256], BF16)
            vat, t_vat = A.alloc([34, 128], BF16)
            OP("pool", lambda e: e.memset(kTa[:, 0:128], 0.0), writes=[t_kTa])
            OP("pool", lambda e: e.memset(kTa[:, S + 128:S + 256], 0.0), writes=[t_kTa])
            OP("pool", lambda e: e.memset(vat[:, 0, :], 0.0), writes=[t_vat])
            OP("pool", lambda e: e.memset(vat[:, 33, :], 0.0), writes=[t_vat])
            dec = [[A.alloc([64], F32) for _ in range(2)] for _ in range(2)]
            m3b = A.mark()
            Wfm, t_Wfm = A.alloc([KC, 1152], BF16)
            Wtm, t_Wtm = A.alloc([KC, 640], BF16)
            wfs = w_fm.rearrange("(k p) n -> p k n", p=128)
            wts = w_tm.rearrange("(k p) n -> p k n", p=128)
            for g in range(2):
                dma("pool", Wfm[:, 8 * g:8 * g + 8, :], wfs[:, 8 * g:8 * g + 8, :], w=[t_Wfm])
            dma("pool", Wtm, wts, w=[t_Wtm])
            hblks = [A.alloc([KC, 512], BF16) for _ in range(2)]
            qf = [A.alloc([512], F32) for _ in range(2)]
            scr0 = [A.alloc([512], F32) for _ in range(11)]
            scr1a = [A.alloc([512], F32) for _ in range(4)]
            scr1b = [A.alloc([512], F32) for _ in range(2)]
            scr = [scr0, scr1a + scr0[4:7] + scr1b + scr0[9:]]
            prods = [A.alloc([3, 512], BF16) for _ in range(2)]
            koutf = [A.alloc([512], BF16) for _ in range(2)]
            koutm = [A.alloc([4, 128], BF16) for _ in range(2)]
            tmo = [A.alloc([512], BF16) for _ in range(2)]
            posi, t_posi = A.alloc([512], I32)
            posf, t_posf = A.alloc([512], F32)
            ang, t_ang = A.alloc([512], F32)
            cosT, t_cosT = A.alloc([512], F32)
            sinT, t_sinT = A.alloc([512], F32)
            qbs = [A.alloc([512], BF16) for _ in range(3)]
            rt1, t_rt1 = A.alloc([512], F32)
            rt2, t_rt2 = A.alloc([512], F32)
            hsrcs = [tmp.rearrange("(r k p) t -> r p k t", r=4, k=KC) for (tmp, _) in hT_tmp]
            v3 = lambda ap: ap.rearrange("p (c l) -> p c l", l=64)
            pcount = [0]
            order3 = [0, 2, 4, 6, 1, 3, 5, 7]

            def load_hblk(it3):
                tb_ = order3[it3]
                r_, jb_ = tb_ // 2, tb_ % 2
                hb_, t_hb_ = hblks[it3 % 2]
                for h_ in range(2):
                    dma("sp", hb_[:, :, h_ * 256:(h_ + 1) * 256], hsrcs[2 * jb_ + h_][r_], r=[hT_tmp[2 * jb_ + h_][1]], w=[t_hb_])
            load_hblk(0)
            for it3, tb in enumerate(order3):
                r, jb = tb // 2, tb % 2
                hblk, t_hblk = hblks[it3 % 2]
                if it3 + 1 < 8:
                    load_hblk(it3 + 1)
                tsl = slice(tb * 512, (tb + 1) * 512)
                dma("sp", posi, pos_b[0:1, tsl].partition_broadcast(128), w=[t_posi])
                OP("dve", lambda e: e.tensor_copy(out=posf, in_=posi), reads=[t_posi], writes=[t_posf])
                OP("dve", lambda e: e.tensor_scalar(out=ang, in0=posf, scalar1=invf, scalar2=None, op0=ALU.mult),
                   reads=[t_posf, t_small], writes=[t_ang])
                C1 = 6.28125
                C2 = 2 * math.pi - 6.28125
                OP("dve", lambda e: e.tensor_scalar(out=rt1, in0=ang, scalar1=1.0 / (2 * math.pi), scalar2=None, op0=ALU.mult), reads=[t_ang], writes=[t_rt1])
                OP("dve", lambda e: e.tensor_copy(out=posi, in_=rt1), reads=[t_rt1], writes=[t_posi])
                OP("dve", lambda e: e.tensor_copy(out=rt1, in_=posi), reads=[t_posi], writes=[t_rt1])
                OP("dve", lambda e: e.scalar_tensor_tensor(out=ang, in0=rt1, scalar=-C1, in1=ang, op0=ALU.mult, op1=ALU.add), reads=[t_rt1, t_ang], writes=[t_ang])
                OP("dve", lambda e: e.scalar_tensor_tensor(out=ang, in0=rt1, scalar=-C2, in1=ang, op0=ALU.mult, op1=ALU.add), reads=[t_rt1, t_ang], writes=[t_ang])

                def wrap(buf, tbuf):
                    OP("dve", lambda e: e.tensor_scalar(out=rt2, in0=buf, scalar1=math.pi, scalar2=-2 * math.pi, op0=ALU.is_gt, op1=ALU.mult), reads=[tbuf], writes=[t_rt2])
                    OP("dve", lambda e: e.tensor_tensor(out=buf, in0=buf, in1=rt2, op=ALU.add), reads=[tbuf, t_rt2], writes=[tbuf])
                    OP("dve", lambda e: e.tensor_scalar(out=rt2, in0=buf, scalar1=-math.pi, scalar2=2 * math.pi, op0=ALU.is_lt, op1=ALU.mult), reads=[tbuf], writes=[t_rt2])
                    OP("dve", lambda e: e.tensor_tensor(out=buf, in0=buf, in1=rt2, op=ALU.add), reads=[tbuf, t_rt2], writes=[tbuf])
                wrap(ang, t_ang)
                OP("act", lambda e: e.activation(out=sinT, in_=ang, func=AF.Sin, scale=sinsign), reads=[t_ang, t_small], writes=[t_sinT])
                OP("dve", lambda e: e.tensor_scalar(out=rt1, in0=ang, scalar1=0.5 * math.pi, scalar2=None, op0=ALU.add), reads=[t_ang], writes=[t_rt1])
                wrap(rt1, t_rt1)
                OP("act", lambda e: e.activation(out=cosT, in_=rt1, func=AF.Sin), reads=[t_rt1], writes=[t_cosT])

                def fm_matmul(cb, bk):
                    for kc in range(KC):
                        OP("pe", lambda e, kc=kc: e.matmul(psf(bk), lhsT=Wfm[:, kc, cb * 128:(cb + 1) * 128], rhs=hblk[:, kc, :],
                                                           start=(kc == 0), stop=(kc == KC - 1)),
                           reads=[t_Wfm, t_hblk], writes=[pst[bk]])

                for hh in range(2):
                    bk = pcount[0] % 4
                    pcount[0] += 1
                    fm_matmul(hh, bk)
                    OP("act", lambda e, hh=hh, bk=bk: e.activation(out=qf[hh][0], in_=psf(bk), func=AF.Silu),
                       reads=[pst[bk]], writes=[qf[hh][1]])
                bkmap = {}

                def stageA(dr, hh):
                    cb = 2 + dr * 2 + hh
                    li = dr * 2 + hh
                    bk = pcount[0] % 4
                    pcount[0] += 1
                    bkmap[(dr, hh)] = bk
                    fm_matmul(cb, bk)
                    sig, t_sig = scr[li % 2][0]
                    OP("act", lambda e, bk=bk: e.activation(out=sig, in_=psf(bk), func=AF.Sigmoid), reads=[pst[bk]], writes=[t_sig])

                def stageB(dr, hh):
                    li = dr * 2 + hh
                    bk = bkmap[(dr, hh)]
                    ((sig, t_sig), (logf, t_logf), (kk, t_kk), (bb, t_bb), (bx, t_bx), (dd, t_dd), (d2, t_d2),
                     (E1, t_E1), (E2, t_E2), (E3, t_E3), (E4, t_E4)) = scr[li % 2]
                    q_ap, t_q = qf[hh]
                    prod, t_prod = prods[(dr * 2 + hh) % 2]
                    kof, t_kof = koutf[(dr * 2 + hh) % 2]
                    kom, t_kom = koutm[(dr * 2 + hh) % 2]
                    dec_ap, t_dec = dec[hh][dr]
                    OP("act", lambda e, li=li: e.activation(out=logf, in_=sig, func=AF.Ln, bias=lbv[:, li, 0:1], scale=lbv[:, li, 1:2]),
                       reads=[t_sig, t_lbv], writes=[t_logf])
                    OP("dve", lambda e, li=li: e.tensor_scalar(out=kk, in0=sig, scalar1=lbv[:, li, 2:3], scalar2=lbv[:, li, 1:2],
                                                               op0=ALU.mult, op1=ALU.add),
                       reads=[t_sig, t_lbv], writes=[t_kk])
                    OP("dve", lambda e: e.tensor_tensor_scan(out=bb, data0=rmask, data1=logf, initial=0.0, op0=ALU.mult, op1=ALU.add),
                       reads=[t_rmask, t_logf], writes=[t_bb])
                    OP("act", lambda e, dec_ap=dec_ap, tb=tb: e.activation(out=dec_ap[:, tb * 8:(tb + 1) * 8], in_=v3(bb)[:, :, 63], func=AF.Exp),
                       reads=[t_bb], writes=[t_dec])
                    if dr == 0:
                        OP("dve", lambda e: e.tensor_tensor(out=v3(dd), in0=v3(bb), in1=v3(bb)[:, :, 32:33].to_broadcast([128, 8, 64]), op=ALU.subtract),
                           reads=[t_bb], writes=[t_dd])
                        OP("dve", lambda e: e.tensor_tensor(out=v3(d2), in0=v3(bb), in1=v3(bb)[:, :, 63:64].to_broadcast([128, 8, 64]), op=ALU.subtract),
                           reads=[t_bb], writes=[t_d2])
                        OP("act", lambda e: e.activation(out=E1, in_=dd, func=AF.Exp), reads=[t_dd], writes=[t_E1])
                        OP("act", lambda e: e.activation(out=E2, in_=dd, func=AF.Exp, scale=-1.0), reads=[t_dd], writes=[t_E2])
                        OP("act", lambda e: e.activation(out=E3, in_=bb, func=AF.Exp), reads=[t_bb], writes=[t_E3])
                        OP("act", lambda e: e.activation(out=E4, in_=d2, func=AF.Exp, scale=-1.0), reads=[t_d2], writes=[t_E4])
                    else:
                        OP("dve", lambda e: e.tensor_tensor(out=bx, in0=bb, in1=logf, op=ALU.subtract), reads=[t_bb, t_logf], writes=[t_bx])
                        OP("dve", lambda e: e.tensor_tensor(out=v3(dd), in0=v3(bx), in1=v3(bx)[:, :, 32:33].to_broadcast([128, 8, 64]), op=ALU.subtract),
                           reads=[t_bx], writes=[t_dd])
                        OP("dve", lambda e: e.tensor_tensor(out=v3(d2), in0=v3(bx), in1=v3(bb)[:, :, 63:64].to_broadcast([128, 8, 64]), op=ALU.subtract),
                           reads=[t_bx, t_bb], writes=[t_d2])
                        OP("act", lambda e: e.activation(out=E1, in_=dd, func=AF.Exp, scale=-1.0), reads=[t_dd], writes=[t_E1])
                        OP("act", lambda e: e.activation(out=E2, in_=dd, func=AF.Exp), reads=[t_dd], writes=[t_E2])
                        OP("act", lambda e: e.activation(out=E3, in_=d2, func=AF.Exp, scale=-1.0), reads=[t_d2], writes=[t_E3])
                        OP("act", lambda e: e.activation(out=E4, in_=bx, func=AF.Exp), reads=[t_bx], writes=[t_E4])
                    OP("pool", lambda e, prod=prod, q_ap=q_ap: e.tensor_tensor(out=prod[:, 0, :], in0=q_ap, in1=E1, op=ALU.mult),
                       reads=[t_q, t_E1], writes=[t_prod])
                    OP("pool", lambda e, prod=prod: e.tensor_tensor(out=prod[:, 1, :], in0=kk, in1=E2, op=ALU.mult),
                       reads=[t_kk, t_E2], writes=[t_prod])
                    OP("dve", lambda e, prod=prod, q_ap=q_ap: e.tensor_tensor(out=prod[:, 2, :], in0=q_ap, in1=E3, op=ALU.mult),
                       reads=[t_q, t_E3], writes=[t_prod])
                    OP("pool", lambda e, kof=kof: e.tensor_tensor(out=kof, in0=kk, in1=E4, op=ALU.mult),
                       reads=[t_kk, t_E4], writes=[t_kof])
                    dma("sp", hgT[hh, dr].rearrange("a p t -> p a t")[:, :, tsl], prod, r=[t_prod], w=[t_dram["hgT"]])

                    def stageC(kof=kof, t_kof=t_kof, kom=kom, t_kom=t_kom, hh=hh, dr=dr):
                        for i in range(4):
                            OP("pe", lambda e, i=i, kof=kof: e.transpose(out=psbf(6)[:, i * 128:(i + 1) * 128], in_=kof[:, i * 128:(i + 1) * 128], identity=identb),
                               reads=[t_kof, t_identb], writes=[pst[6]])
                        OP("act", lambda e, kom=kom: e.copy(out=kom, in_=psbf(6)[:, 0:512].rearrange("p (a b) -> p a b", b=128)),
                           reads=[pst[6]], writes=[t_kom])
                        dma("sp", koutM[hh, dr, tsl, :].rearrange("(i p) d -> p i d", p=128), kom, r=[t_kom], w=[t_dram["koutM"]])
                    return stageC

                items3 = [(0, 0), (0, 1), (1, 0), (1, 1)]
                stageA(*items3[0])
                pendC = []
                for n3, it_ in enumerate(items3):
                    if n3 + 1 < 4:
                        stageA(*items3[n3 + 1])
                    if pendC:
                        pendC.pop(0)()
                    pendC.append(stageB(*it_))
                for ci in range(3):
                    cb = 6 + ci
                    bk = pcount[0] % 4
                    pcount[0] += 1
                    fm_matmul(cb, bk)
                    qb, t_qb = qbs[ci]
                    OP("act", lambda e, bk=bk, qb=qb: e.copy(out=qb, in_=psf(bk)), reads=[pst[bk]], writes=[t_qb])
                while pendC:
                    pendC.pop(0)()

                def ropeB(ci):
                    qb, t_qb = qbs[ci]
                    OP("pe", lambda e: e.matmul(psf(7), lhsT=pswap, rhs=qb, start=True, stop=True), reads=[t_pswap, t_qb], writes=[pst[7]])
                    OP("pool", lambda e: e.tensor_tensor(out=rt1, in0=qb, in1=cosT, op=ALU.mult), reads=[t_qb, t_cosT], writes=[t_rt1])
                    OP("dve", lambda e: e.tensor_tensor(out=rt2, in0=psf(7), in1=sinT, op=ALU.mult), reads=[pst[7], t_sinT], writes=[t_rt2])
                    if ci < 2:
                        dst_ap, t_dst = qTa[ci][0][:, tsl], qTa[ci][1]
                    else:
                        dst_ap, t_dst = kTa[:, 128 + tb * 512:128 + (tb + 1) * 512], t_kTa
                    OP("pool", lambda e, dst_ap=dst_ap: e.tensor_tensor(out=dst_ap, in0=rt1, in1=rt2, op=ALU.add),
                       reads=[t_rt1, t_rt2], writes=[t_dst])
                for i in range(4):
                    tmo_ap, t_tmo = tmo[i % 2]
                    gt = tb * 4 + i
                    for kc in range(KC):
                        OP("pe", lambda e, kc=kc, i=i: e.matmul(psf(4), lhsT=hblk[:, kc, i * 128:(i + 1) * 128], rhs=Wtm[:, kc, 0:512],
                                                                start=(kc == 0), stop=(kc == KC - 1)),
                           reads=[t_hblk, t_Wtm], writes=[pst[4]])
                    for kc in range(KC):
                        OP("pe", lambda e, kc=kc, i=i: e.matmul(psf(5)[:, 0:128], lhsT=hblk[:, kc, i * 128:(i + 1) * 128], rhs=Wtm[:, kc, 512:640],
                                                                start=(kc == 0), stop=(kc == KC - 1)),
                           reads=[t_hblk, t_Wtm], writes=[pst[5]])
                    OP("act", lambda e, tmo_ap=tmo_ap: e.copy(out=tmo_ap[:, 0:256], in_=psf(4)[:, 0:256]), reads=[pst[4]], writes=[t_tmo])
                    OP("act", lambda e, tmo_ap=tmo_ap: e.activation(out=tmo_ap[:, 256:512], in_=psf(4)[:, 256:512], func=AF.Silu),
                       reads=[pst[4]], writes=[t_tmo])
                    OP("dve", lambda e, gt=gt: e.tensor_copy(out=vat[:, gt + 1, :], in_=psf(5)[:, 0:128]), reads=[pst[5]], writes=[t_vat])
                    dma("sp", vg[gt * 128:(gt + 1) * 128, :], tmo_ap, r=[t_tmo], w=[t_dram["vg"]])
                    if i < 3:
                        ropeB(i)
            A.release(m3b)

            stop_here(3)
            m4 = A.mark()
            Ssb = [A.alloc([386], F32) for _ in range(4)]
            Pb = [A.alloc([386], BF16) for _ in range(4)]
            for hh_ in range(4):
                OP("pool", lambda e, hh_=hh_: e.tensor_copy(out=Ssb[hh_][0][:, 384:385], in_=sinkb[:, hh_ % 2:hh_ % 2 + 1]), reads=[t_sinkb], writes=[Ssb[hh_][1]])
            PTs = [A.alloc([384], BF16) for _ in range(2)]
            st4 = [A.alloc([8], F32) for _ in range(4)]
            obt = [A.alloc([256], BF16) for _ in range(2)]
            scale = 128 ** -0.5
            it4 = 0
            for i in range(32):
                ob_ap, t_ob = obt[i % 2]
                var = 0 if i == 0 else (2 if i == 31 else 1)
                for hh in range(2):
                    s_ap, t_s = Ssb[it4 % 4]
                    p_ap, t_p = Pb[it4 % 4]
                    pt_ap, t_pt = PTs[it4 % 2]
                    sc4, t_sc4 = st4[it4 % 4]
                    bS = it4 % 4
                    bT = 4 + it4 % 2
                    bO = 6 + it4 % 2
                    it4 += 1
                    q_ap, t_q = qTa[hh]
                    OP("pe", lambda e, q_ap=q_ap, i=i, bS=bS: e.matmul(psf(bS)[:, 0:384], lhsT=q_ap[:, i * 128:(i + 1) * 128],
                                                                       rhs=kTa[:, i * 128:i * 128 + 384], start=True, stop=True),
                       reads=[t_q, t_kTa], writes=[pst[bS]])
                    OP("dve", lambda e, s_ap=s_ap, bS=bS, var=var: e.scalar_tensor_tensor(out=s_ap[:, 0:384], in0=psf(bS)[:, 0:384], scalar=scale,
                                                                                          in1=band[:, var, :], op0=ALU.mult, op1=ALU.add),
                       reads=[pst[bS], t_band], writes=[t_s])
                    OP("dve", lambda e, s_ap=s_ap, sc4=sc4: e.tensor_reduce(out=sc4[:, 0:1], in_=s_ap[:, 0:385], axis=AX.X, op=ALU.max),
                       reads=[t_s], writes=[t_sc4])
                    OP("dve", lambda e, sc4=sc4: e.tensor_scalar(out=sc4[:, 2:3], in0=sc4[:, 0:1], scalar1=-1.0, scalar2=None, op0=ALU.mult),
                       reads=[t_sc4], writes=[t_sc4])
                    OP("act", lambda e, s_ap=s_ap, p_ap=p_ap, sc4=sc4: e.activation(out=p_ap[:, 0:385], in_=s_ap[:, 0:385], func=AF.Exp, bias=sc4[:, 2:3], scale=1.0,
                                                                                     accum_out=sc4[:, 3:4]),
                       reads=[t_s, t_sc4], writes=[t_p, t_sc4])
                    OP("dve", lambda e, sc4=sc4: e.reciprocal(out=sc4[:, 6:7], in_=sc4[:, 3:4]), reads=[t_sc4], writes=[t_sc4])
                    for kb in range(3):
                        OP("pe", lambda e, kb=kb, p_ap=p_ap, bT=bT: e.transpose(out=psbf(bT)[:, kb * 128:(kb + 1) * 128],
                                                                               in_=p_ap[:, kb * 128:(kb + 1) * 128], identity=identb),
                           reads=[t_p, t_identb], writes=[pst[bT]])
                    OP("act", lambda e, pt_ap=pt_ap, bT=bT: e.copy(out=pt_ap, in_=psbf(bT)[:, 0:384]), reads=[pst[bT]], writes=[t_pt])
                    for kb in range(3):
                        OP("pe", lambda e, kb=kb, pt_ap=pt_ap, bO=bO, i=i: e.matmul(psf(bO)[:, 0:128], lhsT=pt_ap[:, kb * 128:(kb + 1) * 128],
                                                                                    rhs=vat[:, i + kb, :], start=(kb == 0), stop=(kb == 2)),
                           reads=[t_pt, t_vat], writes=[pst[bO]])
                    OP("dve", lambda e, ob_ap=ob_ap, hh=hh, bO=bO, sc4=sc4: e.tensor_scalar(out=ob_ap[:, hh * 128:(hh + 1) * 128], in0=psf(bO)[:, 0:128],
                                                                                            scalar1=sc4[:, 6:7], scalar2=None, op0=ALU.mult),
                       reads=[pst[bO], t_sc4], writes=[t_ob])
                dma("sp", src_o[i * 128:(i + 1) * 128, 256:512], ob_ap, r=[t_ob], w=[t_o_ch[i // 8]])
            if DEBUG:
                dq = nc.dram_tensor("dbg_qT", [2, 128, S], BF16, kind="ExternalOutput").ap()
                dk = nc.dram_tensor("dbg_kT", [128, S + 256], BF16, kind="ExternalOutput").ap()
                dv = nc.dram_tensor("dbg_vat", [128, 34, 128], BF16, kind="ExternalOutput").ap()
                P.final_waits.append(dma("sp", dq[0], qTa[0][0], r=[qTa[0][1]]))
                P.final_waits.append(dma("sp", dq[1], qTa[1][0], r=[qTa[1][1]]))
                P.final_waits.append(dma("sp", dk, kTa, r=[t_kTa]))
                P.final_waits.append(dma("sp", dv, vat, r=[t_vat]))
            A.release(m4)
            A.release(m3)
            A.top = m3b

            stop_here(4)
            m5 = A.mark()
            Sall = [A.alloc([64, 128], BF16) for _ in range(2)]
            Sst = [A.alloc([128], F32) for _ in range(2)]
            kbl = [[A.alloc([8, 128], BF16) for _ in range(2)] for _ in range(2)]
            vbl = [[A.alloc([8, 128], BF16) for _ in range(2)] for _ in range(2)]
            pbl = [[A.alloc([3, 512], BF16) for _ in range(2)] for _ in range(2)]
            vgb = [A.alloc([8, 512], BF16) for _ in range(2)]
            ATs = [[A.alloc([64], BF16) for _ in range(2)] for _ in range(2)]
            ss5s = [A.alloc([4], F32) for _ in range(2)]
            tmp5s = [A.alloc([128], F32) for _ in range(2)]
            junk5s = [A.alloc([128], BF16) for _ in range(2)]
            og5 = [A.alloc([2, 128], BF16) for _ in range(2)]
            junk5, t_junk5 = A.alloc([128], BF16)
            H = slice(0, 64)
            pend_o = []
            for hh in range(2):
                for dr in range(2):
                    OP("pool", lambda e, dr=dr: e.memset(Sst[dr][0], 0.0), writes=[Sst[dr][1]])
                def load_p1(step, hh=hh):
                    for dr in range(2):
                        tb = step if dr == 0 else 7 - step
                        k_ap, t_k = kbl[dr][step % 2]
                        v_ap, t_v = vbl[dr][step % 2]
                        dma("sp", k_ap[H], koutM[hh, dr, tb * 512:(tb + 1) * 512, :].rearrange("(n p) d -> p n d", p=64),
                            r=[t_dram["koutM"]], w=[t_k])
                        dma("sp", v_ap[H], vg[tb * 512:(tb + 1) * 512, hh * 128:(hh + 1) * 128].rearrange("(n p) d -> p n d", p=64),
                            r=[t_dram["vg"]], w=[t_v])
                load_p1(0)
                for step in range(8):
                    if step + 1 < 8:
                        load_p1(step + 1)
                    for cstep in range(8):
                        for dr in range(2):
                            tb = step if dr == 0 else 7 - step
                            cc = cstep if dr == 0 else 7 - cstep
                            n = tb * 8 + cc
                            k_ap, t_k = kbl[dr][step % 2]
                            v_ap, t_v = vbl[dr][step % 2]
                            S_ap, t_S = Sst[dr]
                            Sa_ap, t_Sa = Sall[dr]
                            dec_ap, t_dec = dec[hh][dr]
                            bk = dr * 2 + (cstep % 2)
                            OP("act", lambda e, Sa_ap=Sa_ap, S_ap=S_ap, n=n: e.copy(out=Sa_ap[:, n, :], in_=S_ap), reads=[t_S], writes=[t_Sa])
                            OP("pe", lambda e, k_ap=k_ap, v_ap=v_ap, cc=cc, bk=bk: e.matmul(psf(bk)[:, 0:128], lhsT=k_ap[H, cc, :],
                                                                                             rhs=v_ap[H, cc, :], start=True, stop=True),
                               reads=[t_k, t_v], writes=[pst[bk]])
                            OP("dve", lambda e, S_ap=S_ap, dec_ap=dec_ap, n=n, bk=bk: e.scalar_tensor_tensor(out=S_ap, in0=S_ap, scalar=dec_ap[:, n:n + 1],
                                                                                                             in1=psf(bk)[:, 0:128], op0=ALU.mult, op1=ALU.add),
                               reads=[t_S, t_dec, pst[bk]], writes=[t_S])
                def load_p2(tb, hh=hh):
                    pf_ap, t_pf = pbl[0][tb % 2]
                    pb_ap, t_pb = pbl[1][tb % 2]
                    vg_ap, t_vgb = vgb[tb % 2]
                    tsl = slice(tb * 512, (tb + 1) * 512)
                    dma("sp", pf_ap, hgT[hh, 0].rearrange("a p t -> p a t")[:, :, tsl], r=[t_dram["hgT"]], w=[t_pf])
                    dma("sp", pb_ap, hgT[hh, 1].rearrange("a p t -> p a t")[:, :, tsl], r=[t_dram["hgT"]], w=[t_pb])
                    dma("sp", vg_ap[H], vg[tsl, :].rearrange("(n p) d -> p n d", p=64), r=[t_dram["vg"]], w=[t_vgb])
                load_p2(0)
                for tb in range(8):
                    if tb + 1 < 8:
                        load_p2(tb + 1)
                    pf_ap, t_pf = pbl[0][tb % 2]
                    pb_ap, t_pb = pbl[1][tb % 2]
                    vg_ap, t_vgb = vgb[tb % 2]
                    tsl = slice(tb * 512, (tb + 1) * 512)
                    for i in range(4):
                        gt = tb * 4 + i
                        og_ap, t_og = og5[i % 2]
                        bO = 4 + (i % 2)
                        for c in range(2):
                            cl = i * 2 + c
                            n = tb * 8 + cl
                            cc_ = slice(cl * 64, (cl + 1) * 64)
                            atf, t_atf = ATs[0][c]
                            atb, t_atb = ATs[1][c]
                            ss5, t_ss5 = ss5s[c]
                            tmp5, t_tmp5 = tmp5s[c]
                            junk5, t_junk5 = junk5s[c]
                            bA = c * 2
                            oc = psf(bO)[H, c * 128:(c + 1) * 128]
                            OP("pe", lambda e, pf_ap=pf_ap, cc_=cc_, bA=bA: e.matmul(psf(bA)[H, 0:64], lhsT=pf_ap[:, 1, cc_], rhs=pf_ap[:, 0, cc_], start=True, stop=True),
                               reads=[t_pf], writes=[pst[bA]])
                            OP("dve", lambda e, atf=atf, bA=bA: e.tensor_tensor(out=atf[H], in0=psf(bA)[H, 0:64], in1=maskF[H, 0:64], op=ALU.mult),
                               reads=[pst[bA], t_maskF], writes=[t_atf])
                            OP("pe", lambda e, pb_ap=pb_ap, cc_=cc_, bA=bA: e.matmul(psf(bA + 1)[H, 0:64], lhsT=pb_ap[:, 1, cc_], rhs=pb_ap[:, 0, cc_], start=True, stop=True),
                               reads=[t_pb], writes=[pst[bA + 1]])
                            OP("dve", lambda e, atb=atb, bA=bA: e.tensor_tensor(out=atb[H], in0=psf(bA + 1)[H, 0:64], in1=maskB[H, 0:64], op=ALU.mult),
                               reads=[pst[bA + 1], t_maskB], writes=[t_atb])
                            vv = vg_ap[H, cl, hh * 128:(hh + 1) * 128]
                            OP("pe", lambda e, atf=atf, vv=vv, oc=oc: e.matmul(oc, lhsT=atf[H], rhs=vv, start=True, stop=False),
                               reads=[t_atf, t_vgb], writes=[pst[bO]])
                            OP("pe", lambda e, atb=atb, vv=vv, oc=oc: e.matmul(oc, lhsT=atb[H], rhs=vv, start=False, stop=False),
                               reads=[t_atb, t_vgb], writes=[pst[bO]])
                            OP("pe", lambda e, pf_ap=pf_ap, cc_=cc_, n=n, oc=oc: e.matmul(oc, lhsT=pf_ap[:, 2, cc_], rhs=Sall[0][0][:, n, :], start=False, stop=False),
                               reads=[t_pf, Sall[0][1]], writes=[pst[bO]])
                            OP("pe", lambda e, pb_ap=pb_ap, cc_=cc_, n=n, oc=oc: e.matmul(oc, lhsT=pb_ap[:, 2, cc_], rhs=Sall[1][0][:, n, :], start=False, stop=True),
                               reads=[t_pb, Sall[1][1]], writes=[pst[bO]])
                            OP("act", lambda e, oc=oc, c=c: e.activation(out=junk5[H], in_=oc, func=AF.Square, accum_out=ss5[H, c:c + 1]),
                               reads=[pst[bO]], writes=[t_junk5, t_ss5])
                            rms_rstd(None, ss5[H, c:c + 1], t_ss5, ss5[H, 2 + c:3 + c], t_ss5, 128)
                            OP("dve", lambda e, oc=oc, hh=hh, c=c: e.scalar_tensor_tensor(out=tmp5[H], in0=oc, scalar=ss5[H, 2 + c:3 + c],
                                                                                          in1=hgnb[H, hh * 128:(hh + 1) * 128], op0=ALU.mult, op1=ALU.mult),
                               reads=[pst[bO], t_ss5, t_hgnb], writes=[t_tmp5])
                            OP("pool", lambda e, og_ap=og_ap, vg_ap=vg_ap, cl=cl, hh=hh, c=c: e.tensor_tensor(out=og_ap[H, c, :], in0=tmp5[H],
                                                                                                             in1=vg_ap[H, cl, 256 + hh * 128:256 + (hh + 1) * 128], op=ALU.mult),
                               reads=[t_tmp5, t_vgb], writes=[t_og])
                        dma("sp", src_o[gt * 128:(gt + 1) * 128, hh * 128:(hh + 1) * 128].rearrange("(c p) d -> p c d", p=64), og_ap[H],
                            r=[t_og], w=[t_o_ch[gt // 8]])
                    if hh == 1 and tb % 2 == 1:
                        pend_o.append(ag_start(src_o, 1024, tb // 2, t_o_ch[tb // 2], dst=dst_o))
            A.release(m5)
            A.top = m3
            t_dsto = [t for (_, t) in pend_o]

            stop_here(5)
            m7 = A.mark()
            affown, t_affown = A.alloc([8, 16], F32)
            affT, t_affT = A.alloc([1024], F32)
            posT, t_posT = A.alloc([8, 16], F32)
            selm, t_selm = A.alloc([8, 16], F32)
            sel2, t_sel2 = A.alloc([8, 16], F32)
            affm, t_affm = A.alloc([8, 16], F32)
            idxi, t_idxi = A.alloc([8, 16], I32)
            pmT, t_pmT = A.alloc([32, 4], F32)
            idxg, t_idxg = A.alloc([4, 4], I32)
            idxf, t_idxf = A.alloc([4, 4], F32)
            m7p = A.mark()
            mergedT, t_mergedT = A.alloc([KC, 1024], BF16)
            m7b = A.mark()
            hTo, t_hTo = A.alloc([KC, 1024], BF16)
            oaT, t_oaT = A.alloc([8, 1024], BF16)
            obT, t_obT = A.alloc([8, 1024], BF16)
            for ci in range(4):
                dma("sp", hTo[:, :, ci * 256:(ci + 1) * 256], src_hT[ci * 2048:(ci + 1) * 2048, :].rearrange("(k p) t -> p k t", p=128),
                    r=[t_dram["src_hT"]], w=[t_hTo])
            stop_here(50)
            ots = [A.alloc([4, 512], BF16) for _ in range(2)]
            osrc = dst_o.rearrange("(r t) c -> t r c", r=4)
            for j in range(8):
                ot, t_ot = ots[j % 2]

                for r in range(4):
                    OP("pool", lambda e, ot=ot, j=j, r=r: e.indirect_dma_start(
                        out=ot[:, r, :], out_offset=None, in_=dst_o,
                        in_offset=bass.IndirectOffsetOnAxis(ap=cidx[:, j * 4 + r:j * 4 + r + 1], axis=0)),
                       reads=t_dsto + [t_cidx], writes=[t_ot], dma=True)
                for half in range(2 if KVAR != 1 else 0):
                    bk = 6 + half
                    for rr in range(2):
                        r = half * 2 + rr
                        for w4 in range(4):
                            OP("pe", lambda e, ot=ot, r=r, w4=w4, rr=rr, bk=bk: e.transpose(out=psbf(bk)[:, (rr * 4 + w4) * 128:(rr * 4 + w4 + 1) * 128],
                                                                                           in_=ot[:, r, w4 * 128:(w4 + 1) * 128], identity=identb),
                               reads=[t_ot, t_identb], writes=[pst[bk]])
                    pv = psbf(bk).rearrange("p (a b) -> p a b", b=128)
                    for rr in range(2):
                        r = half * 2 + rr
                        OP("act", lambda e, pv=pv, r=r, rr=rr, j=j: e.copy(out=oaT[:, 2 * r:2 * r + 2, j * 128:(j + 1) * 128], in_=pv[:, rr * 4:rr * 4 + 2, :]),
                           reads=[pst[bk]], writes=[t_oaT])
                        OP("act", lambda e, pv=pv, r=r, rr=rr, j=j: e.copy(out=obT[:, 2 * r:2 * r + 2, j * 128:(j + 1) * 128], in_=pv[:, rr * 4 + 2:rr * 4 + 4, :]),
                           reads=[pst[bk]], writes=[t_obT])
            stop_here(51)
            Wsets = [dict(Wa=A.alloc([8, 256], BF16), Wb=A.alloc([8, 256], BF16), Wga=A.alloc([KC, 256], BF16), Wgb=A.alloc([KC, 256], BF16))
                     for _ in range(2)]
            sgas = [A.alloc([512], F32) for _ in range(2)]
            sgbs = [A.alloc([512], F32) for _ in range(2)]
            wba_v = w_ba.rearrange("(k p) n -> p k n", p=128)
            wbb_v = w_bb.rearrange("(k p) n -> p k n", p=128)
            wgab_v = w_gab.rearrange("(k p) n -> p k n", p=128)

            def load_w7(c8):
                ws = Wsets[c8 % 2]
                csl = slice(c8 * 256, (c8 + 1) * 256)
                dma("pool", ws["Wa"][0], wba_v[:, :, csl], w=[ws["Wa"][1]])
                dma("pool", ws["Wb"][0], wbb_v[:, :, csl], w=[ws["Wb"][1]])
                dma("pool", ws["Wga"][0], wgab_v[:, :, csl], w=[ws["Wga"][1]])
                dma("pool", ws["Wgb"][0], wgab_v[:, :, 2048 + c8 * 256:2048 + (c8 + 1) * 256], w=[ws["Wgb"][1]])
            load_w7(0)
            it7 = 0
            for c8 in range(8):
                if c8 + 1 < 8:
                    load_w7(c8 + 1)
                ws = Wsets[c8 % 2]
                (Wa, t_Wa), (Wb, t_Wb), (Wga, t_Wga), (Wgb, t_Wgb) = ws["Wa"], ws["Wb"], ws["Wga"], ws["Wgb"]
                for th in range(2):
                    tsl = slice(th * 512, (th + 1) * 512)
                    for ci in range(2):
                        cb = c8 * 2 + ci
                        wsl = slice(ci * 128, (ci + 1) * 128)
                        pb0 = (it7 % 2) * 4
                        sga, t_sga = sgas[it7 % 2]
                        sgb, t_sgb = sgbs[it7 % 2]
                        it7 += 1
                        for ch in range(8):
                            OP("pe", lambda e, ch=ch, wsl=wsl, tsl=tsl, Wa=Wa, pb0=pb0: e.matmul(psf(pb0), lhsT=Wa[:, ch, wsl], rhs=oaT[:, ch, tsl], start=(ch == 0), stop=(ch == 7)),
                               reads=[t_Wa, t_oaT], writes=[pst[pb0]])
                        for ch in range(8):
                            OP("pe", lambda e, ch=ch, wsl=wsl, tsl=tsl, Wb=Wb, pb0=pb0: e.matmul(psf(pb0 + 1), lhsT=Wb[:, ch, wsl], rhs=obT[:, ch, tsl], start=(ch == 0), stop=(ch == 7)),
                               reads=[t_Wb, t_obT], writes=[pst[pb0 + 1]])
                        for kc in range(KC):
                            OP("pe", lambda e, kc=kc, wsl=wsl, tsl=tsl, Wga=Wga, pb0=pb0: e.matmul(psf(pb0 + 2), lhsT=Wga[:, kc, wsl], rhs=hTo[:, kc, tsl], start=(kc == 0), stop=(kc == KC - 1)),
                               reads=[t_Wga, t_hTo], writes=[pst[pb0 + 2]])
                        for kc in range(KC):
                            OP("pe", lambda e, kc=kc, wsl=wsl, tsl=tsl, Wgb=Wgb, pb0=pb0: e.matmul(psf(pb0 + 3), lhsT=Wgb[:, kc, wsl], rhs=hTo[:, kc, tsl], start=(kc == 0), stop=(kc == KC - 1)),
                               reads=[t_Wgb, t_hTo], writes=[pst[pb0 + 3]])
                        OP("act", lambda e, sga=sga, pb0=pb0: e.activation(out=sga, in_=psf(pb0 + 2), func=AF.Sigmoid), reads=[pst[pb0 + 2]], writes=[t_sga])
                        OP("act", lambda e, sgb=sgb, pb0=pb0: e.activation(out=sgb, in_=psf(pb0 + 3), func=AF.Sigmoid), reads=[pst[pb0 + 3]], writes=[t_sgb])
                        OP("dve", lambda e, sga=sga, pb0=pb0: e.tensor_tensor(out=sga, in0=sga, in1=psf(pb0), op=ALU.mult), reads=[t_sga, pst[pb0]], writes=[t_sga])
                        OP("dve", lambda e, sgb=sgb, pb0=pb0: e.tensor_tensor(out=sgb, in0=sgb, in1=psf(pb0 + 1), op=ALU.mult), reads=[t_sgb, pst[pb0 + 1]], writes=[t_sgb])
                        OP("pool", lambda e, cb=cb, tsl=tsl, sga=sga, sgb=sgb: e.tensor_tensor(out=mergedT[:, cb, tsl], in0=sga, in1=sgb, op=ALU.add),
                           reads=[t_sga, t_sgb], writes=[t_mergedT])
            stop_here(52)
            A.release(m7b)
            yall, t_yall = A.alloc([8, D], F32)
            m7a = A.mark()
            Wos = [A.alloc([KC, 512], BF16) for _ in range(2)]
            wo_v = w_o.rearrange("(k p) n -> p k n", p=128)
            for cg in range(4):
                Wo, t_Wo = Wos[cg % 2]
                dma("pool", Wo, wo_v[:, :, cg * 512:(cg + 1) * 512], w=[t_Wo])
                for j in range(8):
                    bk = j % 4
                    for mc in range(KC):
                        OP("pe", lambda e, mc=mc, j=j, Wo=Wo, bk=bk: e.matmul(psf(bk), lhsT=mergedT[:, mc, j * 128:(j + 1) * 128], rhs=Wo[:, mc, :],
                                                                              start=(mc == 0), stop=(mc == KC - 1)),
                           reads=[t_mergedT, t_Wo], writes=[pst[bk]])
                    OP("act", lambda e, j=j, cg=cg, bk=bk: e.copy(out=yall[:, j, cg * 512:(cg + 1) * 512], in_=psf(bk)), reads=[pst[bk]], writes=[t_yall])

            A.release(m7a)
            stop_here(7)
            G1, t_G1 = A.alloc([D], F32)
            A2, t_A2 = A.alloc([D], F32)
            B2, t_B2 = A.alloc([D], F32)
            load_modvec(G1, t_G1, 2, 1, "g")
            load_modvec(A2, t_A2, 4, 2, "a")
            load_modvec(B2, t_B2, 3, 2, "b")
            Wr, t_Wr = A.alloc([KC, 16], F32)
            dma("sp", Wr, w_r.rearrange("(k p) n -> p k n", p=128), w=[t_Wr])
            def alias8(k):
                v = mergedT[:, 4 * k:4 * k + 4, :].rearrange("p a b -> p (a b)").bitcast(F32)
                tk = Tok()
                tk.rd = ([t_mergedT.lw] if t_mergedT.lw is not None else []) + list(t_mergedT.rd)
                return v, tk
            xt8s = [A.alloc([D], F32), alias8(0)]
            x1ts = [A.alloc([D], F32), alias8(1)]
            h2fs = [A.alloc([D], F32), alias8(2)]
            h2bs = [A.alloc([D], BF16) for _ in range(2)]
            _h2T0 = A.alloc([KC, 128], F32)
            _v, _tk = alias8(3)
            h2Ts = [_h2T0, (_v.rearrange("p (a b) -> p a b", b=128), _tk)]
            junk8, t_junk8 = A.alloc([D], BF16)
            s8s = [A.alloc([8], F32) for _ in range(2)]
            e16s = [A.alloc([16], F32) for _ in range(2)]
            pend_h2 = []
            for j in range(8):
                (xt8, t_xt8), (x1t, t_x1t), (h2f, t_h2f), (h2b, t_h2b), (h2T, t_h2T) = xt8s[j % 2], x1ts[j % 2], h2fs[j % 2], h2bs[j % 2], h2Ts[j % 2]
                s8, t_s8 = s8s[j % 2]
                e16, t_e16 = e16s[j % 2]
                dma("sp", xt8, x_own[j * 128:(j + 1) * 128, :], w=[t_xt8])
                OP("act", lambda e, j=j: e.activation(out=junk8, in_=yall[:, j, :], func=AF.Square, accum_out=s8[:, 0:1]),
                   reads=[t_yall], writes=[t_junk8, t_s8])
                rms_rstd(None, s8[:, 0:1], t_s8, s8[:, 1:2], t_s8, D)
                OP("dve", lambda e, j=j: e.scalar_tensor_tensor(out=x1t, in0=yall[:, j, :], scalar=s8[:, 1:2], in1=G1, op0=ALU.mult, op1=ALU.mult),
                   reads=[t_yall, t_s8, t_G1], writes=[t_x1t])
                OP("pool", lambda e: e.tensor_tensor(out=x1t, in0=x1t, in1=xt8, op=ALU.add), reads=[t_x1t, t_xt8], writes=[t_x1t])
                dma("sp", x1_d[j * 128:(j + 1) * 128, :], x1t, r=[t_x1t], w=[t_dram["x1_d"]])
                OP("act", lambda e: e.activation(out=junk8, in_=x1t, func=AF.Square, accum_out=s8[:, 2:3]), reads=[t_x1t], writes=[t_junk8, t_s8])
                rms_rstd(None, s8[:, 2:3], t_s8, s8[:, 3:4], t_s8, D)
                OP("dve", lambda e: e.scalar_tensor_tensor(out=h2f, in0=x1t, scalar=s8[:, 3:4], in1=A2, op0=ALU.mult, op1=ALU.mult),
                   reads=[t_x1t, t_s8, t_A2], writes=[t_h2f])
                OP("dve", lambda e: e.tensor_tensor(out=h2f, in0=h2f, in1=B2, op=ALU.add), reads=[t_h2f, t_B2], writes=[t_h2f])
                OP("act", lambda e: e.copy(out=h2b, in_=h2f), reads=[t_h2f], writes=[t_h2b])
                dma("sp", src_h2[j * 128:(j + 1) * 128, :], h2b, r=[t_h2b], w=[t_h2_ch[j // 2]])
                if j % 2 == 1:
                    pend_h2.append(ag_start(src_h2, 256, j // 2, t_h2_ch[j // 2], dst=dst_h2))
                for g in range(4):
                    bk = g
                    for k4 in range(4):
                        kc = g * 4 + k4
                        OP("pe", lambda e, kc=kc, k4=k4, bk=bk: e.transpose(out=psf(bk)[:, k4 * 128:(k4 + 1) * 128], in_=h2f[:, kc * 128:(kc + 1) * 128], identity=identf),
                           reads=[t_h2f, t_identf], writes=[pst[bk]])
                    OP("act", lambda e, g=g, bk=bk: e.copy(out=h2T[:, g * 4:g * 4 + 4, :], in_=psf(bk).rearrange("p (a b) -> p a b", b=128)),
                       reads=[pst[bk]], writes=[t_h2T])
                for kc in range(KC):
                    OP("pe", lambda e, kc=kc: e.matmul(psf(4)[:, 0:16], lhsT=h2T[:, kc, :], rhs=Wr[:, kc, :], start=(kc == 0), stop=(kc == KC - 1)),
                       reads=[t_h2T, t_Wr], writes=[pst[4]])
                OP("dve", lambda e: e.tensor_reduce(out=s8[:, 4:5], in_=psf(4)[:, 0:16], axis=AX.X, op=ALU.max), reads=[pst[4]], writes=[t_s8])
                OP("dve", lambda e: e.tensor_scalar(out=s8[:, 5:6], in0=s8[:, 4:5], scalar1=-1.0, scalar2=None, op0=ALU.mult), reads=[t_s8], writes=[t_s8])
                OP("act", lambda e: e.activation(out=e16, in_=psf(4)[:, 0:16], func=AF.Exp, bias=s8[:, 5:6], scale=1.0, accum_out=s8[:, 6:7]),
                   reads=[pst[4], t_s8], writes=[t_e16, t_s8])
                OP("dve", lambda e: e.reciprocal(out=s8[:, 7:8], in_=s8[:, 6:7]), reads=[t_s8], writes=[t_s8])
                OP("dve", lambda e, j=j: e.tensor_scalar(out=affown[:, j, :], in0=e16, scalar1=s8[:, 7:8], scalar2=None, op0=ALU.mult),
                   reads=[t_e16, t_s8], writes=[t_affown])
                OP("pe", lambda e, j=j: e.transpose(out=psf(5)[0:16, 0:128], in_=affown[:, j, :], identity=identf),
                   reads=[t_affown, t_identf], writes=[pst[5]])
                OP("act", lambda e, j=j: e.copy(out=affT[0:16, j * 128:(j + 1) * 128], in_=psf(5)[0:16, 0:128]), reads=[pst[5]], writes=[t_affT])
            dma("sp", src_aff, affT[0:16, :], r=[t_affT], w=[t_dram["src_aff"]])
            allgather(src_aff, dst_aff, G4, t_dram["src_aff"], t_dram["dst_aff"])
            t_dsth2 = [t for (_, t) in pend_h2]
            A.release(m7p)
            Wg_, t_Wg = A.alloc([KC, 1024], BF16)
            Wu_, t_Wu = A.alloc([KC, 1024], BF16)
            Wd_, t_Wd = A.alloc([8, D], BF16)

            def load_expert(k):
                wg_v = wg[k].rearrange("(k p) n -> p k n", p=128)
                wu_v = wu[k].rearrange("(k p) n -> p k n", p=128)
                wd_v = wd[k].rearrange("(k p) n -> p k n", p=128)
                for g in range(2):
                    dma("pool", Wg_[:, 8 * g:8 * g + 8, :], wg_v[:, 8 * g:8 * g + 8, :], w=[t_Wg])
                    dma("pool", Wu_[:, 8 * g:8 * g + 8, :], wu_v[:, 8 * g:8 * g + 8, :], w=[t_Wu])
                for g in range(2):
                    dma("pool", Wd_[:, 4 * g:4 * g + 4, :], wd_v[:, 4 * g:4 * g + 4, :], w=[t_Wd])
            load_expert(0)
            m8keep = A.mark()

            stop_here(8)
            affR, t_affR = A.alloc([S], F32)
            junk9, t_junk9 = A.alloc([S], F32)
            maskR, t_maskR = A.alloc([S], F32)
            r9, t_r9 = A.alloc([8], F32)
            av_ = dst_aff.rearrange("(q e) t -> e q t", q=4)
            dma("sp", affR[0:16, :].rearrange("p (q t) -> p q t", q=4), av_, r=[t_dram["dst_aff"]], w=[t_affR])
            R32 = slice(0, 16)
            OP("dve", lambda e: e.memset(r9[R32, :], 0.5), writes=[t_r9])
            NIT = 24
            for k in range(NIT):
                wk = 2.0 ** -(k + 1)
                OP("dve", lambda e: e.tensor_scalar(out=junk9[R32, :], in0=affR[R32, :], scalar1=r9[R32, 1:2], scalar2=0.0, op0=ALU.is_gt, op1=ALU.add,
                                                    accum_out=r9[R32, 2:3]),
                   reads=[t_affR, t_r9], writes=[t_junk9, t_r9])
                OP("dve", lambda e, wk=wk: e.tensor_scalar(out=r9[R32, 3:4], in0=r9[R32, 2:3], scalar1=511.5, scalar2=wk, op0=ALU.is_gt, op1=ALU.mult),
                   reads=[t_r9], writes=[t_r9])
                OP("dve", lambda e, wk=wk: e.scalar_tensor_tensor(out=r9[R32, 1:2], in0=r9[R32, 3:4], scalar=-0.5 * wk, in1=r9[R32, 1:2], op0=ALU.add, op1=ALU.add),
                   reads=[t_r9], writes=[t_r9])
            OP("dve", lambda e: e.tensor_scalar(out=r9[R32, 0:1], in0=r9[R32, 1:2], scalar1=-(2.0 ** -(NIT + 1)), scalar2=None, op0=ALU.add),
               reads=[t_r9], writes=[t_r9])
            OP("dve", lambda e: e.tensor_scalar(out=maskR[R32, :], in0=affR[R32, :], scalar1=r9[R32, 0:1], scalar2=None, op0=ALU.is_gt),
               reads=[t_affR, t_r9], writes=[t_maskR])
            OP("pool", lambda e: e.memset(junk9[R32, :], 1.0), writes=[t_junk9])
            OP("dve", lambda e: e.tensor_tensor_scan(out=affR[R32, :], data0=junk9[R32, :], data1=maskR[R32, :], initial=0.0, op0=ALU.mult, op1=ALU.add),
               reads=[t_junk9, t_maskR], writes=[t_affR])
            OP("dve", lambda e: e.tensor_tensor(out=affR[R32, :], in0=affR[R32, :], in1=maskR[R32, :], op=ALU.mult), reads=[t_affR, t_maskR], writes=[t_affR])
            OP("dve", lambda e: e.tensor_scalar(out=affR[R32, :], in0=affR[R32, :], scalar1=-1.0, scalar2=None, op0=ALU.add), reads=[t_affR], writes=[t_affR])
            dma("sp", route_d, affR[R32, :], r=[t_affR], w=[t_dram["route_d"]])
            t_rd2 = Tok()
            for qr in range(4):
                dma("sp", route_d2[qr * 16:(qr + 1) * 16, :], affR[R32, qr * 1024:(qr + 1) * 1024], r=[t_affR], w=[t_rd2])
            A.release(m8keep)
            slab, t_slab = A.alloc([1024], F32)

            OP("pool", lambda e: e.indirect_dma_start(
                out=slab[0:16, :], out_offset=None, in_=route_d2,
                in_offset=bass.IndirectOffsetOnAxis(ap=cidx[0:16, 33:34], axis=0)),
               reads=[t_rd2, t_cidx], writes=[t_slab], dma=True)
            for j in range(8):
                OP("pe", lambda e, j=j: e.transpose(out=psf(0)[:, j * 16:(j + 1) * 16], in_=slab[0:16, j * 128:(j + 1) * 128], identity=identf[0:16, 0:16]),
                   reads=[t_slab, t_identf], writes=[pst[0]])
            OP("act", lambda e: e.copy(out=posT, in_=psf(0)[:, 0:128].rearrange("p (a b) -> p a b", b=16)), reads=[pst[0]], writes=[t_posT])
            OP("dve", lambda e: e.tensor_scalar(out=selm, in0=posT, scalar1=-0.5, scalar2=None, op0=ALU.is_gt), reads=[t_posT], writes=[t_selm])
            OP("dve", lambda e: e.tensor_scalar(out=sel2, in0=posT, scalar1=511.5, scalar2=None, op0=ALU.is_lt), reads=[t_posT], writes=[t_sel2])
            OP("dve", lambda e: e.tensor_tensor(out=selm, in0=selm, in1=sel2, op=ALU.mult), reads=[t_selm, t_sel2], writes=[t_selm])
            OP("dve", lambda e: e.tensor_tensor(out=affm, in0=affown, in1=selm, op=ALU.mult), reads=[t_affown, t_selm], writes=[t_affm])
            OP("dve", lambda e: e.tensor_scalar(out=sel2, in0=posT, scalar1=255.5, scalar2=768.0, op0=ALU.is_gt, op1=ALU.mult), reads=[t_posT], writes=[t_sel2])
            OP("dve", lambda e: e.tensor_tensor(out=posT, in0=posT, in1=sel2, op=ALU.add), reads=[t_posT, t_sel2], writes=[t_posT])
            OP("dve", lambda e: e.tensor_tensor(out=posT, in0=posT, in1=rowbase.unsqueeze(1).to_broadcast([128, 8, 16]), op=ALU.add),
               reads=[t_posT, t_rowbase], writes=[t_posT])
            OP("dve", lambda e: e.tensor_tensor(out=posT, in0=posT, in1=selm, op=ALU.mult), reads=[t_posT, t_selm], writes=[t_posT])
            OP("dve", lambda e: e.tensor_copy(out=idxi, in_=posT), reads=[t_posT], writes=[t_idxi])
            pm, t_pm = A.alloc([S], F32)
            ohall, t_ohall = A.alloc([32, 512], BF16)
            OP("pool", lambda e: e.indirect_dma_start(
                out=pm[0:4, :], out_offset=None, in_=route_d,
                in_offset=bass.IndirectOffsetOnAxis(ap=cidx[0:4, 32:33], axis=0)),
               reads=[t_dram["route_d"], t_cidx], writes=[t_pm], dma=True)
            for tt in range(32):
                OP("pe", lambda e, tt=tt: e.transpose(out=psf(1)[:, tt * 4:(tt + 1) * 4], in_=pm[0:4, tt * 128:(tt + 1) * 128], identity=identf[0:4, 0:4]),
                   reads=[t_pm, t_identf], writes=[pst[1]])
            OP("act", lambda e: e.copy(out=pmT, in_=psf(1)[:, 0:128].rearrange("p (a b) -> p a b", b=4)), reads=[pst[1]], writes=[t_pmT])
            psidx = psf(2)[:, 0:32].rearrange("p (a b c) -> p a b c", b=4, c=2)
            for pp in range(4):
                for tt in range(32):
                    OP("dve", lambda e, tt=tt, pp=pp: e.tensor_scalar(out=ohall[:, tt, :], in0=iota, scalar1=pmT[:, tt, pp:pp + 1], scalar2=None, op0=ALU.is_equal),
                       reads=[t_iota, t_pmT], writes=[t_ohall])
                for s4 in range(4):
                    for tt in range(32):
                        OP("pe", lambda e, tt=tt, pp=pp, s4=s4: e.matmul(psidx[:, pp, s4, :], lhsT=ohall[:, tt, s4 * 128:(s4 + 1) * 128], rhs=tval[:, tt, :],
                                                                         start=(tt == 0), stop=(tt == 31)),
                           reads=[t_ohall, t_tval], writes=[pst[2]])
            idx2, t_idx2 = A.alloc([4, 4, 2], F32)
            OP("act", lambda e: e.copy(out=idx2, in_=psidx), reads=[pst[2]], writes=[t_idx2])
            OP("dve", lambda e: e.scalar_tensor_tensor(out=idxf, in0=idx2[:, :, :, 0], scalar=64.0, in1=idx2[:, :, :, 1], op0=ALU.mult, op1=ALU.add),
               reads=[t_idx2], writes=[t_idxf])
            OP("dve", lambda e: e.tensor_copy(out=idxg, in_=idxf), reads=[t_idxf], writes=[t_idxg])

            stop_here(9)
            A.release(m8keep)
            m10 = A.mark()
            xgs = [A.alloc([D], BF16) for _ in range(2)]
            xeTs = [A.alloc([KC, 512], BF16) for _ in range(2)]
            aT, t_aT = A.alloc([8, 512], BF16)
            sgf, t_sgf = A.alloc([512], F32)
            ysb = [A.alloc([D], BF16) for _ in range(2)]
            pend_Y = []
            dvY = dst_Y.rearrange("(r c m) x -> c r m x", r=4, c=8)

            def load_gu(k):
                wg_v = wg[k].rearrange("(k p) n -> p k n", p=128)
                wu_v = wu[k].rearrange("(k p) n -> p k n", p=128)
                for g in range(2):
                    dma("pool", Wg_[:, 8 * g:8 * g + 8, :], wg_v[:, 8 * g:8 * g + 8, :], w=[t_Wg])
                    dma("pool", Wu_[:, 8 * g:8 * g + 8, :], wu_v[:, 8 * g:8 * g + 8, :], w=[t_Wu])

            def load_d(k):
                wd_v = wd[k].rearrange("(k p) n -> p k n", p=128)
                for g in range(2):
                    dma("pool", Wd_[:, 4 * g:4 * g + 4, :], wd_v[:, 4 * g:4 * g + 4, :], w=[t_Wd])

            def gather_x(k):
                xeT, t_xeT = xeTs[k % 2]
                for s4 in range(4):
                    xg, t_xg = xgs[s4 % 2]
                    OP("pool", lambda e, xg=xg, k=k, s4=s4: e.indirect_dma_start(
                        out=xg, out_offset=None, in_=dst_h2,
                        in_offset=bass.IndirectOffsetOnAxis(ap=idxg[:, k, s4:s4 + 1], axis=0),
                        ),
                       reads=t_dsth2 + [t_idxg], writes=[t_xg], dma=True)
                    for half in range(2):
                        bk = 6 + half
                        for k8 in range(8):
                            kc = half * 8 + k8
                            OP("pe", lambda e, xg=xg, kc=kc, k8=k8, bk=bk: e.transpose(out=psbf(bk)[:, k8 * 128:(k8 + 1) * 128],
                                                                                       in_=xg[:, kc * 128:(kc + 1) * 128], identity=identb),
                               reads=[t_xg, t_identb], writes=[pst[bk]])
                        OP("act", lambda e, half=half, bk=bk, s4=s4, xeT=xeT: e.copy(out=xeT[:, half * 8:half * 8 + 8, s4 * 128:(s4 + 1) * 128],
                                                                                  in_=psbf(bk).rearrange("p (a b) -> p a b", b=128)),
                           reads=[pst[bk]], writes=[t_xeT])

            def gateup(k):
                xeT, t_xeT = xeTs[k % 2]
                for ft in range(8):
                    fsl = slice(ft * 128, (ft + 1) * 128)
                    bg = (ft % 2) * 2
                    for kc in range(KC):
                        OP("pe", lambda e, kc=kc, fsl=fsl, bg=bg, xeT=xeT: e.matmul(psf(bg), lhsT=Wg_[:, kc, fsl], rhs=xeT[:, kc, :], start=(kc == 0), stop=(kc == KC - 1)),
                           reads=[t_Wg, t_xeT], writes=[pst[bg]])
                    for kc in range(KC):
                        OP("pe", lambda e, kc=kc, fsl=fsl, bg=bg, xeT=xeT: e.matmul(psf(bg + 1), lhsT=Wu_[:, kc, fsl], rhs=xeT[:, kc, :], start=(kc == 0), stop=(kc == KC - 1)),
                           reads=[t_Wu, t_xeT], writes=[pst[bg + 1]])
                    OP("act", lambda e, bg=bg: e.activation(out=sgf, in_=psf(bg), func=AF.Silu), reads=[pst[bg]], writes=[t_sgf])
                    OP("dve", lambda e, bg=bg, ft=ft: e.tensor_tensor(out=aT[:, ft, :], in0=sgf, in1=psf(bg + 1), op=ALU.mult),
                       reads=[t_sgf, pst[bg + 1]], writes=[t_aT])

            def down(k):
                pp = k
                for s4 in range(4):
                    y_ap, t_y = ysb[s4 % 2]
                    for cg in range(4):
                        bk = 4 + (cg % 2)
                        for ft in range(8):
                            OP("pe", lambda e, ft=ft, s4=s4, cg=cg, bk=bk: e.matmul(psf(bk), lhsT=aT[:, ft, s4 * 128:(s4 + 1) * 128],
                                                                                    rhs=Wd_[:, ft, cg * 512:(cg + 1) * 512], start=(ft == 0), stop=(ft == 7)),
                               reads=[t_aT, t_Wd], writes=[pst[bk]])
                        OP("act", lambda e, y_ap=y_ap, cg=cg, bk=bk: e.copy(out=y_ap[:, cg * 512:(cg + 1) * 512], in_=psf(bk)), reads=[pst[bk]], writes=[t_y])
                    dma("sp", src_Y[pp * 512 + s4 * 128:pp * 512 + (s4 + 1) * 128, :], y_ap, r=[t_y], w=[t_Y_ch[pp * 2 + s4 // 2]])

            def finish_Y(k):
                for ci in (2 * k, 2 * k + 1):
                    tmp, ttmp = pend_Y[ci]
                    dma("sp", dvY[ci], tmp.rearrange("(r m) x -> r m x", r=4), r=[ttmp], w=[t_dram["dst_Y"]])

            gather_x(0)
            for k in range(4):
                gateup(k)
                if k + 1 < 4:
                    load_gu(k + 1)
                    gather_x(k + 1)
                down(k)
                if k + 1 < 4:
                    load_d(k + 1)
                for ci in (2 * k, 2 * k + 1):
                    pend_Y.append(ag_start(src_Y, 256, ci, t_Y_ch[ci], dst=dst_Y))
            t_dstY = [t for (_, t) in pend_Y]
            A.release(m10)

            stop_here(10)
            G2, t_G2 = A.alloc([D], F32)
            load_modvec(G2, t_G2, 5, 3, "g")
            gbs = [A.alloc([D], BF16) for _ in range(4)]
            accs = [A.alloc([D], F32) for _ in range(2)]
            x1rs = [A.alloc([D], F32) for _ in range(2)]
            dgs = [A.alloc([128], BF16) for _ in range(2)]
            junk11, t_junk11 = A.alloc([D], BF16)
            s11s = [A.alloc([4], F32) for _ in range(2)]
            for gb_, t_gb in gbs:
                OP("pool", lambda e, gb_=gb_: e.memset(gb_, 0.0), writes=[t_gb])
            gi = 0
            finals = []
            for j in range(8):
                acc, t_acc = accs[j % 2]
                x1r, t_x1r = x1rs[j % 2]
                s11, t_s11 = s11s[j % 2]
                pb0 = (j % 2) * 4
                dma("sp", x1r, x1_d[j * 128:(j + 1) * 128, :], r=[t_dram["x1_d"]], w=[t_x1r])
                for ex in range(16):
                    gb_, t_gb = gbs[gi % 4]
                    dg, t_dg = dgs[gi % 2]
                    gi += 1
                    OP("pool", lambda e, gb_=gb_, j=j, ex=ex: e.indirect_dma_start(
                        out=gb_, out_offset=None, in_=dst_Y,
                        in_offset=bass.IndirectOffsetOnAxis(ap=idxi[:, j, ex:ex + 1], axis=0),
                        ),
                       reads=t_dstY + [t_idxi, t_gb], writes=[t_gb], dma=True)
                    OP("dve", lambda e, dg=dg, j=j, ex=ex: e.tensor_scalar(out=dg, in0=identb, scalar1=affm[:, j, ex:ex + 1], scalar2=None, op0=ALU.mult),
                       reads=[t_identb, t_affm], writes=[t_dg])
                    for cg in range(4):
                        OP("pe", lambda e, dg=dg, gb_=gb_, cg=cg, ex=ex, pb0=pb0: e.matmul(psf(pb0 + cg), lhsT=dg, rhs=gb_[:, cg * 512:(cg + 1) * 512],
                                                                                          start=(ex == 0), stop=(ex == 15)),
                           reads=[t_dg, t_gb], writes=[pst[pb0 + cg]])
                for cg in range(4):
                    OP("act", lambda e, acc=acc, cg=cg, pb0=pb0: e.copy(out=acc[:, cg * 512:(cg + 1) * 512], in_=psf(pb0 + cg)), reads=[pst[pb0 + cg]], writes=[t_acc])
                OP("act", lambda e, acc=acc, s11=s11: e.activation(out=junk11, in_=acc, func=AF.Square, accum_out=s11[:, 0:1]), reads=[t_acc], writes=[t_junk11, t_s11])
                rms_rstd(None, s11[:, 0:1], t_s11, s11[:, 1:2], t_s11, D)
                OP("dve", lambda e, acc=acc, s11=s11: e.scalar_tensor_tensor(out=acc, in0=acc, scalar=s11[:, 1:2], in1=G2, op0=ALU.mult, op1=ALU.mult),
                   reads=[t_acc, t_s11, t_G2], writes=[t_acc])
                OP("dve", lambda e, acc=acc, x1r=x1r: e.tensor_tensor(out=acc, in0=acc, in1=x1r, op=ALU.add), reads=[t_acc, t_x1r], writes=[t_acc])
                finals.append(dma("sp", out[j * 128:(j + 1) * 128, :], acc, r=[t_acc]))
            P.final_waits.extend(finals)
        except _Stop:
            pass
        if DEBUG:
            for nm, src, shp, dt in (("dbg_o", src_o, [S, 512], BF16), ("dbg_x1", x1_d, [1024, D], F32),
                                     ("dbg_route", route_d, [16, S], F32), ("dbg_aff", dst_aff, [64, 1024], F32),
                                     ("dbg_mod", dst_mod, [4, 3072], F32), ("dbg_hT", src_hT, [4 * KC * 128, 256], BF16),
                                     ("dbg_Y", src_Y, [2048, D], BF16), ("dbg_h2", src_h2, [1024, D], BF16),
                                     ("dbg_vg", vg, [S, 512], BF16), ("dbg_hgT", hgT.rearrange("a b c p t -> (a b c p) t"), [12 * 128, S], BF16),
                                     ("dbg_koutM", koutM.rearrange("a b t d -> (a b t) d"), [4 * S, 128], BF16),
                                     ):
                dd_ = nc.dram_tensor(nm, shp, dt, kind="ExternalOutput").ap()
                key = {"dbg_o": "src_o", "dbg_x1": "x1_d", "dbg_route": "route_d", "dbg_aff": "dst_aff", "dbg_mod": "dst_mod",
                       "dbg_hT": "src_hT", "dbg_Y": "src_Y", "dbg_h2": "src_h2", "dbg_vg": "vg", "dbg_hgT": "hgT", "dbg_koutM": "koutM", "dbg_dsthT": "dst_hT"}[nm]
                P.final_waits.append(dma("sp", dd_, src, r=[t_dram[key]]))
        P.emit(nc, st)
    return nc


def _consts():
    bf = ml_dtypes.bfloat16
    c = {}
    c["c_identb"] = np.eye(128, dtype=np.float32).astype(bf)
    c["c_identf"] = np.eye(128, dtype=np.float32)
    m = np.arange(128)[:, None]
    l = np.arange(128)[None, :]
    same = (m // 64) == (l // 64)
    c["c_maskF"] = (same & (l >= m)).astype(np.float32)
    c["c_maskB"] = (same & (m >= l)).astype(np.float32)
    ps = np.zeros((128, 128), np.float32)
    for mm_ in range(128):
        ps[(mm_ + 64) % 128, mm_] = 1.0
    c["c_pswap"] = ps.astype(bf)
    half = 64
    inv = (10000.0 ** (-np.arange(half, dtype=np.float32) / half)).astype(np.float32)
    sm = np.zeros((128, 4), np.float32)
    sm[:, 0] = np.concatenate([inv, inv])
    sm[:, 1] = np.concatenate([-np.ones(64), np.ones(64)])
    sm[:, 2] = -math.pi * sm[:, 1]
    sm[:, 3] = -math.pi
    c["c_small"] = sm
    qi = np.arange(128)[:, None]
    kj = np.arange(384)[None, :]
    bandok = np.abs(kj - 128 - qi) <= 128
    bm = np.zeros((128, 3, 384), np.float32)
    for var in range(3):
        ok = bandok.copy()
        if var == 0:
            ok &= (kj >= 128)
        if var == 2:
            ok &= (kj < 256)
        bm[:, var, :] = np.where(ok, 0.0, -30000.0)
    c["c_band"] = bm
    rm = np.ones((128, 512), np.float32)
    rm[:, ::64] = 0.0
    c["c_rmask"] = rm
    c["c_iota"] = np.broadcast_to(np.arange(512, dtype=np.float32)[None, :], (128, 512)).copy()
    t = np.arange(32)[None, :] * 128 + np.arange(128)[:, None]
    trow = ((t // 256) % 4) * 1024 + (t // 1024) * 256 + (t % 256)
    c["c_tval"] = np.stack([trow // 64, trow % 64], axis=-1).astype(np.float32).astype(bf)
    return c


_NC_CACHE = {}


def kernel(x, c, positions, w_ada, b_ada, g_pre_mix, g_post_mix, g_pre_ffn, g_post_ffn,
           w_in, hg_lb_logits, hg_out_norm, attn_sink, w_branch_a, w_branch_b, w_out,
           w_router, w_exp_gate, w_exp_up, w_exp_down):
    f = lambda a: np.ascontiguousarray(np.asarray(a))
    x, c, positions = f(x), f(c), f(positions)
    w_ada, b_ada, w_in = f(w_ada)[0], f(b_ada)[0], f(w_in)[0]
    lbl_all = f(hg_lb_logits)
    hgn_all = f(hg_out_norm)[0]
    sink_all = f(attn_sink)[0]
    w_ba, w_bb, w_o, w_r = f(w_branch_a)[0], f(w_branch_b)[0], f(w_out)[0], f(w_router)[0]
    wg_all, wu_all, wd_all = f(w_exp_gate)[0], f(w_exp_up)[0], f(w_exp_down)[0]
    gv = np.stack([f(g_pre_mix)[0], f(g_post_mix)[0], f(g_pre_ffn)[0], f(g_post_ffn)[0]], axis=0)
    consts = _consts()
    if "nc" not in _NC_CACHE:
        _NC_CACHE["nc"] = build_program()
    nc = _NC_CACHE["nc"]
    O = {"hq": 0, "ff": 1024, "fb": 2048, "hi": 3072, "hg": 4096, "aq": 5120, "ak": 6144, "av": 6400, "ga": 6656, "gb": 8704}
    w_gab = np.ascontiguousarray(w_in[:, O["ga"]:O["ga"] + 4096])
    in_maps = []
    for core in range(8):
        b, q = core // 4, core % 4
        hs = [2 * q, 2 * q + 1]
        kvh = q // 2
        cols_fm = []
        for base in ("hq", "ff", "fb", "aq"):
            for h in hs:
                cols_fm.append(np.arange(O[base] + h * 128, O[base] + (h + 1) * 128))
        cols_fm.append(np.arange(O["ak"] + kvh * 128, O["ak"] + (kvh + 1) * 128))
        cols_fm = np.concatenate(cols_fm)
        cols_tm = np.concatenate([np.arange(O["hi"] + h * 128, O["hi"] + (h + 1) * 128) for h in hs] +
                                 [np.arange(O["hg"] + h * 128, O["hg"] + (h + 1) * 128) for h in hs] +
                                 [np.arange(O["av"] + kvh * 128, O["av"] + (kvh + 1) * 128)])
        lbl = np.zeros((128, 4, 2), np.float32)
        for dr in range(2):
            for hh in range(2):
                h = hs[hh]
                lbl[:, dr * 2 + hh, :] = lbl_all[dr, :, h * 128:(h + 1) * 128].T
        cidx_h = np.zeros((128, 40), np.int32)
        pp_ = np.arange(128)
        for j_ in range(8):
            for r_ in range(4):
                cidx_h[:, j_ * 4 + r_] = q * 4096 + r_ * 1024 + j_ * 128 + pp_
        cidx_h[:, 32] = np.minimum(4 * q + pp_, 15)
        cidx_h[:, 33] = np.minimum(16 * q + pp_, 63)
        rowbase = np.array([(e % 4) * 2048 + (e // 4) * 256 for e in range(16)], np.float32)
        m = {
            "x_own": x[b, q * 1024:(q + 1) * 1024, :],
            "c_b": np.ascontiguousarray(c[b].reshape(KC, 128).T),
            "pos_b": positions[b:b + 1, :].astype(np.int32),
            "w_ada_q": np.ascontiguousarray(w_ada[:, q * 3072:(q + 1) * 3072]),
            "b_ada_q": b_ada[None, q * 3072:(q + 1) * 3072],
            "gvecs": gv,
            "w_fm": np.ascontiguousarray(w_in[:, cols_fm]),
            "w_tm": np.ascontiguousarray(w_in[:, cols_tm]),
            "w_gab": w_gab,
            "lbl": lbl,
            "hgn": np.ascontiguousarray(hgn_all[hs].reshape(1, 256)),
            "sink": np.ascontiguousarray(sink_all[hs].reshape(1, 2)),
            "w_ba": w_ba, "w_bb": w_bb, "w_o": w_o, "w_r": w_r,
            "wg": wg_all[4 * q:4 * q + 4], "wu": wu_all[4 * q:4 * q + 4], "wd": wd_all[4 * q:4 * q + 4],
            "c_rowbase": np.broadcast_to(rowbase[None, :], (128, 16)).copy(),
            "c_idx": cidx_h,
        }
        m.update(consts)
        in_maps.append({k: np.ascontiguousarray(v) for k, v in m.items()})
    res = run_bass_kernel_spmd(nc, in_maps, core_ids=list(range(8)))
    outp = np.zeros((2, S, D), np.float32)
    for core in range(8):
        b, q = core // 4, core % 4
        outp[b, q * 1024:(q + 1) * 1024, :] = res.results[core]["out"]
    if DEBUG:
        kernel.debug = res.results
    return outp
```

```python
import os
import math
import types
from contextlib import ExitStack
import numpy as np
import ml_dtypes
import concourse.bass as bass
import concourse.mybir as mybir
from concourse.bass_utils import run_bass_kernel_spmd

F32 = mybir.dt.float32
BF16 = mybir.dt.bfloat16
I32 = mybir.dt.int32
U8 = mybir.dt.uint8
AF = mybir.ActivationFunctionType
ALU = mybir.AluOpType
AX = mybir.AxisListType
DSZ = {F32: 4, BF16: 2, I32: 4, U8: 1}

DEBUG = bool(int(os.environ.get("KDEBUG", "0")))
KSTOP = int(os.environ.get("KSTOP", "99"))
KVAR = int(os.environ.get("KVAR", "0"))


class _Stop(Exception):
    pass
D = 2048
S = 4096
KC = 16
EPS = 1e-6
BIG = 1.0e6
G4 = [[0, 1, 2, 3], [4, 5, 6, 7]]
G8 = [list(range(8))]


class Tok:
    __slots__ = ("lw", "rd")

    def __init__(self):
        self.lw = None
        self.rd = []


class Op:
    __slots__ = ("eng", "fn", "deps", "dma", "ms", "msidx", "dsem", "dcount", "dprev", "seq")

    def __init__(self, eng, fn, dma):
        self.eng = eng
        self.fn = fn
        self.deps = []
        self.dma = dma
        self.ms = False
        self.msidx = None
        self.dsem = None
        self.dcount = None
        self.dprev = None


ENGS = ("pe", "act", "dve", "pool", "sp")


def _freeze(f):
    if getattr(f, "__closure__", None) is None:
        return f
    cells = []
    for c in f.__closure__:
        try:
            cells.append(types.CellType(c.cell_contents))
        except ValueError:
            cells.append(c)
    return types.FunctionType(f.__code__, f.__globals__, f.__name__, f.__defaults__, tuple(cells))


class Prog:
    def __init__(self, n_dma_sems=40):
        self.ops = {e: [] for e in ENGS}
        self.n_dma_sems = n_dma_sems
        self.all = []
        self.final_waits = []

    def op(self, eng, fn, reads=(), writes=(), dma=False):
        o = Op(eng, _freeze(fn), dma)
        deps = []
        for t in reads:
            if t.lw is not None:
                deps.append(t.lw)
        for t in writes:
            if t.lw is not None:
                deps.append(t.lw)
            deps.extend(t.rd)
        o.seq = len(self.all)
        seen = set()
        last = {}
        for d in deps:
            if id(d) in seen:
                continue
            seen.add(id(d))
            if d.dma:
                o.deps.append(d)
                continue
            if (not dma) and d.eng == "pe" and eng == "pe":
                continue
            if d.eng not in last or last[d.eng].seq < d.seq:
                last[d.eng] = d
        for d in last.values():
            o.deps.append(d)
            d.ms = True
        for t in reads:
            if not dma:
                t.rd = [x for x in t.rd if x.dma or x.eng != eng]
            t.rd.append(o)
        for t in writes:
            t.lw = o
            t.rd = []
        self.ops[eng].append(o)
        self.all.append(o)
        return o

    def get_dyn(self, eng, key):
        if getattr(self, "dyn", None) is None:
            pid = eng.partition_id()
            self.dyn = {"q1024": eng.snap((pid % 4) * 1024), "q4": eng.snap((pid % 4) * 4)}
        return self.dyn[key]

    def emit(self, nc, stack):
        for e in ENGS:
            k = 0
            for o in self.ops[e]:
                if (not o.dma) and o.ms:
                    k += 1
                    o.msidx = k
        esem = {e: stack.enter_context(nc.semaphore("s_" + e)) for e in ENGS}
        dsems = [stack.enter_context(nc.semaphore("d%d" % i)) for i in range(self.n_dma_sems)]
        dcum = [0] * self.n_dma_sems
        k = 0
        ksw = 0
        n_sw = 12
        n_hw = self.n_dma_sems - n_sw
        for o in self.all:
            if o.dma == "cc":
                dsems.append(stack.enter_context(nc.semaphore("cc%d" % len(dsems))))
                o.dsem = len(dsems) - 1
                o.dprev = 0
                o.dcount = 1
            elif o.dma:
                if o.eng == "pool":
                    i = n_hw + (ksw % n_sw)
                    ksw += 1
                else:
                    i = k % n_hw
                    k += 1
                o.dsem = i
                o.dprev = dcum[i]
                dcum[i] += 16
                o.dcount = dcum[i]
        finals = self.final_waits
        LIMIT = 2400
        segs = [[]]
        cnt = {e: 0 for e in ENGS}
        for o in self.all:
            c = 2 + len(o.deps)
            if cnt[o.eng] + c > LIMIT:
                segs.append([])
                cnt = {e: 0 for e in ENGS}
            cnt[o.eng] += c
            segs[-1].append(o)
        waited_all = {e: {} for e in ENGS}

        def run(ename, eng, seg, last):
            waited = waited_all[ename]

            def w(key, sem, val):
                if val <= 0 or waited.get(key, 0) >= val:
                    return
                waited[key] = val
                eng.wait_ge(sem, val)

            for o in seg:
                if o.eng != ename:
                    continue
                for d in o.deps:
                    if d.dma:
                        w(("d", d.dsem), dsems[d.dsem], d.dcount)
                    else:
                        w(("e", d.eng), esem[d.eng], d.msidx)
                if o.dma:
                    w(("d", o.dsem), dsems[o.dsem], o.dprev)
                    ins = o.fn(eng)
                    ins.then_inc(dsems[o.dsem], 1 if o.dma == "cc" else 16)
                else:
                    ins = o.fn(eng)
                    if o.ms:
                        ins.then_inc(esem[ename], 1)
            if last and ename == "sp":
                for d in finals:
                    if d.dma:
                        w(("d", d.dsem), dsems[d.dsem], d.dcount)
                    else:
                        w(("e", d.eng), esem[d.eng], d.msidx)

        for si, seg in enumerate(segs):
            last = si == len(segs) - 1
            with nc.Block() as block:
                @block.sync
                def _(eng):
                    run("sp", eng, seg, last)

                @block.tensor
                def _(eng):
                    run("pe", eng, seg, last)

                @block.scalar
                def _(eng):
                    run("act", eng, seg, last)

                @block.vector
                def _(eng):
                    run("dve", eng, seg, last)

                @block.gpsimd
                def _(eng):
                    run("pool", eng, seg, last)


class Arena:
    def __init__(self, ap_u8, nbytes):
        self.ap = ap_u8
        self.n = nbytes
        self.top = 0
        self.hist = []

    def mark(self):
        return self.top

    def release(self, m):
        self.top = m

    def alloc(self, shape, dt):
        n = DSZ[dt]
        for s in shape:
            n *= s
        start = (self.top + 31) // 32 * 32
        end = start + n
        assert end <= self.n, ("SBUF arena overflow", end, self.n)
        self.top = end
        tok = Tok()
        live = []
        for (s0, e0, t0) in self.hist:
            if s0 < end and start < e0:
                if t0.lw is not None:
                    tok.rd.append(t0.lw)
                tok.rd.extend(t0.rd)
            if t0.lw is not None or t0.rd:
                live.append((s0, e0, t0))
        self.hist = [h for h in self.hist if not (h[0] >= start and h[1] <= end)]
        self.hist.append((start, end, tok))
        v = self.ap[:, start:end].bitcast(dt)
        if len(shape) == 2:
            v = v.rearrange("p (a b) -> p a b", b=shape[1])
        elif len(shape) == 3:
            v = v.rearrange("p (a b c) -> p a b c", b=shape[1], c=shape[2])
        return v, tok


def build_program():
    nc = bass.Bass("TRN2", target_bir_lowering=False)
    P = Prog()

    def din(name, shape, dt):
        return nc.dram_tensor(name, shape, dt, kind="ExternalInput").ap()

    def dscr(name, shape, dt):
        return nc.dram_tensor(name, shape, dt, kind="Internal").ap()

    x_own = din("x_own", [1024, D], F32)
    c_b = din("c_b", [128, KC], F32)
    pos_b = din("pos_b", [1, S], I32)
    w_ada_q = din("w_ada_q", [D, 3072], F32)
    b_ada_q = din("b_ada_q", [1, 3072], F32)
    gvecs = din("gvecs", [4, D], F32)
    w_fm = din("w_fm", [D, 1152], F32)
    w_tm = din("w_tm", [D, 640], F32)
    w_gab = din("w_gab", [D, 4096], F32)
    lbl = din("lbl", [128, 4, 2], F32)
    hgn = din("hgn", [1, 256], F32)
    sink = din("sink", [1, 2], F32)
    w_ba = din("w_ba", [1024, D], F32)
    w_bb = din("w_bb", [1024, D], F32)
    w_o = din("w_o", [D, D], F32)
    w_r = din("w_r", [D, 16], F32)
    wg = din("wg", [4, D, 1024], F32)
    wu = din("wu", [4, D, 1024], F32)
    wd = din("wd", [4, 1024, D], F32)
    c_identb = din("c_identb", [128, 128], BF16)
    c_identf = din("c_identf", [128, 128], F32)
    c_maskF = din("c_maskF", [128, 128], F32)
    c_maskB = din("c_maskB", [128, 128], F32)
    c_pswap = din("c_pswap", [128, 128], BF16)
    c_small = din("c_small", [128, 4], F32)
    c_band = din("c_band", [128, 3, 384], F32)
    c_rmask = din("c_rmask", [128, 512], F32)
    c_iota = din("c_iota", [128, 512], F32)
    c_tval = din("c_tval", [128, 32, 2], BF16)
    c_rowbase = din("c_rowbase", [128, 16], F32)
    c_idx = din("c_idx", [128, 40], I32)
    out = nc.dram_tensor("out", [1024, D], F32, kind="ExternalOutput").ap()

    src_mod = dscr("src_mod", [1, 3072], F32)
    dst_mod = dscr("dst_mod", [4, 3072], F32)
    src_hT = dscr("src_hT", [4 * KC * 128, 256], BF16)
    dst_hT = dscr("dst_hT", [4 * KC * 128, 1024], BF16)
    hgT = dscr("hgT", [2, 2, 3, 128, S], BF16)
    koutM = dscr("koutM", [2, 2, S, 128], BF16)
    vg = dscr("vg", [S, 512], BF16)
    src_o = dscr("src_o", [S, 512], BF16)
    dst_o = dscr("dst_o", [4 * S, 512], BF16)
    x1_d = dscr("x1_d", [1024, D], F32)
    src_h2 = dscr("src_h2", [1024, D], BF16)
    dst_h2 = dscr("dst_h2", [4096, D], BF16)
    src_aff = dscr("src_aff", [16, 1024], F32)
    dst_aff = dscr("dst_aff", [64, 1024], F32)
    route_d = dscr("route_d", [16, S], F32)
    route_d2 = dscr("route_d2", [64, 1024], F32)
    src_Y = dscr("src_Y", [2048, D], BF16)
    dst_Y = dscr("dst_Y", [8192, D], BF16)
    t_o_ch = [Tok() for _ in range(4)]
    t_h2_ch = [Tok() for _ in range(4)]
    t_Y_ch = [Tok() for _ in range(8)]
    t_dram = {n: Tok() for n in ("src_mod", "dst_mod", "src_hT", "dst_hT", "hgT", "koutM", "vg", "src_o", "dst_o",
                                 "x1_d", "src_h2", "dst_h2", "src_aff", "dst_aff", "route_d", "src_Y", "dst_Y")}

    with ExitStack() as st:
        ARENA_BYTES = 204 * 1024
        arena_t = st.enter_context(nc.sbuf_tensor("arena", [128, ARENA_BYTES], U8))
        A = Arena(arena_t, ARENA_BYTES)
        psb = [st.enter_context(nc.psum_tensor("ps%d" % i, [128, 512], F32)) for i in range(8)]
        pst = [Tok() for _ in range(8)]

        def psf(i):
            return psb[i][:, :]

        def psbf(i):
            return psb[i][:, :].bitcast(BF16)

        OP = P.op

        def stop_here(n):
            if KSTOP == n:
                raise _Stop()

        def dma(eng, out_ap, in_ap, r=(), w=()):
            return OP(eng, lambda e: e.dma_start(out=out_ap, in_=in_ap), reads=r, writes=w, dma=True)

        agc = [0]

        def ag_start(src, rows, ci, tsrc, dst=None):
            ttmp = Tok()
            if dst is not None:
                allgather(src[ci * rows:(ci + 1) * rows, :], dst[ci * 4 * rows:(ci + 1) * 4 * rows, :], G4, tsrc, ttmp)
                return None, ttmp
            agc[0] += 1
            tmp = dscr("agtmp%d" % agc[0], [4 * rows, src.shape[1]], src.dtype)
            allgather(src[ci * rows:(ci + 1) * rows, :], tmp, G4, tsrc, ttmp)
            return tmp, ttmp

        def ag_finish(dst, nch, pend, tdst, only=None):
            dv = dst.rearrange("(r c m) x -> c r m x", r=4, c=nch)
            for ci, (tmp, ttmp) in enumerate(pend):
                if only is not None and ci != only:
                    continue
                dma("sp", dv[ci], tmp.rearrange("(r m) x -> r m x", r=4), r=[ttmp], w=[tdst])

        def ag_chunked(src, dst, rows, nch, tsrc, tdst):
            C = src.shape[1]
            dv = dst.rearrange("(r c m) x -> c r m x", r=4, c=nch)
            for ci in range(nch):
                agc[0] += 1
                tmp = dscr("agtmp%d" % agc[0], [4 * rows, C], src.dtype)
                ttmp = Tok()
                allgather(src[ci * rows:(ci + 1) * rows, :], tmp, G4, tsrc[ci] if isinstance(tsrc, list) else tsrc, ttmp)
                dma("sp", dv[ci], tmp.rearrange("(r m) x -> r m x", r=4), r=[ttmp], w=[tdst])

        def allgather(src, dst, groups, tsrc, tdst):
            return OP("pool", lambda e: e.collective_compute("AllGather", ALU.bypass, replica_groups=groups,
                                                             ins=[src.opt()], outs=[dst.opt()]),
                      reads=[tsrc], writes=[tdst], dma="cc")

        identb, t_identb = A.alloc([128], BF16)
        identf, t_identf = A.alloc([128], F32)
        maskF, t_maskF = A.alloc([128], F32)
        maskB, t_maskB = A.alloc([128], F32)
        pswap, t_pswap = A.alloc([128], BF16)
        small, t_small = A.alloc([4], F32)
        band, t_band = A.alloc([3, 384], F32)
        rmask, t_rmask = A.alloc([512], F32)
        iota, t_iota = A.alloc([512], F32)
        tval, t_tval = A.alloc([32, 2], BF16)
        rowbase, t_rowbase = A.alloc([16], F32)
        cidx, t_cidx = A.alloc([40], I32)
        lbt, t_lbt = A.alloc([4, 2], F32)
        lbv, t_lbv = A.alloc([4, 3], F32)
        hgnb, t_hgnb = A.alloc([256], F32)
        sinkb, t_sinkb = A.alloc([2], F32)
        for (sb_, src_, tk) in ((identb, c_identb, t_identb), (identf, c_identf, t_identf), (maskF, c_maskF, t_maskF),
                                (maskB, c_maskB, t_maskB), (pswap, c_pswap, t_pswap), (small, c_small, t_small),
                                (band, c_band, t_band), (rmask, c_rmask, t_rmask), (iota, c_iota, t_iota),
                                (tval, c_tval, t_tval), (rowbase, c_rowbase, t_rowbase), (lbt, lbl, t_lbt), (cidx, c_idx, t_cidx)):
            dma("sp", sb_, src_, w=[tk])
        dma("sp", hgnb, hgn.partition_broadcast(128), w=[t_hgnb])
        dma("sp", sinkb, sink.partition_broadcast(128), w=[t_sinkb])
        OP("dve", lambda e: e.tensor_tensor(out=lbv[:, :, 0], in0=lbt[:, :, 0], in1=lbt[:, :, 1], op=ALU.subtract),
           reads=[t_lbt], writes=[t_lbv])
        OP("act", lambda e: e.activation(out=lbv[:, :, 0], in_=lbv[:, :, 0], func=AF.Sigmoid), reads=[t_lbv], writes=[t_lbv])
        OP("dve", lambda e: e.tensor_scalar(out=lbv[:, :, 1], in0=lbv[:, :, 0], scalar1=-1.0, scalar2=1.0, op0=ALU.mult, op1=ALU.add),
           reads=[t_lbv], writes=[t_lbv])
        OP("dve", lambda e: e.tensor_scalar(out=lbv[:, :, 2], in0=lbv[:, :, 1], scalar1=-1.0, scalar2=None, op0=ALU.mult),
           reads=[t_lbv], writes=[t_lbv])
        invf = small[:, 0:1]
        sinsign = small[:, 1:2]
        negpis = small[:, 2:3]
        negpi = small[:, 3:4]

        def rms_rstd(eng_unused, ss, tss, rstd, trstd, n):
            OP("dve", lambda e: e.tensor_scalar(out=rstd, in0=ss, scalar1=1.0 / n, scalar2=EPS, op0=ALU.mult, op1=ALU.add),
               reads=[tss], writes=[trstd])
            OP("act", lambda e: e.activation(out=rstd, in_=rstd, func=AF.Sqrt), reads=[trstd], writes=[trstd])
            OP("dve", lambda e: e.reciprocal(out=rstd, in_=rstd), reads=[trstd], writes=[trstd])

        modflat = dst_mod.rearrange("r n -> (r n)").rearrange("(s d) -> s d", d=D)

        try:
            m1 = A.mark()
            cs, t_cs = A.alloc([KC], F32)
            csb, t_csb = A.alloc([KC], BF16)
            wada, t_wada = A.alloc([KC, 3072], BF16)
            bada, t_bada = A.alloc([3072], F32)
            modrow, t_modrow = A.alloc([3072], F32)
            dma("sp", cs, c_b, w=[t_cs])
            dma("sp", bada[0:1, :], b_ada_q, w=[t_bada])
            wsrc = w_ada_q.rearrange("(k p) n -> p k n", p=128)
            for g in range(4):
                dma("pool", wada[:, 4 * g:4 * g + 4, :], wsrc[:, 4 * g:4 * g + 4, :], w=[t_wada])
            OP("act", lambda e: e.activation(out=csb, in_=cs, func=AF.Silu), reads=[t_cs], writes=[t_csb])
            for g in range(6):
                bk = g % 4
                for kc in range(KC):
                    OP("pe", lambda e, g=g, kc=kc, bk=bk: e.matmul(psf(bk)[0:1, :], lhsT=csb[:, kc:kc + 1],
                                                                   rhs=wada[:, kc, g * 512:(g + 1) * 512],
                                                                   start=(kc == 0), stop=(kc == KC - 1)),
                       reads=[t_csb, t_wada], writes=[pst[bk]])
                OP("dve", lambda e, g=g, bk=bk: e.tensor_tensor(out=modrow[0:1, g * 512:(g + 1) * 512], in0=psf(bk)[0:1, :],
                                                                in1=bada[0:1, g * 512:(g + 1) * 512], op=ALU.add),
                   reads=[pst[bk], t_bada], writes=[t_modrow])
            dma("sp", src_mod, modrow[0:1, :], r=[t_modrow], w=[t_dram["src_mod"]])
            allgather(src_mod, dst_mod, G4, t_dram["src_mod"], t_dram["dst_mod"])
            A.release(m1)

            def load_modvec(dst_ap, tok, sidx, gidx, mode):
                if mode == "b":
                    dma("sp", dst_ap, modflat[sidx:sidx + 1, :].partition_broadcast(128), r=[t_dram["dst_mod"]], w=[tok])
                    return
                mk = A.mark()
                tmp, t_tmp = A.alloc([D], F32)
                dma("sp", dst_ap, modflat[sidx:sidx + 1, :].partition_broadcast(128), r=[t_dram["dst_mod"]], w=[tok])
                dma("sp", tmp, gvecs[gidx:gidx + 1, :].partition_broadcast(128), w=[t_tmp])
                if mode == "a":
                    OP("dve", lambda e: e.scalar_tensor_tensor(out=dst_ap, in0=dst_ap, scalar=1.0, in1=tmp, op0=ALU.add, op1=ALU.mult),
                       reads=[tok, t_tmp], writes=[tok])
                else:
                    OP("dve", lambda e: e.tensor_tensor(out=dst_ap, in0=dst_ap, in1=tmp, op=ALU.mult), reads=[tok, t_tmp], writes=[tok])
                A.release(mk)

            stop_here(1)
            m2 = A.mark()
            hTown, t_hTown = A.alloc([KC, 1024], BF16)
            A1, t_A1 = A.alloc([D], F32)
            B1, t_B1 = A.alloc([D], F32)
            load_modvec(A1, t_A1, 1, 0, "a")
            load_modvec(B1, t_B1, 0, 0, "b")
            xts = [A.alloc([D], F32) for _ in range(2)]
            junk, t_junk = A.alloc([D], BF16)
            hfs = [A.alloc([D], F32) for _ in range(2)]
            hbs = [A.alloc([D], BF16) for _ in range(2)]
            ss8, t_ss8 = A.alloc([8], F32)
            rs8, t_rs8 = A.alloc([8], F32)
            hT_tmp = []
            for j in range(8):
                xt, t_xt = xts[j % 2]
                hb, t_hb = hbs[j % 2]
                hf, t_hf = hfs[j % 2]
                dma("sp", xt, x_own[j * 128:(j + 1) * 128, :], w=[t_xt])
                OP("act", lambda e, xt=xt, j=j: e.activation(out=junk, in_=xt, func=AF.Square, accum_out=ss8[:, j:j + 1]),
                   reads=[t_xt], writes=[t_junk, t_ss8])
                rms_rstd(None, ss8[:, j:j + 1], t_ss8, rs8[:, j:j + 1], t_rs8, D)
                OP("dve", lambda e, xt=xt, j=j: e.scalar_tensor_tensor(out=hf, in0=xt, scalar=rs8[:, j:j + 1], in1=A1,
                                                                       op0=ALU.mult, op1=ALU.mult),
                   reads=[t_xt, t_rs8, t_A1], writes=[t_hf])
                OP("pool", lambda e, hb=hb: e.tensor_tensor(out=hb, in0=hf, in1=B1, op=ALU.add), reads=[t_hf, t_B1], writes=[t_hb])
                for half in range(2):
                    bk = 6 + half
                    for k8 in range(8):
                        kc = half * 8 + k8
                        OP("pe", lambda e, hb=hb, kc=kc, k8=k8, bk=bk: e.transpose(out=psbf(bk)[:, k8 * 128:(k8 + 1) * 128],
                                                                                   in_=hb[:, kc * 128:(kc + 1) * 128], identity=identb),
                           reads=[t_hb, t_identb], writes=[pst[bk]])
                    OP("act", lambda e, half=half, bk=bk, j=j: e.copy(out=hTown[:, half * 8:half * 8 + 8, j * 128:(j + 1) * 128],
                                                                      in_=psbf(bk).rearrange("p (a b) -> p a b", b=128)),
                       reads=[pst[bk]], writes=[t_hTown])
                if j % 2 == 1:
                    ci = j // 2
                    tch = Tok()
                    dma("sp", src_hT[ci * 2048:(ci + 1) * 2048, :].rearrange("(k p) t -> p k t", p=128), hTown[:, :, ci * 256:(ci + 1) * 256],
                        r=[t_hTown], w=[tch, t_dram["src_hT"]])
                    hT_tmp.append(ag_start(src_hT, 2048, ci, tch))

            A.release(m2)

            stop_here(2)
            m3 = A.mark()
            qTa = [A.alloc([S], BF16) for _ in range(2)]
            kTa, t_kTa = A.alloc([S + 256], BF16)
            vat, t_vat = A.alloc([34, 128], BF16)
            OP("pool", lambda e: e.memset(kTa[:, 0:128], 0.0), writes=[t_kTa])
            OP("pool", lambda e: e.memset(kTa[:, S + 128:S + 256], 0.0), writes=[t_kTa])
            OP("pool", lambda e: e.memset(vat[:, 0, :], 0.0), writes=[t_vat])
            OP("pool", lambda e: e.memset(vat[:, 33, :], 0.0), writes=[t_vat])
            dec = [[A.alloc([64], F32) for _ in range(2)] for _ in range(2)]
            m3b = A.mark()
            Wfm, t_Wfm = A.alloc([KC, 1152], BF16)
            Wtm, t_Wtm = A.alloc([KC, 640], BF16)
            wfs = w_fm.rearrange("(k p) n -> p k n", p=128)
            wts = w_tm.rearrange("(k p) n -> p k n", p=128)
            for g in range(2):
                dma("pool", Wfm[:, 8 * g:8 * g + 8, :], wfs[:, 8 * g:8 * g + 8, :], w=[t_Wfm])
            dma("pool", Wtm, wts, w=[t_Wtm])
            hblks = [A.alloc([KC, 512], BF16) for _ in range(2)]
            qf = [A.alloc([512], F32) for _ in range(2)]
            scr0 = [A.alloc([512], F32) for _ in range(11)]
            scr1a = [A.alloc([512], F32) for _ in range(4)]
            scr1b = [A.alloc([512], F32) for _ in range(2)]
            scr = [scr0, scr1a + scr0[4:7] + scr1b + scr0[9:]]
            prods = [A.alloc([3, 512], BF16) for _ in range(2)]
            koutf = [A.alloc([512], BF16) for _ in range(2)]
            koutm = [A.alloc([4, 128], BF16) for _ in range(2)]
            tmo = [A.alloc([512], BF16) for _ in range(2)]
            posi, t_posi = A.alloc([512], I32)
            posf, t_posf = A.alloc([512], F32)
            ang, t_ang = A.alloc([512], F32)
            cosT, t_cosT = A.alloc([512], F32)
            sinT, t_sinT = A.alloc([512], F32)
            qbs = [A.alloc([512], BF16) for _ in range(3)]
            rt1, t_rt1 = A.alloc([512], F32)
            rt2, t_rt2 = A.alloc([512], F32)
            hsrcs = [tmp.rearrange("(r k p) t -> r p k t", r=4, k=KC) for (tmp, _) in hT_tmp]
            v3 = lambda ap: ap.rearrange("p (c l) -> p c l", l=64)
            pcount = [0]
            order3 = [0, 2, 4, 6, 1, 3, 5, 7]

            def load_hblk(it3):
                tb_ = order3[it3]
                r_, jb_ = tb_ // 2, tb_ % 2
                hb_, t_hb_ = hblks[it3 % 2]
                for h_ in range(2):
                    dma("sp", hb_[:, :, h_ * 256:(h_ + 1) * 256], hsrcs[2 * jb_ + h_][r_], r=[hT_tmp[2 * jb_ + h_][1]], w=[t_hb_])
            load_hblk(0)
            for it3, tb in enumerate(order3):
                r, jb = tb // 2, tb % 2
                hblk, t_hblk = hblks[it3 % 2]
                if it3 + 1 < 8:
                    load_hblk(it3 + 1)
                tsl = slice(tb * 512, (tb + 1) * 512)
                dma("sp", posi, pos_b[0:1, tsl].partition_broadcast(128), w=[t_posi])
                OP("dve", lambda e: e.tensor_copy(out=posf, in_=posi), reads=[t_posi], writes=[t_posf])
                OP("dve", lambda e: e.tensor_scalar(out=ang, in0=posf, scalar1=invf, scalar2=None, op0=ALU.mult),
                   reads=[t_posf, t_small], writes=[t_ang])
                C1 = 6.28125
                C2 = 2 * math.pi - 6.28125
                OP("dve", lambda e: e.tensor_scalar(out=rt1, in0=ang, scalar1=1.0 / (2 * math.pi), scalar2=None, op0=ALU.mult), reads=[t_ang], writes=[t_rt1])
                OP("dve", lambda e: e.tensor_copy(out=posi, in_=rt1), reads=[t_rt1], writes=[t_posi])
                OP("dve", lambda e: e.tensor_copy(out=rt1, in_=posi), reads=[t_posi], writes=[t_rt1])
                OP("dve", lambda e: e.scalar_tensor_tensor(out=ang, in0=rt1, scalar=-C1, in1=ang, op0=ALU.mult, op1=ALU.add), reads=[t_rt1, t_ang], writes=[t_ang])
                OP("dve", lambda e: e.scalar_tensor_tensor(out=ang, in0=rt1, scalar=-C2, in1=ang, op0=ALU.mult, op1=ALU.add), reads=[t_rt1, t_ang], writes=[t_ang])

                def wrap(buf, tbuf):
                    OP("dve", lambda e: e.tensor_scalar(out=rt2, in0=buf, scalar1=math.pi, scalar2=-2 * math.pi, op0=ALU.is_gt, op1=ALU.mult), reads=[tbuf], writes=[t_rt2])
                    OP("dve", lambda e: e.tensor_tensor(out=buf, in0=buf, in1=rt2, op=ALU.add), reads=[tbuf, t_rt2], writes=[tbuf])
                    OP("dve", lambda e: e.tensor_scalar(out=rt2, in0=buf, scalar1=-math.pi, scalar2=2 * math.pi, op0=ALU.is_lt, op1=ALU.mult), reads=[tbuf], writes=[t_rt2])
                    OP("dve", lambda e: e.tensor_tensor(out=buf, in0=buf, in1=rt2, op=ALU.add), reads=[tbuf, t_rt2], writes=[tbuf])
                wrap(ang, t_ang)
                OP("act", lambda e: e.activation(out=sinT, in_=ang, func=AF.Sin, scale=sinsign), reads=[t_ang, t_small], writes=[t_sinT])
                OP("dve", lambda e: e.tensor_scalar(out=rt1, in0=ang, scalar1=0.5 * math.pi, scalar2=None, op0=ALU.add), reads=[t_ang], writes=[t_rt1])
                wrap(rt1, t_rt1)
                OP("act", lambda e: e.activation(out=cosT, in_=rt1, func=AF.Sin), reads=[t_rt1], writes=[t_cosT])

                def fm_matmul(cb, bk):
                    for kc in range(KC):
                        OP("pe", lambda e, kc=kc: e.matmul(psf(bk), lhsT=Wfm[:, kc, cb * 128:(cb + 1) * 128], rhs=hblk[:, kc, :],
                                                           start=(kc == 0), stop=(kc == KC - 1)),
                           reads=[t_Wfm, t_hblk], writes=[pst[bk]])

                for hh in range(2):
                    bk = pcount[0] % 4
                    pcount[0] += 1
                    fm_matmul(hh, bk)
                    OP("act", lambda e, hh=hh, bk=bk: e.activation(out=qf[hh][0], in_=psf(bk), func=AF.Silu),
                       reads=[pst[bk]], writes=[qf[hh][1]])
                bkmap = {}

                def stageA(dr, hh):
                    cb = 2 + dr * 2 + hh
                    li = dr * 2 + hh
                    bk = pcount[0] % 4
                    pcount[0] += 1
                    bkmap[(dr, hh)] = bk
                    fm_matmul(cb, bk)
                    sig, t_sig = scr[li % 2][0]
                    OP("act", lambda e, bk=bk: e.activation(out=sig, in_=psf(bk), func=AF.Sigmoid), reads=[pst[bk]], writes=[t_sig])

                def stageB(dr, hh):
                    li = dr * 2 + hh
                    bk = bkmap[(dr, hh)]
                    ((sig, t_sig), (logf, t_logf), (kk, t_kk), (bb, t_bb), (bx, t_bx), (dd, t_dd), (d2, t_d2),
                     (E1, t_E1), (E2, t_E2), (E3, t_E3), (E4, t_E4)) = scr[li % 2]
                    q_ap, t_q = qf[hh]
                    prod, t_prod = prods[(dr * 2 + hh) % 2]
                    kof, t_kof = koutf[(dr * 2 + hh) % 2]
                    kom, t_kom = koutm[(dr * 2 + hh) % 2]
                    dec_ap, t_dec = dec[hh][dr]
                    OP("act", lambda e, li=li: e.activation(out=logf, in_=sig, func=AF.Ln, bias=lbv[:, li, 0:1], scale=lbv[:, li, 1:2]),
                       reads=[t_sig, t_lbv], writes=[t_logf])
                    OP("dve", lambda e, li=li: e.tensor_scalar(out=kk, in0=sig, scalar1=lbv[:, li, 2:3], scalar2=lbv[:, li, 1:2],
                                                               op0=ALU.mult, op1=ALU.add),
                       reads=[t_sig, t_lbv], writes=[t_kk])
                    OP("dve", lambda e: e.tensor_tensor_scan(out=bb, data0=rmask, data1=logf, initial=0.0, op0=ALU.mult, op1=ALU.add),
                       reads=[t_rmask, t_logf], writes=[t_bb])
                    OP("act", lambda e, dec_ap=dec_ap, tb=tb: e.activation(out=dec_ap[:, tb * 8:(tb + 1) * 8], in_=v3(bb)[:, :, 63], func=AF.Exp),
                       reads=[t_bb], writes=[t_dec])
                    if dr == 0:
                        OP("dve", lambda e: e.tensor_tensor(out=v3(dd), in0=v3(bb), in1=v3(bb)[:, :, 32:33].to_broadcast([128, 8, 64]), op=ALU.subtract),
                           reads=[t_bb], writes=[t_dd])
                        OP("dve", lambda e: e.tensor_tensor(out=v3(d2), in0=v3(bb), in1=v3(bb)[:, :, 63:64].to_broadcast([128, 8, 64]), op=ALU.subtract),
                           reads=[t_bb], writes=[t_d2])
                        OP("act", lambda e: e.activation(out=E1, in_=dd, func=AF.Exp), reads=[t_dd], writes=[t_E1])
                        OP("act", lambda e: e.activation(out=E2, in_=dd, func=AF.Exp, scale=-1.0), reads=[t_dd], writes=[t_E2])
                        OP("act", lambda e: e.activation(out=E3, in_=bb, func=AF.Exp), reads=[t_bb], writes=[t_E3])
                        OP("act", lambda e: e.activation(out=E4, in_=d2, func=AF.Exp, scale=-1.0), reads=[t_d2], writes=[t_E4])
                    else:
                        OP("dve", lambda e: e.tensor_tensor(out=bx, in0=bb, in1=logf, op=ALU.subtract), reads=[t_bb, t_logf], writes=[t_bx])
                        OP("dve", lambda e: e.tensor_tensor(out=v3(dd), in0=v3(bx), in1=v3(bx)[:, :, 32:33].to_broadcast([128, 8, 64]), op=ALU.subtract),
                           reads=[t_bx], writes=[t_dd])
                        OP("dve", lambda e: e.tensor_tensor(out=v3(d2), in0=v3(bx), in1=v3(bb)[:, :, 63:64].to_broadcast([128, 8, 64]), op=ALU.subtract),
                           reads=[t_bx, t_bb], writes=[t_d2])
                        OP("act", lambda e: e.activation(out=E1, in_=dd, func=AF.Exp, scale=-1.0), reads=[t_dd], writes=[t_E1])
                        OP("act", lambda e: e.activation(out=E2, in_=dd, func=AF.Exp), reads=[t_dd], writes=[t_E2])
                        OP("act", lambda e: e.activation(out=E3, in_=d2, func=AF.Exp, scale=-1.0), reads=[t_d2], writes=[t_E3])
                        OP("act", lambda e: e.activation(out=E4, in_=bx, func=AF.Exp), reads=[t_bx], writes=[t_E4])
                    OP("pool", lambda e, prod=prod, q_ap=q_ap: e.tensor_tensor(out=prod[:, 0, :], in0=q_ap, in1=E1, op=ALU.mult),
                       reads=[t_q, t_E1], writes=[t_prod])
                    OP("pool", lambda e, prod=prod: e.tensor_tensor(out=prod[:, 1, :], in0=kk, in1=E2, op=ALU.mult),
                       reads=[t_kk, t_E2], writes=[t_prod])
                    OP("dve", lambda e, prod=prod, q_ap=q_ap: e.tensor_tensor(out=prod[:, 2, :], in0=q_ap, in1=E3, op=ALU.mult),
                       reads=[t_q, t_E3], writes=[t_prod])
                    OP("pool", lambda e, kof=kof: e.tensor_tensor(out=kof, in0=kk, in1=E4, op=ALU.mult),
                       reads=[t_kk, t_E4], writes=[t_kof])
                    dma("sp", hgT[hh, dr].rearrange("a p t -> p a t")[:, :, tsl], prod, r=[t_prod], w=[t_dram["hgT"]])

                    def stageC(kof=kof, t_kof=t_kof, kom=kom, t_kom=t_kom, hh=hh, dr=dr):
                        for i in range(4):
                            OP("pe", lambda e, i=i, kof=kof: e.transpose(out=psbf(6)[:, i * 128:(i + 1) * 128], in_=kof[:, i * 128:(i + 1) * 128], identity=identb),
                               reads=[t_kof, t_identb], writes=[pst[6]])
                        OP("act", lambda e, kom=kom: e.copy(out=kom, in_=psbf(6)[:, 0:512].rearrange("p (a b) -> p a b", b=128)),
                           reads=[pst[6]], writes=[t_kom])
                        dma("sp", koutM[hh, dr, tsl, :].rearrange("(i p) d -> p i d", p=128), kom, r=[t_kom], w=[t_dram["koutM"]])
                    return stageC

                items3 = [(0, 0), (0, 1), (1, 0), (1, 1)]
                stageA(*items3[0])
                pendC = []
                for n3, it_ in enumerate(items3):
                    if n3 + 1 < 4:
                        stageA(*items3[n3 + 1])
                    if pendC:
                        pendC.pop(0)()
                    pendC.append(stageB(*it_))
                for ci in range(3):
                    cb = 6 + ci
                    bk = pcount[0] % 4
                    pcount[0] += 1
                    fm_matmul(cb, bk)
                    qb, t_qb = qbs[ci]
                    OP("act", lambda e, bk=bk, qb=qb: e.copy(out=qb, in_=psf(bk)), reads=[pst[bk]], writes=[t_qb])
                while pendC:
                    pendC.pop(0)()

                def ropeB(ci):
                    qb, t_qb = qbs[ci]
                    OP("pe", lambda e: e.matmul(psf(7), lhsT=pswap, rhs=qb, start=True, stop=True), reads=[t_pswap, t_qb], writes=[pst[7]])
                    OP("pool", lambda e: e.tensor_tensor(out=rt1, in0=qb, in1=cosT, op=ALU.mult), reads=[t_qb, t_cosT], writes=[t_rt1])
                    OP("dve", lambda e: e.tensor_tensor(out=rt2, in0=psf(7), in1=sinT, op=ALU.mult), reads=[pst[7], t_sinT], writes=[t_rt2])
                    if ci < 2:
                        dst_ap, t_dst = qTa[ci][0][:, tsl], qTa[ci][1]
                    else:
                        dst_ap, t_dst = kTa[:, 128 + tb * 512:128 + (tb + 1) * 512], t_kTa
                    OP("pool", lambda e, dst_ap=dst_ap: e.tensor_tensor(out=dst_ap, in0=rt1, in1=rt2, op=ALU.add),
                       reads=[t_rt1, t_rt2], writes=[t_dst])
                for i in range(4):
                    tmo_ap, t_tmo = tmo[i % 2]
                    gt = tb * 4 + i
                    for kc in range(KC):
                        OP("pe", lambda e, kc=kc, i=i: e.matmul(psf(4), lhsT=hblk[:, kc, i * 128:(i + 1) * 128], rhs=Wtm[:, kc, 0:512],
                                                                start=(kc == 0), stop=(kc == KC - 1)),
                           reads=[t_hblk, t_Wtm], writes=[pst[4]])
                    for kc in range(KC):
                        OP("pe", lambda e, kc=kc, i=i: e.matmul(psf(5)[:, 0:128], lhsT=hblk[:, kc, i * 128:(i + 1) * 128], rhs=Wtm[:, kc, 512:640],
                                                                start=(kc == 0), stop=(kc == KC - 1)),
                           reads=[t_hblk, t_Wtm], writes=[pst[5]])
                    OP("act", lambda e, tmo_ap=tmo_ap: e.copy(out=tmo_ap[:, 0:256], in_=psf(4)[:, 0:256]), reads=[pst[4]], writes=[t_tmo])
                    OP("act", lambda e, tmo_ap=tmo_ap: e.activation(out=tmo_ap[:, 256:512], in_=psf(4)[:, 256:512], func=AF.Silu),
                       reads=[pst[4]], writes=[t_tmo])
                    OP("dve", lambda e, gt=gt: e.tensor_copy(out=vat[:, gt + 1, :], in_=psf(5)[:, 0:128]), reads=[pst[5]], writes=[t_vat])
                    dma("sp", vg[gt * 128:(gt + 1) * 128, :], tmo_ap, r=[t_tmo], w=[t_dram["vg"]])
                    if i < 3:
                        ropeB(i)
            A.release(m3b)

            stop_here(3)
            m4 = A.mark()
            Ssb = [A.alloc([386], F32) for _ in range(4)]
            Pb = [A.alloc([386], BF16) for _ in range(4)]
            for hh_ in range(4):
                OP("pool", lambda e, hh_=hh_: e.tensor_copy(out=Ssb[hh_][0][:, 384:385], in_=sinkb[:, hh_ % 2:hh_ % 2 + 1]), reads=[t_sinkb], writes=[Ssb[hh_][1]])
            PTs = [A.alloc([384], BF16) for _ in range(2)]
            st4 = [A.alloc([8], F32) for _ in range(4)]
            obt = [A.alloc([256], BF16) for _ in range(2)]
            scale = 128 ** -0.5
            it4 = 0
            pend4 = []

            def att_stageA(i, hh, it4):
                var = 0 if i == 0 else (2 if i == 31 else 1)
                s_ap, t_s = Ssb[it4 % 4]
                p_ap, t_p = Pb[it4 % 4]
                sc4, t_sc4 = st4[it4 % 4]
                bS = it4 % 4
                q_ap, t_q = qTa[hh]
                OP("pe", lambda e, q_ap=q_ap, i=i, bS=bS: e.matmul(psf(bS)[:, 0:384], lhsT=q_ap[:, i * 128:(i + 1) * 128],
                                                                   rhs=kTa[:, i * 128:i * 128 + 384], start=True, stop=True),
                   reads=[t_q, t_kTa], writes=[pst[bS]])
                OP("dve", lambda e, s_ap=s_ap, bS=bS, var=var: e.scalar_tensor_tensor(out=s_ap[:, 0:384], in0=psf(bS)[:, 0:384], scalar=scale,
                                                                                      in1=band[:, var, :], op0=ALU.mult, op1=ALU.add),
                   reads=[pst[bS], t_band], writes=[t_s])
                OP("dve", lambda e, s_ap=s_ap, sc4=sc4: e.tensor_reduce(out=sc4[:, 0:1], in_=s_ap[:, 0:385], axis=AX.X, op=ALU.max),
                   reads=[t_s], writes=[t_sc4])
                OP("dve", lambda e, sc4=sc4: e.tensor_scalar(out=sc4[:, 2:3], in0=sc4[:, 0:1], scalar1=-1.0, scalar2=None, op0=ALU.mult),
                   reads=[t_sc4], writes=[t_sc4])
                OP("act", lambda e, s_ap=s_ap, p_ap=p_ap, sc4=sc4: e.activation(out=p_ap[:, 0:385], in_=s_ap[:, 0:385], func=AF.Exp, bias=sc4[:, 2:3], scale=1.0,
                                                                                 accum_out=sc4[:, 3:4]),
                   reads=[t_s, t_sc4], writes=[t_p, t_sc4])
                OP("dve", lambda e, sc4=sc4: e.reciprocal(out=sc4[:, 6:7], in_=sc4[:, 3:4]), reads=[t_sc4], writes=[t_sc4])

            def att_stageB(i, hh, it4):
                ob_ap, t_ob = obt[i % 2]
                p_ap, t_p = Pb[it4 % 4]
                pt_ap, t_pt = PTs[it4 % 2]
                sc4, t_sc4 = st4[it4 % 4]
                bT = 4 + it4 % 2
                bO = 6 + it4 % 2
                for kb in range(3):
                    OP("pe", lambda e, kb=kb, p_ap=p_ap, bT=bT: e.transpose(out=psbf(bT)[:, kb * 128:(kb + 1) * 128],
                                                                           in_=p_ap[:, kb * 128:(kb + 1) * 128], identity=identb),
                       reads=[t_p, t_identb], writes=[pst[bT]])
                OP("act", lambda e, pt_ap=pt_ap, bT=bT: e.copy(out=pt_ap, in_=psbf(bT)[:, 0:384]), reads=[pst[bT]], writes=[t_pt])
                for kb in range(3):
                    OP("pe", lambda e, kb=kb, pt_ap=pt_ap, bO=bO, i=i: e.matmul(psf(bO)[:, 0:128], lhsT=pt_ap[:, kb * 128:(kb + 1) * 128],
                                                                                rhs=vat[:, i + kb, :], start=(kb == 0), stop=(kb == 2)),
                       reads=[t_pt, t_vat], writes=[pst[bO]])
                OP("dve", lambda e, ob_ap=ob_ap, hh=hh, bO=bO, sc4=sc4: e.tensor_scalar(out=ob_ap[:, hh * 128:(hh + 1) * 128], in0=psf(bO)[:, 0:128],
                                                                                        scalar1=sc4[:, 6:7], scalar2=None, op0=ALU.mult),
                   reads=[pst[bO], t_sc4], writes=[t_ob])
                if hh == 1:
                    dma("sp", src_o[i * 128:(i + 1) * 128, 256:512], ob_ap, r=[t_ob], w=[t_o_ch[i // 8]])

            for i in range(32):
                for hh in range(2):
                    att_stageA(i, hh, it4)
                    while len(pend4) > 1:
                        pend4.pop(0)()
                    pend4.append(lambda i=i, hh=hh, it4=it4: att_stageB(i, hh, it4))
                    it4 += 1
            while pend4:
                pend4.pop(0)()
            if DEBUG:
                dq = nc.dram_tensor("dbg_qT", [2, 128, S], BF16, kind="ExternalOutput").ap()
                dk = nc.dram_tensor("dbg_kT", [128, S + 256], BF16, kind="ExternalOutput").ap()
                dv = nc.dram_tensor("dbg_vat", [128, 34, 128], BF16, kind="ExternalOutput").ap()
                P.final_waits.append(dma("sp", dq[0], qTa[0][0], r=[qTa[0][1]]))
                P.final_waits.append(dma("sp", dq[1], qTa[1][0], r=[qTa[1][1]]))
                P.final_waits.append(dma("sp", dk, kTa, r=[t_kTa]))
                P.final_waits.append(dma("sp", dv, vat, r=[t_vat]))
            A.release(m4)
            A.release(m3)
            A.top = m3b

            stop_here(4)
            m5 = A.mark()
            Sall = [A.alloc([64, 128], BF16) for _ in range(2)]
            Sst = [A.alloc([128], F32) for _ in range(2)]
            kbl = [[A.alloc([8, 128], BF16) for _ in range(2)] for _ in range(2)]
            vbl = [[A.alloc([8, 128], BF16) for _ in range(2)] for _ in range(2)]
            pbl = [[A.alloc([3, 512], BF16) for _ in range(2)] for _ in range(2)]
            vgb = [A.alloc([8, 512], BF16) for _ in range(2)]
            ATs = [[A.alloc([64], BF16) for _ in range(2)] for _ in range(2)]
            ss5s = [A.alloc([4], F32) for _ in range(2)]
            tmp5s = [A.alloc([128], F32) for _ in range(2)]
            junk5s = [A.alloc([128], BF16) for _ in range(2)]
            og5 = [A.alloc([2, 128], BF16) for _ in range(2)]
            junk5, t_junk5 = A.alloc([128], BF16)
            H = slice(0, 64)
            pend_o = []
            for hh in range(2):
                for dr in range(2):
                    OP("pool", lambda e, dr=dr: e.memset(Sst[dr][0], 0.0), writes=[Sst[dr][1]])
                def load_p1(step, hh=hh):
                    for dr in range(2):
                        tb = step if dr == 0 else 7 - step
                        k_ap, t_k = kbl[dr][step % 2]
                        v_ap, t_v = vbl[dr][step % 2]
                        dma("sp", k_ap[H], koutM[hh, dr, tb * 512:(tb + 1) * 512, :].rearrange("(n p) d -> p n d", p=64),
                            r=[t_dram["koutM"]], w=[t_k])
                        dma("sp", v_ap[H], vg[tb * 512:(tb + 1) * 512, hh * 128:(hh + 1) * 128].rearrange("(n p) d -> p n d", p=64),
                            r=[t_dram["vg"]], w=[t_v])
                load_p1(0)
                for step in range(8):
                    if step + 1 < 8:
                        load_p1(step + 1)
                    for cstep in range(8):
                        for dr in range(2):
                            tb = step if dr == 0 else 7 - step
                            cc = cstep if dr == 0 else 7 - cstep
                            n = tb * 8 + cc
                            k_ap, t_k = kbl[dr][step % 2]
                            v_ap, t_v = vbl[dr][step % 2]
                            S_ap, t_S = Sst[dr]
                            Sa_ap, t_Sa = Sall[dr]
                            dec_ap, t_dec = dec[hh][dr]
                            bk = dr * 2 + (cstep % 2)
                            OP("act", lambda e, Sa_ap=Sa_ap, S_ap=S_ap, n=n: e.copy(out=Sa_ap[:, n, :], in_=S_ap), reads=[t_S], writes=[t_Sa])
                            OP("pe", lambda e, k_ap=k_ap, v_ap=v_ap, cc=cc, bk=bk: e.matmul(psf(bk)[:, 0:128], lhsT=k_ap[H, cc, :],
                                                                                             rhs=v_ap[H, cc, :], start=True, stop=True),
                               reads=[t_k, t_v], writes=[pst[bk]])
                            OP("dve", lambda e, S_ap=S_ap, dec_ap=dec_ap, n=n, bk=bk: e.scalar_tensor_tensor(out=S_ap, in0=S_ap, scalar=dec_ap[:, n:n + 1],
                                                                                                             in1=psf(bk)[:, 0:128], op0=ALU.mult, op1=ALU.add),
                               reads=[t_S, t_dec, pst[bk]], writes=[t_S])
                def load_p2(tb, hh=hh):
                    pf_ap, t_pf = pbl[0][tb % 2]
                    pb_ap, t_pb = pbl[1][tb % 2]
                    vg_ap, t_vgb = vgb[tb % 2]
                    tsl = slice(tb * 512, (tb + 1) * 512)
                    dma("sp", pf_ap, hgT[hh, 0].rearrange("a p t -> p a t")[:, :, tsl], r=[t_dram["hgT"]], w=[t_pf])
                    dma("sp", pb_ap, hgT[hh, 1].rearrange("a p t -> p a t")[:, :, tsl], r=[t_dram["hgT"]], w=[t_pb])
                    dma("sp", vg_ap[H], vg[tsl, :].rearrange("(n p) d -> p n d", p=64), r=[t_dram["vg"]], w=[t_vgb])
                load_p2(0)
                for tb in range(8):
                    if tb + 1 < 8:
                        load_p2(tb + 1)
                    pf_ap, t_pf = pbl[0][tb % 2]
                    pb_ap, t_pb = pbl[1][tb % 2]
                    vg_ap, t_vgb = vgb[tb % 2]
                    tsl = slice(tb * 512, (tb + 1) * 512)
                    for i in range(4):
                        gt = tb * 4 + i
                        og_ap, t_og = og5[i % 2]
                        bO = 4 + (i % 2)
                        for c in range(2):
                            cl = i * 2 + c
                            n = tb * 8 + cl
                            cc_ = slice(cl * 64, (cl + 1) * 64)
                            atf, t_atf = ATs[0][c]
                            atb, t_atb = ATs[1][c]
                            ss5, t_ss5 = ss5s[c]
                            tmp5, t_tmp5 = tmp5s[c]
                            junk5, t_junk5 = junk5s[c]
                            bA = c * 2
                            oc = psf(bO)[H, c * 128:(c + 1) * 128]
                            OP("pe", lambda e, pf_ap=pf_ap, cc_=cc_, bA=bA: e.matmul(psf(bA)[H, 0:64], lhsT=pf_ap[:, 1, cc_], rhs=pf_ap[:, 0, cc_], start=True, stop=True),
                               reads=[t_pf], writes=[pst[bA]])
                            OP("dve", lambda e, atf=atf, bA=bA: e.tensor_tensor(out=atf[H], in0=psf(bA)[H, 0:64], in1=maskF[H, 0:64], op=ALU.mult),
                               reads=[pst[bA], t_maskF], writes=[t_atf])
                            OP("pe", lambda e, pb_ap=pb_ap, cc_=cc_, bA=bA: e.matmul(psf(bA + 1)[H, 0:64], lhsT=pb_ap[:, 1, cc_], rhs=pb_ap[:, 0, cc_], start=True, stop=True),
                               reads=[t_pb], writes=[pst[bA + 1]])
                            OP("dve", lambda e, atb=atb, bA=bA: e.tensor_tensor(out=atb[H], in0=psf(bA + 1)[H, 0:64], in1=maskB[H, 0:64], op=ALU.mult),
                               reads=[pst[bA + 1], t_maskB], writes=[t_atb])
                            vv = vg_ap[H, cl, hh * 128:(hh + 1) * 128]
                            OP("pe", lambda e, atf=atf, vv=vv, oc=oc: e.matmul(oc, lhsT=atf[H], rhs=vv, start=True, stop=False),
                               reads=[t_atf, t_vgb], writes=[pst[bO]])
                            OP("pe", lambda e, atb=atb, vv=vv, oc=oc: e.matmul(oc, lhsT=atb[H], rhs=vv, start=False, stop=False),
                               reads=[t_atb, t_vgb], writes=[pst[bO]])
                            OP("pe", lambda e, pf_ap=pf_ap, cc_=cc_, n=n, oc=oc: e.matmul(oc, lhsT=pf_ap[:, 2, cc_], rhs=Sall[0][0][:, n, :], start=False, stop=False),
                               reads=[t_pf, Sall[0][1]], writes=[pst[bO]])
                            OP("pe", lambda e, pb_ap=pb_ap, cc_=cc_, n=n, oc=oc: e.matmul(oc, lhsT=pb_ap[:, 2, cc_], rhs=Sall[1][0][:, n, :], start=False, stop=True),
                               reads=[t_pb, Sall[1][1]], writes=[pst[bO]])
                            OP("act", lambda e, oc=oc, c=c: e.activation(out=junk5[H], in_=oc, func=AF.Square, accum_out=ss5[H, c:c + 1]),
                               reads=[pst[bO]], writes=[t_junk5, t_ss5])
                            rms_rstd(None, ss5[H, c:c + 1], t_ss5, ss5[H, 2 + c:3 + c], t_ss5, 128)
                            OP("dve", lambda e, oc=oc, hh=hh, c=c: e.scalar_tensor_tensor(out=tmp5[H], in0=oc, scalar=ss5[H, 2 + c:3 + c],
                                                                                          in1=hgnb[H, hh * 128:(hh + 1) * 128], op0=ALU.mult, op1=ALU.mult),
                               reads=[pst[bO], t_ss5, t_hgnb], writes=[t_tmp5])
                            OP("pool", lambda e, og_ap=og_ap, vg_ap=vg_ap, cl=cl, hh=hh, c=c: e.tensor_tensor(out=og_ap[H, c, :], in0=tmp5[H],
                                                                                                             in1=vg_ap[H, cl, 256 + hh * 128:256 + (hh + 1) * 128], op=ALU.mult),
                               reads=[t_tmp5, t_vgb], writes=[t_og])
                        dma("sp", src_o[gt * 128:(gt + 1) * 128, hh * 128:(hh + 1) * 128].rearrange("(c p) d -> p c d", p=64), og_ap[H],
                            r=[t_og], w=[t_o_ch[gt // 8]])
                    if hh == 1 and tb % 2 == 1:
                        pend_o.append(ag_start(src_o, 1024, tb // 2, t_o_ch[tb // 2], dst=dst_o))
            A.release(m5)
            A.top = m3
            t_dsto = [t for (_, t) in pend_o]

            stop_here(5)
            m7 = A.mark()
            affown, t_affown = A.alloc([8, 16], F32)
            affT, t_affT = A.alloc([1024], F32)
            posT, t_posT = A.alloc([8, 16], F32)
            selm, t_selm = A.alloc([8, 16], F32)
            sel2, t_sel2 = A.alloc([8, 16], F32)
            affm, t_affm = A.alloc([8, 16], F32)
            idxi, t_idxi = A.alloc([8, 16], I32)
            pmT, t_pmT = A.alloc([32, 4], F32)
            idxg, t_idxg = A.alloc([4, 4], I32)
            idxf, t_idxf = A.alloc([4, 4], F32)
            m7p = A.mark()
            mergedT, t_mergedT = A.alloc([KC, 1024], BF16)
            m7b = A.mark()
            hTo, t_hTo = A.alloc([KC, 1024], BF16)
            oaT, t_oaT = A.alloc([8, 1024], BF16)
            obT, t_obT = A.alloc([8, 1024], BF16)
            for ci in range(4):
                dma("sp", hTo[:, :, ci * 256:(ci + 1) * 256], src_hT[ci * 2048:(ci + 1) * 2048, :].rearrange("(k p) t -> p k t", p=128),
                    r=[t_dram["src_hT"]], w=[t_hTo])
            stop_here(50)
            ots = [A.alloc([4, 512], BF16) for _ in range(2)]
            osrc = dst_o.rearrange("(r t) c -> t r c", r=4)
            for j in range(8):
                ot, t_ot = ots[j % 2]

                for r in range(4):
                    OP("pool", lambda e, ot=ot, j=j, r=r: e.indirect_dma_start(
                        out=ot[:, r, :], out_offset=None, in_=dst_o,
                        in_offset=bass.IndirectOffsetOnAxis(ap=cidx[:, j * 4 + r:j * 4 + r + 1], axis=0)),
                       reads=t_dsto + [t_cidx], writes=[t_ot], dma=True)
                for half in range(2 if KVAR != 1 else 0):
                    bk = 6 + half
                    for rr in range(2):
                        r = half * 2 + rr
                        for w4 in range(4):
                            OP("pe", lambda e, ot=ot, r=r, w4=w4, rr=rr, bk=bk: e.transpose(out=psbf(bk)[:, (rr * 4 + w4) * 128:(rr * 4 + w4 + 1) * 128],
                                                                                           in_=ot[:, r, w4 * 128:(w4 + 1) * 128], identity=identb),
                               reads=[t_ot, t_identb], writes=[pst[bk]])
                    pv = psbf(bk).rearrange("p (a b) -> p a b", b=128)
                    for rr in range(2):
                        r = half * 2 + rr
                        OP("act", lambda e, pv=pv, r=r, rr=rr, j=j: e.copy(out=oaT[:, 2 * r:2 * r + 2, j * 128:(j + 1) * 128], in_=pv[:, rr * 4:rr * 4 + 2, :]),
                           reads=[pst[bk]], writes=[t_oaT])
                        OP("act", lambda e, pv=pv, r=r, rr=rr, j=j: e.copy(out=obT[:, 2 * r:2 * r + 2, j * 128:(j + 1) * 128], in_=pv[:, rr * 4 + 2:rr * 4 + 4, :]),
                           reads=[pst[bk]], writes=[t_obT])
            stop_here(51)
            Wsets = [dict(Wa=A.alloc([8, 256], BF16), Wb=A.alloc([8, 256], BF16), Wga=A.alloc([KC, 256], BF16), Wgb=A.alloc([KC, 256], BF16))
                     for _ in range(2)]
            sgas = [A.alloc([512], F32) for _ in range(2)]
            sgbs = [A.alloc([512], F32) for _ in range(2)]
            wba_v = w_ba.rearrange("(k p) n -> p k n", p=128)
            wbb_v = w_bb.rearrange("(k p) n -> p k n", p=128)
            wgab_v = w_gab.rearrange("(k p) n -> p k n", p=128)

            def load_w7(c8):
                ws = Wsets[c8 % 2]
                csl = slice(c8 * 256, (c8 + 1) * 256)
                dma("pool", ws["Wa"][0], wba_v[:, :, csl], w=[ws["Wa"][1]])
                dma("pool", ws["Wb"][0], wbb_v[:, :, csl], w=[ws["Wb"][1]])
                dma("pool", ws["Wga"][0], wgab_v[:, :, csl], w=[ws["Wga"][1]])
                dma("pool", ws["Wgb"][0], wgab_v[:, :, 2048 + c8 * 256:2048 + (c8 + 1) * 256], w=[ws["Wgb"][1]])
            load_w7(0)
            it7 = 0
            for c8 in range(8):
                if c8 + 1 < 8:
                    load_w7(c8 + 1)
                ws = Wsets[c8 % 2]
                (Wa, t_Wa), (Wb, t_Wb), (Wga, t_Wga), (Wgb, t_Wgb) = ws["Wa"], ws["Wb"], ws["Wga"], ws["Wgb"]
                for th in range(2):
                    tsl = slice(th * 512, (th + 1) * 512)
                    for ci in range(2):
                        cb = c8 * 2 + ci
                        wsl = slice(ci * 128, (ci + 1) * 128)
                        pb0 = (it7 % 2) * 4
                        sga, t_sga = sgas[it7 % 2]
                        sgb, t_sgb = sgbs[it7 % 2]
                        it7 += 1
                        for ch in range(8):
                            OP("pe", lambda e, ch=ch, wsl=wsl, tsl=tsl, Wa=Wa, pb0=pb0: e.matmul(psf(pb0), lhsT=Wa[:, ch, wsl], rhs=oaT[:, ch, tsl], start=(ch == 0), stop=(ch == 7)),
                               reads=[t_Wa, t_oaT], writes=[pst[pb0]])
                        for ch in range(8):
                            OP("pe", lambda e, ch=ch, wsl=wsl, tsl=tsl, Wb=Wb, pb0=pb0: e.matmul(psf(pb0 + 1), lhsT=Wb[:, ch, wsl], rhs=obT[:, ch, tsl], start=(ch == 0), stop=(ch == 7)),
                               reads=[t_Wb, t_obT], writes=[pst[pb0 + 1]])
                        for kc in range(KC):
                            OP("pe", lambda e, kc=kc, wsl=wsl, tsl=tsl, Wga=Wga, pb0=pb0: e.matmul(psf(pb0 + 2), lhsT=Wga[:, kc, wsl], rhs=hTo[:, kc, tsl], start=(kc == 0), stop=(kc == KC - 1)),
                               reads=[t_Wga, t_hTo], writes=[pst[pb0 + 2]])
                        for kc in range(KC):
                            OP("pe", lambda e, kc=kc, wsl=wsl, tsl=tsl, Wgb=Wgb, pb0=pb0: e.matmul(psf(pb0 + 3), lhsT=Wgb[:, kc, wsl], rhs=hTo[:, kc, tsl], start=(kc == 0), stop=(kc == KC - 1)),
                               reads=[t_Wgb, t_hTo], writes=[pst[pb0 + 3]])
                        OP("act", lambda e, sga=sga, pb0=pb0: e.activation(out=sga, in_=psf(pb0 + 2), func=AF.Sigmoid), reads=[pst[pb0 + 2]], writes=[t_sga])
                        OP("act", lambda e, sgb=sgb, pb0=pb0: e.activation(out=sgb, in_=psf(pb0 + 3), func=AF.Sigmoid), reads=[pst[pb0 + 3]], writes=[t_sgb])
                        OP("dve", lambda e, sga=sga, pb0=pb0: e.tensor_tensor(out=sga, in0=sga, in1=psf(pb0), op=ALU.mult), reads=[t_sga, pst[pb0]], writes=[t_sga])
                        OP("dve", lambda e, sgb=sgb, pb0=pb0: e.tensor_tensor(out=sgb, in0=sgb, in1=psf(pb0 + 1), op=ALU.mult), reads=[t_sgb, pst[pb0 + 1]], writes=[t_sgb])
                        OP("pool", lambda e, cb=cb, tsl=tsl, sga=sga, sgb=sgb: e.tensor_tensor(out=mergedT[:, cb, tsl], in0=sga, in1=sgb, op=ALU.add),
                           reads=[t_sga, t_sgb], writes=[t_mergedT])
            stop_here(52)
            A.release(m7b)
            yall, t_yall = A.alloc([8, D], F32)
            m7a = A.mark()
            Wos = [A.alloc([KC, 512], BF16) for _ in range(2)]
            wo_v = w_o.rearrange("(k p) n -> p k n", p=128)
            for cg in range(4):
                Wo, t_Wo = Wos[cg % 2]
                dma("pool", Wo, wo_v[:, :, cg * 512:(cg + 1) * 512], w=[t_Wo])
                for j in range(8):
                    bk = j % 4
                    for mc in range(KC):
                        OP("pe", lambda e, mc=mc, j=j, Wo=Wo, bk=bk: e.matmul(psf(bk), lhsT=mergedT[:, mc, j * 128:(j + 1) * 128], rhs=Wo[:, mc, :],
                                                                              start=(mc == 0), stop=(mc == KC - 1)),
                           reads=[t_mergedT, t_Wo], writes=[pst[bk]])
                    OP("act", lambda e, j=j, cg=cg, bk=bk: e.copy(out=yall[:, j, cg * 512:(cg + 1) * 512], in_=psf(bk)), reads=[pst[bk]], writes=[t_yall])

            A.release(m7a)
            stop_here(7)
            G1, t_G1 = A.alloc([D], F32)
            A2, t_A2 = A.alloc([D], F32)
            B2, t_B2 = A.alloc([D], F32)
            load_modvec(G1, t_G1, 2, 1, "g")
            load_modvec(A2, t_A2, 4, 2, "a")
            load_modvec(B2, t_B2, 3, 2, "b")
            Wr, t_Wr = A.alloc([KC, 16], F32)
            dma("sp", Wr, w_r.rearrange("(k p) n -> p k n", p=128), w=[t_Wr])
            def alias8(k):
                v = mergedT[:, 4 * k:4 * k + 4, :].rearrange("p a b -> p (a b)").bitcast(F32)
                tk = Tok()
                tk.rd = ([t_mergedT.lw] if t_mergedT.lw is not None else []) + list(t_mergedT.rd)
                return v, tk
            xt8s = [A.alloc([D], F32), alias8(0)]
            x1ts = [A.alloc([D], F32), alias8(1)]
            h2fs = [A.alloc([D], F32), alias8(2)]
            h2bs = [A.alloc([D], BF16) for _ in range(2)]
            _h2T0 = A.alloc([KC, 128], F32)
            _v, _tk = alias8(3)
            h2Ts = [_h2T0, (_v.rearrange("p (a b) -> p a b", b=128), _tk)]
            junk8, t_junk8 = A.alloc([D], BF16)
            s8s = [A.alloc([8], F32) for _ in range(2)]
            e16s = [A.alloc([16], F32) for _ in range(2)]
            pend_h2 = []
            for j in range(8):
                (xt8, t_xt8), (x1t, t_x1t), (h2f, t_h2f), (h2b, t_h2b), (h2T, t_h2T) = xt8s[j % 2], x1ts[j % 2], h2fs[j % 2], h2bs[j % 2], h2Ts[j % 2]
                s8, t_s8 = s8s[j % 2]
                e16, t_e16 = e16s[j % 2]
                dma("sp", xt8, x_own[j * 128:(j + 1) * 128, :], w=[t_xt8])
                OP("act", lambda e, j=j: e.activation(out=junk8, in_=yall[:, j, :], func=AF.Square, accum_out=s8[:, 0:1]),
                   reads=[t_yall], writes=[t_junk8, t_s8])
                rms_rstd(None, s8[:, 0:1], t_s8, s8[:, 1:2], t_s8, D)
                OP("dve", lambda e, j=j: e.scalar_tensor_tensor(out=x1t, in0=yall[:, j, :], scalar=s8[:, 1:2], in1=G1, op0=ALU.mult, op1=ALU.mult),
                   reads=[t_yall, t_s8, t_G1], writes=[t_x1t])
                OP("pool", lambda e: e.tensor_tensor(out=x1t, in0=x1t, in1=xt8, op=ALU.add), reads=[t_x1t, t_xt8], writes=[t_x1t])
                dma("sp", x1_d[j * 128:(j + 1) * 128, :], x1t, r=[t_x1t], w=[t_dram["x1_d"]])
                OP("act", lambda e: e.activation(out=junk8, in_=x1t, func=AF.Square, accum_out=s8[:, 2:3]), reads=[t_x1t], writes=[t_junk8, t_s8])
                rms_rstd(None, s8[:, 2:3], t_s8, s8[:, 3:4], t_s8, D)
                OP("dve", lambda e: e.scalar_tensor_tensor(out=h2f, in0=x1t, scalar=s8[:, 3:4], in1=A2, op0=ALU.mult, op1=ALU.mult),
                   reads=[t_x1t, t_s8, t_A2], writes=[t_h2f])
                OP("dve", lambda e: e.tensor_tensor(out=h2f, in0=h2f, in1=B2, op=ALU.add), reads=[t_h2f, t_B2], writes=[t_h2f])
                OP("act", lambda e: e.copy(out=h2b, in_=h2f), reads=[t_h2f], writes=[t_h2b])
                dma("sp", src_h2[j * 128:(j + 1) * 128, :], h2b, r=[t_h2b], w=[t_h2_ch[j // 2]])
                if j % 2 == 1:
                    pend_h2.append(ag_start(src_h2, 256, j // 2, t_h2_ch[j // 2], dst=dst_h2))
                for g in range(4):
                    bk = g
                    for k4 in range(4):
                        kc = g * 4 + k4
                        OP("pe", lambda e, kc=kc, k4=k4, bk=bk: e.transpose(out=psf(bk)[:, k4 * 128:(k4 + 1) * 128], in_=h2f[:, kc * 128:(kc + 1) * 128], identity=identf),
                           reads=[t_h2f, t_identf], writes=[pst[bk]])
                    OP("act", lambda e, g=g, bk=bk: e.copy(out=h2T[:, g * 4:g * 4 + 4, :], in_=psf(bk).rearrange("p (a b) -> p a b", b=128)),
                       reads=[pst[bk]], writes=[t_h2T])
                for kc in range(KC):
                    OP("pe", lambda e, kc=kc: e.matmul(psf(4)[:, 0:16], lhsT=h2T[:, kc, :], rhs=Wr[:, kc, :], start=(kc == 0), stop=(kc == KC - 1)),
                       reads=[t_h2T, t_Wr], writes=[pst[4]])
                OP("dve", lambda e: e.tensor_reduce(out=s8[:, 4:5], in_=psf(4)[:, 0:16], axis=AX.X, op=ALU.max), reads=[pst[4]], writes=[t_s8])
                OP("dve", lambda e: e.tensor_scalar(out=s8[:, 5:6], in0=s8[:, 4:5], scalar1=-1.0, scalar2=None, op0=ALU.mult), reads=[t_s8], writes=[t_s8])
                OP("act", lambda e: e.activation(out=e16, in_=psf(4)[:, 0:16], func=AF.Exp, bias=s8[:, 5:6], scale=1.0, accum_out=s8[:, 6:7]),
                   reads=[pst[4], t_s8], writes=[t_e16, t_s8])
                OP("dve", lambda e: e.reciprocal(out=s8[:, 7:8], in_=s8[:, 6:7]), reads=[t_s8], writes=[t_s8])
                OP("dve", lambda e, j=j: e.tensor_scalar(out=affown[:, j, :], in0=e16, scalar1=s8[:, 7:8], scalar2=None, op0=ALU.mult),
                   reads=[t_e16, t_s8], writes=[t_affown])
                OP("pe", lambda e, j=j: e.transpose(out=psf(5)[0:16, 0:128], in_=affown[:, j, :], identity=identf),
                   reads=[t_affown, t_identf], writes=[pst[5]])
                OP("act", lambda e, j=j: e.copy(out=affT[0:16, j * 128:(j + 1) * 128], in_=psf(5)[0:16, 0:128]), reads=[pst[5]], writes=[t_affT])
            dma("sp", src_aff, affT[0:16, :], r=[t_affT], w=[t_dram["src_aff"]])
            allgather(src_aff, dst_aff, G4, t_dram["src_aff"], t_dram["dst_aff"])
            t_dsth2 = [t for (_, t) in pend_h2]
            A.release(m7p)
            Wg_, t_Wg = A.alloc([KC, 1024], BF16)
            Wu_, t_Wu = A.alloc([KC, 1024], BF16)
            Wd_, t_Wd = A.alloc([8, D], BF16)

            def load_expert(k):
                wg_v = wg[k].rearrange("(k p) n -> p k n", p=128)
                wu_v = wu[k].rearrange("(k p) n -> p k n", p=128)
                wd_v = wd[k].rearrange("(k p) n -> p k n", p=128)
                for g in range(2):
                    dma("pool", Wg_[:, 8 * g:8 * g + 8, :], wg_v[:, 8 * g:8 * g + 8, :], w=[t_Wg])
                    dma("pool", Wu_[:, 8 * g:8 * g + 8, :], wu_v[:, 8 * g:8 * g + 8, :], w=[t_Wu])
                for g in range(2):
                    dma("pool", Wd_[:, 4 * g:4 * g + 4, :], wd_v[:, 4 * g:4 * g + 4, :], w=[t_Wd])
            load_expert(0)
            m8keep = A.mark()

            stop_here(8)
            affR, t_affR = A.alloc([S], F32)
            junk9, t_junk9 = A.alloc([S], F32)
            maskR, t_maskR = A.alloc([S], F32)
            r9, t_r9 = A.alloc([8], F32)
            av_ = dst_aff.rearrange("(q e) t -> e q t", q=4)
            dma("sp", affR[0:16, :].rearrange("p (q t) -> p q t", q=4), av_, r=[t_dram["dst_aff"]], w=[t_affR])
            R32 = slice(0, 16)
            OP("dve", lambda e: e.memset(r9[R32, :], 0.5), writes=[t_r9])
            NIT = 24
            for k in range(NIT):
                wk = 2.0 ** -(k + 1)
                OP("dve", lambda e: e.tensor_scalar(out=junk9[R32, :], in0=affR[R32, :], scalar1=r9[R32, 1:2], scalar2=0.0, op0=ALU.is_gt, op1=ALU.add,
                                                    accum_out=r9[R32, 2:3]),
                   reads=[t_affR, t_r9], writes=[t_junk9, t_r9])
                OP("dve", lambda e, wk=wk: e.tensor_scalar(out=r9[R32, 3:4], in0=r9[R32, 2:3], scalar1=511.5, scalar2=wk, op0=ALU.is_gt, op1=ALU.mult),
                   reads=[t_r9], writes=[t_r9])
                OP("dve", lambda e, wk=wk: e.scalar_tensor_tensor(out=r9[R32, 1:2], in0=r9[R32, 3:4], scalar=-0.5 * wk, in1=r9[R32, 1:2], op0=ALU.add, op1=ALU.add),
                   reads=[t_r9], writes=[t_r9])
            OP("dve", lambda e: e.tensor_scalar(out=r9[R32, 0:1], in0=r9[R32, 1:2], scalar1=-(2.0 ** -(NIT + 1)), scalar2=None, op0=ALU.add),
               reads=[t_r9], writes=[t_r9])
            OP("dve", lambda e: e.tensor_scalar(out=maskR[R32, :], in0=affR[R32, :], scalar1=r9[R32, 0:1], scalar2=None, op0=ALU.is_gt),
               reads=[t_affR, t_r9], writes=[t_maskR])
            OP("pool", lambda e: e.memset(junk9[R32, :], 1.0), writes=[t_junk9])
            OP("dve", lambda e: e.tensor_tensor_scan(out=affR[R32, :], data0=junk9[R32, :], data1=maskR[R32, :], initial=0.0, op0=ALU.mult, op1=ALU.add),
               reads=[t_junk9, t_maskR], writes=[t_affR])
            OP("dve", lambda e: e.tensor_tensor(out=affR[R32, :], in0=affR[R32, :], in1=maskR[R32, :], op=ALU.mult), reads=[t_affR, t_maskR], writes=[t_affR])
            OP("dve", lambda e: e.tensor_scalar(out=affR[R32, :], in0=affR[R32, :], scalar1=-1.0, scalar2=None, op0=ALU.add), reads=[t_affR], writes=[t_affR])
            dma("sp", route_d, affR[R32, :], r=[t_affR], w=[t_dram["route_d"]])
            t_rd2 = Tok()
            for qr in range(4):
                dma("sp", route_d2[qr * 16:(qr + 1) * 16, :], affR[R32, qr * 1024:(qr + 1) * 1024], r=[t_affR], w=[t_rd2])
            A.release(m8keep)
            slab, t_slab = A.alloc([1024], F32)

            OP("pool", lambda e: e.indirect_dma_start(
                out=slab[0:16, :], out_offset=None, in_=route_d2,
                in_offset=bass.IndirectOffsetOnAxis(ap=cidx[0:16, 33:34], axis=0)),
               reads=[t_rd2, t_cidx], writes=[t_slab], dma=True)
            for j in range(8):
                OP("pe", lambda e, j=j: e.transpose(out=psf(0)[:, j * 16:(j + 1) * 16], in_=slab[0:16, j * 128:(j + 1) * 128], identity=identf[0:16, 0:16]),
                   reads=[t_slab, t_identf], writes=[pst[0]])
            OP("act", lambda e: e.copy(out=posT, in_=psf(0)[:, 0:128].rearrange("p (a b) -> p a b", b=16)), reads=[pst[0]], writes=[t_posT])
            OP("dve", lambda e: e.tensor_scalar(out=selm, in0=posT, scalar1=-0.5, scalar2=None, op0=ALU.is_gt), reads=[t_posT], writes=[t_selm])
            OP("dve", lambda e: e.tensor_scalar(out=sel2, in0=posT, scalar1=511.5, scalar2=None, op0=ALU.is_lt), reads=[t_posT], writes=[t_sel2])
            OP("dve", lambda e: e.tensor_tensor(out=selm, in0=selm, in1=sel2, op=ALU.mult), reads=[t_selm, t_sel2], writes=[t_selm])
            OP("dve", lambda e: e.tensor_tensor(out=affm, in0=affown, in1=selm, op=ALU.mult), reads=[t_affown, t_selm], writes=[t_affm])
            OP("dve", lambda e: e.tensor_scalar(out=sel2, in0=posT, scalar1=255.5, scalar2=768.0, op0=ALU.is_gt, op1=ALU.mult), reads=[t_posT], writes=[t_sel2])
            OP("dve", lambda e: e.tensor_tensor(out=posT, in0=posT, in1=sel2, op=ALU.add), reads=[t_posT, t_sel2], writes=[t_posT])
            OP("dve", lambda e: e.tensor_tensor(out=posT, in0=posT, in1=rowbase.unsqueeze(1).to_broadcast([128, 8, 16]), op=ALU.add),
               reads=[t_posT, t_rowbase], writes=[t_posT])
            OP("dve", lambda e: e.tensor_tensor(out=posT, in0=posT, in1=selm, op=ALU.mult), reads=[t_posT, t_selm], writes=[t_posT])
            OP("dve", lambda e: e.tensor_copy(out=idxi, in_=posT), reads=[t_posT], writes=[t_idxi])
            pm, t_pm = A.alloc([S], F32)
            ohall, t_ohall = A.alloc([32, 512], BF16)
            OP("pool", lambda e: e.indirect_dma_start(
                out=pm[0:4, :], out_offset=None, in_=route_d,
                in_offset=bass.IndirectOffsetOnAxis(ap=cidx[0:4, 32:33], axis=0)),
               reads=[t_dram["route_d"], t_cidx], writes=[t_pm], dma=True)
            for tt in range(32):
                OP("pe", lambda e, tt=tt: e.transpose(out=psf(1)[:, tt * 4:(tt + 1) * 4], in_=pm[0:4, tt * 128:(tt + 1) * 128], identity=identf[0:4, 0:4]),
                   reads=[t_pm, t_identf], writes=[pst[1]])
            OP("act", lambda e: e.copy(out=pmT, in_=psf(1)[:, 0:128].rearrange("p (a b) -> p a b", b=4)), reads=[pst[1]], writes=[t_pmT])
            psidx = psf(2)[:, 0:32].rearrange("p (a b c) -> p a b c", b=4, c=2)
            for pp in range(4):
                for tt in range(32):
                    OP("dve", lambda e, tt=tt, pp=pp: e.tensor_scalar(out=ohall[:, tt, :], in0=iota, scalar1=pmT[:, tt, pp:pp + 1], scalar2=None, op0=ALU.is_equal),
                       reads=[t_iota, t_pmT], writes=[t_ohall])
                for s4 in range(4):
                    for tt in range(32):
                        OP("pe", lambda e, tt=tt, pp=pp, s4=s4: e.matmul(psidx[:, pp, s4, :], lhsT=ohall[:, tt, s4 * 128:(s4 + 1) * 128], rhs=tval[:, tt, :],
                                                                         start=(tt == 0), stop=(tt == 31)),
                           reads=[t_ohall, t_tval], writes=[pst[2]])
            idx2, t_idx2 = A.alloc([4, 4, 2], F32)
            OP("act", lambda e: e.copy(out=idx2, in_=psidx), reads=[pst[2]], writes=[t_idx2])
            OP("dve", lambda e: e.scalar_tensor_tensor(out=idxf, in0=idx2[:, :, :, 0], scalar=64.0, in1=idx2[:, :, :, 1], op0=ALU.mult, op1=ALU.add),
               reads=[t_idx2], writes=[t_idxf])
            OP("dve", lambda e: e.tensor_copy(out=idxg, in_=idxf), reads=[t_idxf], writes=[t_idxg])

            stop_here(9)
            A.release(m8keep)
            m10 = A.mark()
            xgs = [A.alloc([D], BF16) for _ in range(2)]
            xeTs = [A.alloc([KC, 512], BF16) for _ in range(2)]
            aT, t_aT = A.alloc([8, 512], BF16)
            sgf, t_sgf = A.alloc([512], F32)
            ysb = [A.alloc([D], BF16) for _ in range(2)]
            pend_Y = []
            dvY = dst_Y.rearrange("(r c m) x -> c r m x", r=4, c=8)

            def load_gu(k):
                wg_v = wg[k].rearrange("(k p) n -> p k n", p=128)
                wu_v = wu[k].rearrange("(k p) n -> p k n", p=128)
                for g in range(2):
                    dma("pool", Wg_[:, 8 * g:8 * g + 8, :], wg_v[:, 8 * g:8 * g + 8, :], w=[t_Wg])
                    dma("pool", Wu_[:, 8 * g:8 * g + 8, :], wu_v[:, 8 * g:8 * g + 8, :], w=[t_Wu])

            def load_d(k):
                wd_v = wd[k].rearrange("(k p) n -> p k n", p=128)
                for g in range(2):
                    dma("pool", Wd_[:, 4 * g:4 * g + 4, :], wd_v[:, 4 * g:4 * g + 4, :], w=[t_Wd])

            def gather_x(k):
                xeT, t_xeT = xeTs[k % 2]
                for s4 in range(4):
                    xg, t_xg = xgs[s4 % 2]
                    OP("pool", lambda e, xg=xg, k=k, s4=s4: e.indirect_dma_start(
                        out=xg, out_offset=None, in_=dst_h2,
                        in_offset=bass.IndirectOffsetOnAxis(ap=idxg[:, k, s4:s4 + 1], axis=0),
                        ),
                       reads=t_dsth2 + [t_idxg], writes=[t_xg], dma=True)
                    for half in range(2):
                        bk = 6 + half
                        for k8 in range(8):
                            kc = half * 8 + k8
                            OP("pe", lambda e, xg=xg, kc=kc, k8=k8, bk=bk: e.transpose(out=psbf(bk)[:, k8 * 128:(k8 + 1) * 128],
                                                                                       in_=xg[:, kc * 128:(kc + 1) * 128], identity=identb),
                               reads=[t_xg, t_identb], writes=[pst[bk]])
                        OP("act", lambda e, half=half, bk=bk, s4=s4, xeT=xeT: e.copy(out=xeT[:, half * 8:half * 8 + 8, s4 * 128:(s4 + 1) * 128],
                                                                                  in_=psbf(bk).rearrange("p (a b) -> p a b", b=128)),
                           reads=[pst[bk]], writes=[t_xeT])

            def gateup(k):
                xeT, t_xeT = xeTs[k % 2]
                for ft in range(8):
                    fsl = slice(ft * 128, (ft + 1) * 128)
                    bg = (ft % 2) * 2
                    for kc in range(KC):
                        OP("pe", lambda e, kc=kc, fsl=fsl, bg=bg, xeT=xeT: e.matmul(psf(bg), lhsT=Wg_[:, kc, fsl], rhs=xeT[:, kc, :], start=(kc == 0), stop=(kc == KC - 1)),
                           reads=[t_Wg, t_xeT], writes=[pst[bg]])
                    for kc in range(KC):
                        OP("pe", lambda e, kc=kc, fsl=fsl, bg=bg, xeT=xeT: e.matmul(psf(bg + 1), lhsT=Wu_[:, kc, fsl], rhs=xeT[:, kc, :], start=(kc == 0), stop=(kc == KC - 1)),
                           reads=[t_Wu, t_xeT], writes=[pst[bg + 1]])
                    OP("act", lambda e, bg=bg: e.activation(out=sgf, in_=psf(bg), func=AF.Silu), reads=[pst[bg]], writes=[t_sgf])
                    OP("dve", lambda e, bg=bg, ft=ft: e.tensor_tensor(out=aT[:, ft, :], in0=sgf, in1=psf(bg + 1), op=ALU.mult),
                       reads=[t_sgf, pst[bg + 1]], writes=[t_aT])

            def down(k):
                pp = k
                for s4 in range(4):
                    y_ap, t_y = ysb[s4 % 2]
                    for cg in range(4):
                        bk = 4 + (cg % 2)
                        for ft in range(8):
                            OP("pe", lambda e, ft=ft, s4=s4, cg=cg, bk=bk: e.matmul(psf(bk), lhsT=aT[:, ft, s4 * 128:(s4 + 1) * 128],
                                                                                    rhs=Wd_[:, ft, cg * 512:(cg + 1) * 512], start=(ft == 0), stop=(ft == 7)),
                               reads=[t_aT, t_Wd], writes=[pst[bk]])
                        OP("act", lambda e, y_ap=y_ap, cg=cg, bk=bk: e.copy(out=y_ap[:, cg * 512:(cg + 1) * 512], in_=psf(bk)), reads=[pst[bk]], writes=[t_y])
                    dma("sp", src_Y[pp * 512 + s4 * 128:pp * 512 + (s4 + 1) * 128, :], y_ap, r=[t_y], w=[t_Y_ch[pp * 2 + s4 // 2]])

            def finish_Y(k):
                for ci in (2 * k, 2 * k + 1):
                    tmp, ttmp = pend_Y[ci]
                    dma("sp", dvY[ci], tmp.rearrange("(r m) x -> r m x", r=4), r=[ttmp], w=[t_dram["dst_Y"]])

            gather_x(0)
            for k in range(4):
                gateup(k)
                if k + 1 < 4:
                    load_gu(k + 1)
                    gather_x(k + 1)
                down(k)
                if k + 1 < 4:
                    load_d(k + 1)
                for ci in (2 * k, 2 * k + 1):
                    pend_Y.append(ag_start(src_Y, 256, ci, t_Y_ch[ci], dst=dst_Y))
            t_dstY = [t for (_, t) in pend_Y]
            A.release(m10)

            stop_here(10)
            G2, t_G2 = A.alloc([D], F32)
            load_modvec(G2, t_G2, 5, 3, "g")
            gbs = [A.alloc([D], BF16) for _ in range(4)]
            accs = [A.alloc([D], F32) for _ in range(2)]
            x1rs = [A.alloc([D], F32) for _ in range(2)]
            dgs = [A.alloc([128], BF16) for _ in range(2)]
            junk11, t_junk11 = A.alloc([D], BF16)
            s11s = [A.alloc([4], F32) for _ in range(2)]
            for gb_, t_gb in gbs:
                OP("pool", lambda e, gb_=gb_: e.memset(gb_, 0.0), writes=[t_gb])
            gi = 0
            finals = []
            for j in range(8):
                acc, t_acc = accs[j % 2]
                x1r, t_x1r = x1rs[j % 2]
                s11, t_s11 = s11s[j % 2]
                pb0 = (j % 2) * 4
                dma("sp", x1r, x1_d[j * 128:(j + 1) * 128, :], r=[t_dram["x1_d"]], w=[t_x1r])
                for ex in range(16):
                    gb_, t_gb = gbs[gi % 4]
                    dg, t_dg = dgs[gi % 2]
                    gi += 1
                    OP("pool", lambda e, gb_=gb_, j=j, ex=ex: e.indirect_dma_start(
                        out=gb_, out_offset=None, in_=dst_Y,
                        in_offset=bass.IndirectOffsetOnAxis(ap=idxi[:, j, ex:ex + 1], axis=0),
                        ),
                       reads=t_dstY + [t_idxi, t_gb], writes=[t_gb], dma=True)
                    OP("dve", lambda e, dg=dg, j=j, ex=ex: e.tensor_scalar(out=dg, in0=identb, scalar1=affm[:, j, ex:ex + 1], scalar2=None, op0=ALU.mult),
                       reads=[t_identb, t_affm], writes=[t_dg])
                    for cg in range(4):
                        OP("pe", lambda e, dg=dg, gb_=gb_, cg=cg, ex=ex, pb0=pb0: e.matmul(psf(pb0 + cg), lhsT=dg, rhs=gb_[:, cg * 512:(cg + 1) * 512],
                                                                                          start=(ex == 0), stop=(ex == 15)),
                           reads=[t_dg, t_gb], writes=[pst[pb0 + cg]])
                for cg in range(4):
                    OP("act", lambda e, acc=acc, cg=cg, pb0=pb0: e.copy(out=acc[:, cg * 512:(cg + 1) * 512], in_=psf(pb0 + cg)), reads=[pst[pb0 + cg]], writes=[t_acc])
                OP("act", lambda e, acc=acc, s11=s11: e.activation(out=junk11, in_=acc, func=AF.Square, accum_out=s11[:, 0:1]), reads=[t_acc], writes=[t_junk11, t_s11])
                rms_rstd(None, s11[:, 0:1], t_s11, s11[:, 1:2], t_s11, D)
                OP("dve", lambda e, acc=acc, s11=s11: e.scalar_tensor_tensor(out=acc, in0=acc, scalar=s11[:, 1:2], in1=G2, op0=ALU.mult, op1=ALU.mult),
                   reads=[t_acc, t_s11, t_G2], writes=[t_acc])
                OP("dve", lambda e, acc=acc, x1r=x1r: e.tensor_tensor(out=acc, in0=acc, in1=x1r, op=ALU.add), reads=[t_acc, t_x1r], writes=[t_acc])
                finals.append(dma("sp", out[j * 128:(j + 1) * 128, :], acc, r=[t_acc]))
            P.final_waits.extend(finals)
        except _Stop:
            pass
        if DEBUG:
            for nm, src, shp, dt in (("dbg_o", src_o, [S, 512], BF16), ("dbg_x1", x1_d, [1024, D], F32),
                                     ("dbg_route", route_d, [16, S], F32), ("dbg_aff", dst_aff, [64, 1024], F32),
                                     ("dbg_mod", dst_mod, [4, 3072], F32), ("dbg_hT", src_hT, [4 * KC * 128, 256], BF16),
                                     ("dbg_Y", src_Y, [2048, D], BF16), ("dbg_h2", src_h2, [1024, D], BF16),
                                     ("dbg_vg", vg, [S, 512], BF16), ("dbg_hgT", hgT.rearrange("a b c p t -> (a b c p) t"), [12 * 128, S], BF16),
                                     ("dbg_koutM", koutM.rearrange("a b t d -> (a b t) d"), [4 * S, 128], BF16),
                                     ):
                dd_ = nc.dram_tensor(nm, shp, dt, kind="ExternalOutput").ap()
                key = {"dbg_o": "src_o", "dbg_x1": "x1_d", "dbg_route": "route_d", "dbg_aff": "dst_aff", "dbg_mod": "dst_mod",
                       "dbg_hT": "src_hT", "dbg_Y": "src_Y", "dbg_h2": "src_h2", "dbg_vg": "vg", "dbg_hgT": "hgT", "dbg_koutM": "koutM", "dbg_dsthT": "dst_hT"}[nm]
                P.final_waits.append(dma("sp", dd_, src, r=[t_dram[key]]))
        P.emit(nc, st)
    return nc


def _consts():
    bf = ml_dtypes.bfloat16
    c = {}
    c["c_identb"] = np.eye(128, dtype=np.float32).astype(bf)
    c["c_identf"] = np.eye(128, dtype=np.float32)
    m = np.arange(128)[:, None]
    l = np.arange(128)[None, :]
    same = (m // 64) == (l // 64)
    c["c_maskF"] = (same & (l >= m)).astype(np.float32)
    c["c_maskB"] = (same & (m >= l)).astype(np.float32)
    ps = np.zeros((128, 128), np.float32)
    for mm_ in range(128):
        ps[(mm_ + 64) % 128, mm_] = 1.0
    c["c_pswap"] = ps.astype(bf)
    half = 64
    inv = (10000.0 ** (-np.arange(half, dtype=np.float32) / half)).astype(np.float32)
    sm = np.zeros((128, 4), np.float32)
    sm[:, 0] = np.concatenate([inv, inv])
    sm[:, 1] = np.concatenate([-np.ones(64), np.ones(64)])
    sm[:, 2] = -math.pi * sm[:, 1]
    sm[:, 3] = -math.pi
    c["c_small"] = sm
    qi = np.arange(128)[:, None]
    kj = np.arange(384)[None, :]
    bandok = np.abs(kj - 128 - qi) <= 128
    bm = np.zeros((128, 3, 384), np.float32)
    for var in range(3):
        ok = bandok.copy()
        if var == 0:
            ok &= (kj >= 128)
        if var == 2:
            ok &= (kj < 256)
        bm[:, var, :] = np.where(ok, 0.0, -30000.0)
    c["c_band"] = bm
    rm = np.ones((128, 512), np.float32)
    rm[:, ::64] = 0.0
    c["c_rmask"] = rm
    c["c_iota"] = np.broadcast_to(np.arange(512, dtype=np.float32)[None, :], (128, 512)).copy()
    t = np.arange(32)[None, :] * 128 + np.arange(128)[:, None]
    trow = ((t // 256) % 4) * 1024 + (t // 1024) * 256 + (t % 256)
    c["c_tval"] = np.stack([trow // 64, trow % 64], axis=-1).astype(np.float32).astype(bf)
    return c


_NC_CACHE = {}


def kernel(x, c, positions, w_ada, b_ada, g_pre_mix, g_post_mix, g_pre_ffn, g_post_ffn,
           w_in, hg_lb_logits, hg_out_norm, attn_sink, w_branch_a, w_branch_b, w_out,
           w_router, w_exp_gate, w_exp_up, w_exp_down):
    f = lambda a: np.ascontiguousarray(np.asarray(a))
    x, c, positions = f(x), f(c), f(positions)
    w_ada, b_ada, w_in = f(w_ada)[0], f(b_ada)[0], f(w_in)[0]
    lbl_all = f(hg_lb_logits)
    hgn_all = f(hg_out_norm)[0]
    sink_all = f(attn_sink)[0]
    w_ba, w_bb, w_o, w_r = f(w_branch_a)[0], f(w_branch_b)[0], f(w_out)[0], f(w_router)[0]
    wg_all, wu_all, wd_all = f(w_exp_gate)[0], f(w_exp_up)[0], f(w_exp_down)[0]
    gv = np.stack([f(g_pre_mix)[0], f(g_post_mix)[0], f(g_pre_ffn)[0], f(g_post_ffn)[0]], axis=0)
    consts = _consts()
    if "nc" not in _NC_CACHE:
        _NC_CACHE["nc"] = build_program()
    nc = _NC_CACHE["nc"]
    O = {"hq": 0, "ff": 1024, "fb": 2048, "hi": 3072, "hg": 4096, "aq": 5120, "ak": 6144, "av": 6400, "ga": 6656, "gb": 8704}
    w_gab = np.ascontiguousarray(w_in[:, O["ga"]:O["ga"] + 4096])
    in_maps = []
    for core in range(8):
        b, q = core // 4, core % 4
        hs = [2 * q, 2 * q + 1]
        kvh = q // 2
        cols_fm = []
        for base in ("hq", "ff", "fb", "aq"):
            for h in hs:
                cols_fm.append(np.arange(O[base] + h * 128, O[base] + (h + 1) * 128))
        cols_fm.append(np.arange(O["ak"] + kvh * 128, O["ak"] + (kvh + 1) * 128))
        cols_fm = np.concatenate(cols_fm)
        cols_tm = np.concatenate([np.arange(O["hi"] + h * 128, O["hi"] + (h + 1) * 128) for h in hs] +
                                 [np.arange(O["hg"] + h * 128, O["hg"] + (h + 1) * 128) for h in hs] +
                                 [np.arange(O["av"] + kvh * 128, O["av"] + (kvh + 1) * 128)])
        lbl = np.zeros((128, 4, 2), np.float32)
        for dr in range(2):
            for hh in range(2):
                h = hs[hh]
                lbl[:, dr * 2 + hh, :] = lbl_all[dr, :, h * 128:(h + 1) * 128].T
        cidx_h = np.zeros((128, 40), np.int32)
        pp_ = np.arange(128)
        for j_ in range(8):
            for r_ in range(4):
                cidx_h[:, j_ * 4 + r_] = q * 4096 + r_ * 1024 + j_ * 128 + pp_
        cidx_h[:, 32] = np.minimum(4 * q + pp_, 15)
        cidx_h[:, 33] = np.minimum(16 * q + pp_, 63)
        rowbase = np.array([(e % 4) * 2048 + (e // 4) * 256 for e in range(16)], np.float32)
        m = {
            "x_own": x[b, q * 1024:(q + 1) * 1024, :],
            "c_b": np.ascontiguousarray(c[b].reshape(KC, 128).T),
            "pos_b": positions[b:b + 1, :].astype(np.int32),
            "w_ada_q": np.ascontiguousarray(w_ada[:, q * 3072:(q + 1) * 3072]),
            "b_ada_q": b_ada[None, q * 3072:(q + 1) * 3072],
            "gvecs": gv,
            "w_fm": np.ascontiguousarray(w_in[:, cols_fm]),
            "w_tm": np.ascontiguousarray(w_in[:, cols_tm]),
            "w_gab": w_gab,
            "lbl": lbl,
            "hgn": np.ascontiguousarray(hgn_all[hs].reshape(1, 256)),
            "sink": np.ascontiguousarray(sink_all[hs].reshape(1, 2)),
            "w_ba": w_ba, "w_bb": w_bb, "w_o": w_o, "w_r": w_r,
            "wg": wg_all[4 * q:4 * q + 4], "wu": wu_all[4 * q:4 * q + 4], "wd": wd_all[4 * q:4 * q + 4],
            "c_rowbase": np.broadcast_to(rowbase[None, :], (128, 16)).copy(),
            "c_idx": cidx_h,
        }
        m.update(consts)
        in_maps.append({k: np.ascontiguousarray(v) for k, v in m.items()})
    res = run_bass_kernel_spmd(nc, in_maps, core_ids=list(range(8)))
    outp = np.zeros((2, S, D), np.float32)
    for core in range(8):
        b, q = core // 4, core % 4
        outp[b, q * 1024:(q + 1) * 1024, :] = res.results[core]["out"]
    if DEBUG:
        kernel.debug = res.results
    return outp
```

```python
import os
import math
import types
from contextlib import ExitStack
import numpy as np
import ml_dtypes
import concourse.bass as bass
import concourse.mybir as mybir
from concourse.bass_utils import run_bass_kernel_spmd

F32 = mybir.dt.float32
BF16 = mybir.dt.bfloat16
I32 = mybir.dt.int32
U8 = mybir.dt.uint8
AF = mybir.ActivationFunctionType
ALU = mybir.AluOpType
AX = mybir.AxisListType
DSZ = {F32: 4, BF16: 2, I32: 4, U8: 1}

DEBUG = bool(int(os.environ.get("KDEBUG", "0")))
KSTOP = int(os.environ.get("KSTOP", "99"))
KVAR = int(os.environ.get("KVAR", "0"))


class _Stop(Exception):
    pass
D = 2048
S = 4096
KC = 16
EPS = 1e-6
BIG = 1.0e6
G4 = [[0, 1, 2, 3], [4, 5, 6, 7]]
G8 = [list(range(8))]


class Tok:
    __slots__ = ("lw", "rd")

    def __init__(self):
        self.lw = None
        self.rd = []


class Op:
    __slots__ = ("eng", "fn", "deps", "dma", "ms", "msidx", "dsem", "dcount", "dprev", "seq")

    def __init__(self, eng, fn, dma):
        self.eng = eng
        self.fn = fn
        self.deps = []
        self.dma = dma
        self.ms = False
        self.msidx = None
        self.dsem = None
        self.dcount = None
        self.dprev = None


ENGS = ("pe", "act", "dve", "pool", "sp")


def _freeze(f):
    if getattr(f, "__closure__", None) is None:
        return f
    cells = []
    for c in f.__closure__:
        try:
            cells.append(types.CellType(c.cell_contents))
        except ValueError:
            cells.append(c)
    return types.FunctionType(f.__code__, f.__globals__, f.__name__, f.__defaults__, tuple(cells))


class Prog:
    def __init__(self, n_dma_sems=40):
        self.ops = {e: [] for e in ENGS}
        self.n_dma_sems = n_dma_sems
        self.all = []
        self.final_waits = []

    def op(self, eng, fn, reads=(), writes=(), dma=False):
        o = Op(eng, _freeze(fn), dma)
        deps = []
        for t in reads:
            if t.lw is not None:
                deps.append(t.lw)
        for t in writes:
            if t.lw is not None:
                deps.append(t.lw)
            deps.extend(t.rd)
        o.seq = len(self.all)
        seen = set()
        last = {}
        for d in deps:
            if id(d) in seen:
                continue
            seen.add(id(d))
            if d.dma:
                o.deps.append(d)
                continue
            if (not dma) and d.eng == "pe" and eng == "pe":
                continue
            if d.eng not in last or last[d.eng].seq < d.seq:
                last[d.eng] = d
        for d in last.values():
            o.deps.append(d)
            d.ms = True
        for t in reads:
            if not dma:
                t.rd = [x for x in t.rd if x.dma or x.eng != eng]
            t.rd.append(o)
        for t in writes:
            t.lw = o
            t.rd = []
        self.ops[eng].append(o)
        self.all.append(o)
        return o

    def get_dyn(self, eng, key):
        if getattr(self, "dyn", None) is None:
            pid = eng.partition_id()
            self.dyn = {"q1024": eng.snap((pid % 4) * 1024), "q4": eng.snap((pid % 4) * 4)}
        return self.dyn[key]

    def emit(self, nc, stack):
        for e in ENGS:
            k = 0
            for o in self.ops[e]:
                if (not o.dma) and o.ms:
                    k += 1
                    o.msidx = k
        esem = {e: stack.enter_context(nc.semaphore("s_" + e)) for e in ENGS}
        dsems = [stack.enter_context(nc.semaphore("d%d" % i)) for i in range(self.n_dma_sems)]
        dcum = [0] * self.n_dma_sems
        k = 0
        ksw = 0
        n_sw = 12
        n_hw = self.n_dma_sems - n_sw
        for o in self.all:
            if o.dma == "cc":
                dsems.append(stack.enter_context(nc.semaphore("cc%d" % len(dsems))))
                o.dsem = len(dsems) - 1
                o.dprev = 0
                o.dcount = 1
            elif o.dma:
                if o.eng == "pool":
                    i = n_hw + (ksw % n_sw)
                    ksw += 1
                else:
                    i = k % n_hw
                    k += 1
                o.dsem = i
                o.dprev = dcum[i]
                dcum[i] += 16
                o.dcount = dcum[i]
        finals = self.final_waits
        LIMIT = 2400
        segs = [[]]
        cnt = {e: 0 for e in ENGS}
        for o in self.all:
            c = 2 + len(o.deps)
            if cnt[o.eng] + c > LIMIT:
                segs.append([])
                cnt = {e: 0 for e in ENGS}
            cnt[o.eng] += c
            segs[-1].append(o)
        waited_all = {e: {} for e in ENGS}

        def run(ename, eng, seg, last):
            waited = waited_all[ename]

            def w(key, sem, val):
                if val <= 0 or waited.get(key, 0) >= val:
                    return
                waited[key] = val
                eng.wait_ge(sem, val)

            for o in seg:
                if o.eng != ename:
                    continue
                for d in o.deps:
                    if d.dma:
                        w(("d", d.dsem), dsems[d.dsem], d.dcount)
                    else:
                        w(("e", d.eng), esem[d.eng], d.msidx)
                if o.dma:
                    w(("d", o.dsem), dsems[o.dsem], o.dprev)
                    ins = o.fn(eng)
                    ins.then_inc(dsems[o.dsem], 1 if o.dma == "cc" else 16)
                else:
                    ins = o.fn(eng)
                    if o.ms:
                        ins.then_inc(esem[ename], 1)
            if last and ename == "sp":
                for d in finals:
                    if d.dma:
                        w(("d", d.dsem), dsems[d.dsem], d.dcount)
                    else:
                        w(("e", d.eng), esem[d.eng], d.msidx)

        for si, seg in enumerate(segs):
            last = si == len(segs) - 1
            with nc.Block() as block:
                @block.sync
                def _(eng):
                    run("sp", eng, seg, last)

                @block.tensor
                def _(eng):
                    run("pe", eng, seg, last)

                @block.scalar
                def _(eng):
                    run("act", eng, seg, last)

                @block.vector
                def _(eng):
                    run("dve", eng, seg, last)

                @block.gpsimd
                def _(eng):
                    run("pool", eng, seg, last)


class Arena:
    def __init__(self, ap_u8, nbytes):
        self.ap = ap_u8
        self.n = nbytes
        self.top = 0
        self.hist = []

    def mark(self):
        return self.top

    def release(self, m):
        self.top = m

    def alloc(self, shape, dt):
        n = DSZ[dt]
        for s in shape:
            n *= s
        start = (self.top + 31) // 32 * 32
        end = start + n
        assert end <= self.n, ("SBUF arena overflow", end, self.n)
        self.top = end
        tok = Tok()
        live = []
        for (s0, e0, t0) in self.hist:
            if s0 < end and start < e0:
                if t0.lw is not None:
                    tok.rd.append(t0.lw)
                tok.rd.extend(t0.rd)
            if t0.lw is not None or t0.rd:
                live.append((s0, e0, t0))
        self.hist = [h for h in self.hist if not (h[0] >= start and h[1] <= end)]
        self.hist.append((start, end, tok))
        v = self.ap[:, start:end].bitcast(dt)
        if len(shape) == 2:
            v = v.rearrange("p (a b) -> p a b", b=shape[1])
        elif len(shape) == 3:
            v = v.rearrange("p (a b c) -> p a b c", b=shape[1], c=shape[2])
        return v, tok


def build_program():
    nc = bass.Bass("TRN2", target_bir_lowering=False)
    P = Prog()

    def din(name, shape, dt):
        return nc.dram_tensor(name, shape, dt, kind="ExternalInput").ap()

    def dscr(name, shape, dt):
        return nc.dram_tensor(name, shape, dt, kind="Internal").ap()

    x_own = din("x_own", [1024, D], F32)
    c_b = din("c_b", [128, KC], F32)
    pos_b = din("pos_b", [1, S], I32)
    w_ada_q = din("w_ada_q", [D, 3072], F32)
    b_ada_q = din("b_ada_q", [1, 3072], F32)
    gvecs = din("gvecs", [4, D], F32)
    w_fm = din("w_fm", [D, 1152], F32)
    w_tm = din("w_tm", [D, 640], F32)
    w_gab = din("w_gab", [D, 4096], F32)
    lbl = din("lbl", [128, 4, 2], F32)
    hgn = din("hgn", [1, 256], F32)
    sink = din("sink", [1, 2], F32)
    w_ba = din("w_ba", [1024, D], F32)
    w_bb = din("w_bb", [1024, D], F32)
    w_o = din("w_o", [D, D], F32)
    w_r = din("w_r", [D, 16], F32)
    wg = din("wg", [4, D, 1024], F32)
    wu = din("wu", [4, D, 1024], F32)
    wd = din("wd", [4, 1024, D], F32)
    c_identb = din("c_identb", [128, 128], BF16)
    c_identf = din("c_identf", [128, 128], F32)
    c_maskF = din("c_maskF", [128, 128], F32)
    c_maskB = din("c_maskB", [128, 128], F32)
    c_pswap = din("c_pswap", [128, 128], BF16)
    c_small = din("c_small", [128, 4], F32)
    c_band = din("c_band", [128, 3, 384], F32)
    c_rmask = din("c_rmask", [128, 512], F32)
    c_iota = din("c_iota", [128, 512], F32)
    c_tval = din("c_tval", [128, 32, 2], BF16)
    c_rowbase = din("c_rowbase", [128, 16], F32)
    c_idx = din("c_idx", [128, 40], I32)
    out = nc.dram_tensor("out", [1024, D], F32, kind="ExternalOutput").ap()

    src_mod = dscr("src_mod", [1, 3072], F32)
    dst_mod = dscr("dst_mod", [4, 3072], F32)
    src_hT = dscr("src_hT", [4 * KC * 128, 256], BF16)
    dst_hT = dscr("dst_hT", [4 * KC * 128, 1024], BF16)
    hgT = dscr("hgT", [2, 2, 3, 128, S], BF16)
    koutM = dscr("koutM", [2, 2, S, 128], BF16)
    vg = dscr("vg", [S, 512], BF16)
    src_o = dscr("src_o", [S, 512], BF16)
    dst_o = dscr("dst_o", [4 * S, 512], BF16)
    x1_d = dscr("x1_d", [1024, D], F32)
    src_h2 = dscr("src_h2", [1024, D], BF16)
    dst_h2 = dscr("dst_h2", [4096, D], BF16)
    src_aff = dscr("src_aff", [16, 1024], F32)
    dst_aff = dscr("dst_aff", [64, 1024], F32)
    route_d = dscr("route_d", [16, S], F32)
    route_d2 = dscr("route_d2", [64, 1024], F32)
    src_Y = dscr("src_Y", [2048, D], BF16)
    dst_Y = dscr("dst_Y", [8192, D], BF16)
    t_o_ch = [Tok() for _ in range(4)]
    t_h2_ch = [Tok() for _ in range(4)]
    t_Y_ch = [Tok() for _ in range(8)]
    t_dram = {n: Tok() for n in ("src_mod", "dst_mod", "src_hT", "dst_hT", "hgT", "koutM", "vg", "src_o", "dst_o",
                                 "x1_d", "src_h2", "dst_h2", "src_aff", "dst_aff", "route_d", "src_Y", "dst_Y")}

    with ExitStack() as st:
        ARENA_BYTES = 204 * 1024
        arena_t = st.enter_context(nc.sbuf_tensor("arena", [128, ARENA_BYTES], U8))
        A = Arena(arena_t, ARENA_BYTES)
        psb = [st.enter_context(nc.psum_tensor("ps%d" % i, [128, 512], F32)) for i in range(8)]
        pst = [Tok() for _ in range(8)]

        def psf(i):
            return psb[i][:, :]

        def psbf(i):
            return psb[i][:, :].bitcast(BF16)

        OP = P.op

        def stop_here(n):
            if KSTOP == n:
                raise _Stop()

        def dma(eng, out_ap, in_ap, r=(), w=()):
            return OP(eng, lambda e: e.dma_start(out=out_ap, in_=in_ap), reads=r, writes=w, dma=True)

        agc = [0]

        def ag_start(src, rows, ci, tsrc, dst=None):
            ttmp = Tok()
            if dst is not None:
                allgather(src[ci * rows:(ci + 1) * rows, :], dst[ci * 4 * rows:(ci + 1) * 4 * rows, :], G4, tsrc, ttmp)
                return None, ttmp
            agc[0] += 1
            tmp = dscr("agtmp%d" % agc[0], [4 * rows, src.shape[1]], src.dtype)
            allgather(src[ci * rows:(ci + 1) * rows, :], tmp, G4, tsrc, ttmp)
            return tmp, ttmp

        def ag_finish(dst, nch, pend, tdst, only=None):
            dv = dst.rearrange("(r c m) x -> c r m x", r=4, c=nch)
            for ci, (tmp, ttmp) in enumerate(pend):
                if only is not None and ci != only:
                    continue
                dma("sp", dv[ci], tmp.rearrange("(r m) x -> r m x", r=4), r=[ttmp], w=[tdst])

        def ag_chunked(src, dst, rows, nch, tsrc, tdst):
            C = src.shape[1]
            dv = dst.rearrange("(r c m) x -> c r m x", r=4, c=nch)
            for ci in range(nch):
                agc[0] += 1
                tmp = dscr("agtmp%d" % agc[0], [4 * rows, C], src.dtype)
                ttmp = Tok()
                allgather(src[ci * rows:(ci + 1) * rows, :], tmp, G4, tsrc[ci] if isinstance(tsrc, list) else tsrc, ttmp)
                dma("sp", dv[ci], tmp.rearrange("(r m) x -> r m x", r=4), r=[ttmp], w=[tdst])

        def allgather(src, dst, groups, tsrc, tdst):
            return OP("pool", lambda e: e.collective_compute("AllGather", ALU.bypass, replica_groups=groups,
                                                             ins=[src.opt()], outs=[dst.opt()]),
                      reads=[tsrc], writes=[tdst], dma="cc")

        identb, t_identb = A.alloc([128], BF16)
        identf, t_identf = A.alloc([128], F32)
        maskF, t_maskF = A.alloc([128], F32)
        maskB, t_maskB = A.alloc([128], F32)
        pswap, t_pswap = A.alloc([128], BF16)
        small, t_small = A.alloc([4], F32)
        band, t_band = A.alloc([3, 384], F32)
        rmask, t_rmask = A.alloc([512], F32)
        iota, t_iota = A.alloc([512], F32)
        tval, t_tval = A.alloc([32, 2], BF16)
        rowbase, t_rowbase = A.alloc([16], F32)
        cidx, t_cidx = A.alloc([40], I32)
        lbt, t_lbt = A.alloc([4, 2], F32)
        lbv, t_lbv = A.alloc([4, 3], F32)
        hgnb, t_hgnb = A.alloc([256], F32)
        sinkb, t_sinkb = A.alloc([2], F32)
        for (sb_, src_, tk) in ((identb, c_identb, t_identb), (identf, c_identf, t_identf), (maskF, c_maskF, t_maskF),
                                (maskB, c_maskB, t_maskB), (pswap, c_pswap, t_pswap), (small, c_small, t_small),
                                (band, c_band, t_band), (rmask, c_rmask, t_rmask), (iota, c_iota, t_iota),
                                (tval, c_tval, t_tval), (rowbase, c_rowbase, t_rowbase), (lbt, lbl, t_lbt), (cidx, c_idx, t_cidx)):
            dma("sp", sb_, src_, w=[tk])
        dma("sp", hgnb, hgn.partition_broadcast(128), w=[t_hgnb])
        dma("sp", sinkb, sink.partition_broadcast(128), w=[t_sinkb])
        OP("dve", lambda e: e.tensor_tensor(out=lbv[:, :, 0], in0=lbt[:, :, 0], in1=lbt[:, :, 1], op=ALU.subtract),
           reads=[t_lbt], writes=[t_lbv])
        OP("act", lambda e: e.activation(out=lbv[:, :, 0], in_=lbv[:, :, 0], func=AF.Sigmoid), reads=[t_lbv], writes=[t_lbv])
        OP("dve", lambda e: e.tensor_scalar(out=lbv[:, :, 1], in0=lbv[:, :, 0], scalar1=-1.0, scalar2=1.0, op0=ALU.mult, op1=ALU.add),
           reads=[t_lbv], writes=[t_lbv])
        OP("dve", lambda e: e.tensor_scalar(out=lbv[:, :, 2], in0=lbv[:, :, 1], scalar1=-1.0, scalar2=None, op0=ALU.mult),
           reads=[t_lbv], writes=[t_lbv])
        invf = small[:, 0:1]
        sinsign = small[:, 1:2]
        negpis = small[:, 2:3]
        negpi = small[:, 3:4]

        def rms_rstd(eng_unused, ss, tss, rstd, trstd, n):
            OP("dve", lambda e: e.tensor_scalar(out=rstd, in0=ss, scalar1=1.0 / n, scalar2=EPS, op0=ALU.mult, op1=ALU.add),
               reads=[tss], writes=[trstd])
            OP("act", lambda e: e.activation(out=rstd, in_=rstd, func=AF.Sqrt), reads=[trstd], writes=[trstd])
            OP("dve", lambda e: e.reciprocal(out=rstd, in_=rstd), reads=[trstd], writes=[trstd])

        modflat = dst_mod.rearrange("r n -> (r n)").rearrange("(s d) -> s d", d=D)

        try:
            m1 = A.mark()
            cs, t_cs = A.alloc([KC], F32)
            csb, t_csb = A.alloc([KC], BF16)
            wada, t_wada = A.alloc([KC, 3072], BF16)
            bada, t_bada = A.alloc([3072], F32)
            modrow, t_modrow = A.alloc([3072], F32)
            dma("sp", cs, c_b, w=[t_cs])
            dma("sp", bada[0:1, :], b_ada_q, w=[t_bada])
            wsrc = w_ada_q.rearrange("(k p) n -> p k n", p=128)
            for g in range(4):
                dma("pool", wada[:, 4 * g:4 * g + 4, :], wsrc[:, 4 * g:4 * g + 4, :], w=[t_wada])
            OP("act", lambda e: e.activation(out=csb, in_=cs, func=AF.Silu), reads=[t_cs], writes=[t_csb])
            for g in range(6):
                bk = g % 4
                for kc in range(KC):
                    OP("pe", lambda e, g=g, kc=kc, bk=bk: e.matmul(psf(bk)[0:1, :], lhsT=csb[:, kc:kc + 1],
                                                                   rhs=wada[:, kc, g * 512:(g + 1) * 512],
                                                                   start=(kc == 0), stop=(kc == KC - 1)),
                       reads=[t_csb, t_wada], writes=[pst[bk]])
                OP("dve", lambda e, g=g, bk=bk: e.tensor_tensor(out=modrow[0:1, g * 512:(g + 1) * 512], in0=psf(bk)[0:1, :],
                                                                in1=bada[0:1, g * 512:(g + 1) * 512], op=ALU.add),
                   reads=[pst[bk], t_bada], writes=[t_modrow])
            dma("sp", src_mod, modrow[0:1, :], r=[t_modrow], w=[t_dram["src_mod"]])
            allgather(src_mod, dst_mod, G4, t_dram["src_mod"], t_dram["dst_mod"])
            A.release(m1)

            def load_modvec(dst_ap, tok, sidx, gidx, mode):
                if mode == "b":
                    dma("sp", dst_ap, modflat[sidx:sidx + 1, :].partition_broadcast(128), r=[t_dram["dst_mod"]], w=[tok])
                    return
                mk = A.mark()
                tmp, t_tmp = A.alloc([D], F32)
                dma("sp", dst_ap, modflat[sidx:sidx + 1, :].partition_broadcast(128), r=[t_dram["dst_mod"]], w=[tok])
                dma("sp", tmp, gvecs[gidx:gidx + 1, :].partition_broadcast(128), w=[t_tmp])
                if mode == "a":
                    OP("dve", lambda e: e.scalar_tensor_tensor(out=dst_ap, in0=dst_ap, scalar=1.0, in1=tmp, op0=ALU.add, op1=ALU.mult),
                       reads=[tok, t_tmp], writes=[tok])
                else:
                    OP("dve", lambda e: e.tensor_tensor(out=dst_ap, in0=dst_ap, in1=tmp, op=ALU.mult), reads=[tok, t_tmp], writes=[tok])
                A.release(mk)

            stop_here(1)
            m2 = A.mark()
            hTown, t_hTown = A.alloc([KC, 1024], BF16)
            A1, t_A1 = A.alloc([D], F32)
            B1, t_B1 = A.alloc([D], F32)
            load_modvec(A1, t_A1, 1, 0, "a")
            load_modvec(B1, t_B1, 0, 0, "b")
            xts = [A.alloc([D], F32) for _ in range(2)]
            junk, t_junk = A.alloc([D], BF16)
            hfs = [A.alloc([D], F32) for _ in range(2)]
            hbs = [A.alloc([D], BF16) for _ in range(2)]
            ss8, t_ss8 = A.alloc([8], F32)
            rs8, t_rs8 = A.alloc([8], F32)
            hT_tmp = []
            for j in range(8):
                xt, t_xt = xts[j % 2]
                hb, t_hb = hbs[j % 2]
                hf, t_hf = hfs[j % 2]
                dma("sp", xt, x_own[j * 128:(j + 1) * 128, :], w=[t_xt])
                OP("act", lambda e, xt=xt, j=j: e.activation(out=junk, in_=xt, func=AF.Square, accum_out=ss8[:, j:j + 1]),
                   reads=[t_xt], writes=[t_junk, t_ss8])
                rms_rstd(None, ss8[:, j:j + 1], t_ss8, rs8[:, j:j + 1], t_rs8, D)
                OP("dve", lambda e, xt=xt, j=j: e.scalar_tensor_tensor(out=hf, in0=xt, scalar=rs8[:, j:j + 1], in1=A1,
                                                                       op0=ALU.mult, op1=ALU.mult),
                   reads=[t_xt, t_rs8, t_A1], writes=[t_hf])
                OP("pool", lambda e, hb=hb: e.tensor_tensor(out=hb, in0=hf, in1=B1, op=ALU.add), reads=[t_hf, t_B1], writes=[t_hb])
                for half in range(2):
                    bk = 6 + half
                    for k8 in range(8):
                        kc = half * 8 + k8
                        OP("pe", lambda e, hb=hb, kc=kc, k8=k8, bk=bk: e.transpose(out=psbf(bk)[:, k8 * 128:(k8 + 1) * 128],
                                                                                   in_=hb[:, kc * 128:(kc + 1) * 128], identity=identb),
                           reads=[t_hb, t_identb], writes=[pst[bk]])
                    OP("act", lambda e, half=half, bk=bk, j=j: e.copy(out=hTown[:, half * 8:half * 8 + 8, j * 128:(j + 1) * 128],
                                                                      in_=psbf(bk).rearrange("p (a b) -> p a b", b=128)),
                       reads=[pst[bk]], writes=[t_hTown])
                if j % 2 == 1:
                    ci = j // 2
                    tch = Tok()
                    dma("sp", src_hT[ci * 2048:(ci + 1) * 2048, :].rearrange("(k p) t -> p k t", p=128), hTown[:, :, ci * 256:(ci + 1) * 256],
                        r=[t_hTown], w=[tch, t_dram["src_hT"]])
                    hT_tmp.append(ag_start(src_hT, 2048, ci, tch))

            A.release(m2)

            stop_here(2)
            m3 = A.mark()
            qTa = [A.alloc([S], BF16) for _ in range(2)]
            kTa, t_kTa = A.alloc([S + 256], BF16)
            vat, t_vat = A.alloc([34, 128], BF16)
            OP("pool", lambda e: e.memset(kTa[:, 0:128], 0.0), writes=[t_kTa])
            OP("pool", lambda e: e.memset(kTa[:, S + 128:S + 256], 0.0), writes=[t_kTa])
            OP("pool", lambda e: e.memset(vat[:, 0, :], 0.0), writes=[t_vat])
            OP("pool", lambda e: e.memset(vat[:, 33, :], 0.0), writes=[t_vat])
            dec = [[A.alloc([64], F32) for _ in range(2)] for _ in range(2)]
            m3b = A.mark()
            Wfm, t_Wfm = A.alloc([KC, 1152], BF16)
            Wtm, t_Wtm = A.alloc([KC, 640], BF16)
            wfs = w_fm.rearrange("(k p) n -> p k n", p=128)
            wts = w_tm.rearrange("(k p) n -> p k n", p=128)
            for g in range(2):
                dma("pool", Wfm[:, 8 * g:8 * g + 8, :], wfs[:, 8 * g:8 * g + 8, :], w=[t_Wfm])
            dma("pool", Wtm, wts, w=[t_Wtm])
            hblks = [A.alloc([KC, 512], BF16) for _ in range(2)]
            qf = [A.alloc([512], F32) for _ in range(2)]
            scr0 = [A.alloc([512], F32) for _ in range(11)]
            scr1a = [A.alloc([512], F32) for _ in range(4)]
            scr1b = [A.alloc([512], F32) for _ in range(2)]
            scr = [scr0, scr1a + scr0[4:7] + scr1b + scr0[9:]]
            prods = [A.alloc([3, 512], BF16) for _ in range(2)]
            koutf = [A.alloc([512], BF16) for _ in range(2)]
            koutm = [A.alloc([4, 128], BF16) for _ in range(2)]
            tmo = [A.alloc([512], BF16) for _ in range(2)]
            posi, t_posi = A.alloc([512], I32)
            posf, t_posf = A.alloc([512], F32)
            ang, t_ang = A.alloc([512], F32)
            cosT, t_cosT = A.alloc([512], F32)
            sinT, t_sinT = A.alloc([512], F32)
            qbs = [A.alloc([512], BF16) for _ in range(3)]
            rt1, t_rt1 = A.alloc([512], F32)
            rt2, t_rt2 = A.alloc([512], F32)
            hsrcs = [tmp.rearrange("(r k p) t -> r p k t", r=4, k=KC) for (tmp, _) in hT_tmp]
            v3 = lambda ap: ap.rearrange("p (c l) -> p c l", l=64)
            pcount = [0]
            order3 = [0, 2, 4, 6, 1, 3, 5, 7]

            def load_hblk(it3):
                tb_ = order3[it3]
                r_, jb_ = tb_ // 2, tb_ % 2
                hb_, t_hb_ = hblks[it3 % 2]
                for h_ in range(2):
                    dma("sp", hb_[:, :, h_ * 256:(h_ + 1) * 256], hsrcs[2 * jb_ + h_][r_], r=[hT_tmp[2 * jb_ + h_][1]], w=[t_hb_])
            load_hblk(0)
            for it3, tb in enumerate(order3):
                r, jb = tb // 2, tb % 2
                hblk, t_hblk = hblks[it3 % 2]
                if it3 + 1 < 8:
                    load_hblk(it3 + 1)
                tsl = slice(tb * 512, (tb + 1) * 512)
                dma("sp", posi, pos_b[0:1, tsl].partition_broadcast(128), w=[t_posi])
                OP("dve", lambda e: e.tensor_copy(out=posf, in_=posi), reads=[t_posi], writes=[t_posf])
                OP("dve", lambda e: e.tensor_scalar(out=ang, in0=posf, scalar1=invf, scalar2=None, op0=ALU.mult),
                   reads=[t_posf, t_small], writes=[t_ang])
                C1 = 6.28125
                C2 = 2 * math.pi - 6.28125
                OP("dve", lambda e: e.tensor_scalar(out=rt1, in0=ang, scalar1=1.0 / (2 * math.pi), scalar2=None, op0=ALU.mult), reads=[t_ang], writes=[t_rt1])
                OP("dve", lambda e: e.tensor_copy(out=posi, in_=rt1), reads=[t_rt1], writes=[t_posi])
                OP("dve", lambda e: e.tensor_copy(out=rt1, in_=posi), reads=[t_posi], writes=[t_rt1])
                OP("dve", lambda e: e.scalar_tensor_tensor(out=ang, in0=rt1, scalar=-C1, in1=ang, op0=ALU.mult, op1=ALU.add), reads=[t_rt1, t_ang], writes=[t_ang])
                OP("dve", lambda e: e.scalar_tensor_tensor(out=ang, in0=rt1, scalar=-C2, in1=ang, op0=ALU.mult, op1=ALU.add), reads=[t_rt1, t_ang], writes=[t_ang])

                def wrap(buf, tbuf):
                    OP("dve", lambda e: e.tensor_scalar(out=rt2, in0=buf, scalar1=math.pi, scalar2=-2 * math.pi, op0=ALU.is_gt, op1=ALU.mult), reads=[tbuf], writes=[t_rt2])
                    OP("dve", lambda e: e.tensor_tensor(out=buf, in0=buf, in1=rt2, op=ALU.add), reads=[tbuf, t_rt2], writes=[tbuf])
                    OP("dve", lambda e: e.tensor_scalar(out=rt2, in0=buf, scalar1=-math.pi, scalar2=2 * math.pi, op0=ALU.is_lt, op1=ALU.mult), reads=[tbuf], writes=[t_rt2])
                    OP("dve", lambda e: e.tensor_tensor(out=buf, in0=buf, in1=rt2, op=ALU.add), reads=[tbuf, t_rt2], writes=[tbuf])
                wrap(ang, t_ang)
                OP("act", lambda e: e.activation(out=sinT, in_=ang, func=AF.Sin, scale=sinsign), reads=[t_ang, t_small], writes=[t_sinT])
                OP("dve", lambda e: e.tensor_scalar(out=rt1, in0=ang, scalar1=0.5 * math.pi, scalar2=None, op0=ALU.add), reads=[t_ang], writes=[t_rt1])
                wrap(rt1, t_rt1)
                OP("act", lambda e: e.activation(out=cosT, in_=rt1, func=AF.Sin), reads=[t_rt1], writes=[t_cosT])

                def fm_matmul(cb, bk):
                    for kc in range(KC):
                        OP("pe", lambda e, kc=kc: e.matmul(psf(bk), lhsT=Wfm[:, kc, cb * 128:(cb + 1) * 128], rhs=hblk[:, kc, :],
                                                           start=(kc == 0), stop=(kc == KC - 1)),
                           reads=[t_Wfm, t_hblk], writes=[pst[bk]])

                for hh in range(2):
                    bk = pcount[0] % 4
                    pcount[0] += 1
                    fm_matmul(hh, bk)
                    OP("act", lambda e, hh=hh, bk=bk: e.activation(out=qf[hh][0], in_=psf(bk), func=AF.Silu),
                       reads=[pst[bk]], writes=[qf[hh][1]])
                bkmap = {}

                def stageA(dr, hh):
                    cb = 2 + dr * 2 + hh
                    li = dr * 2 + hh
                    bk = pcount[0] % 4
                    pcount[0] += 1
                    bkmap[(dr, hh)] = bk
                    fm_matmul(cb, bk)
                    sig, t_sig = scr[li % 2][0]
                    OP("act", lambda e, bk=bk: e.activation(out=sig, in_=psf(bk), func=AF.Sigmoid), reads=[pst[bk]], writes=[t_sig])

                def stageB(dr, hh):
                    li = dr * 2 + hh
                    bk = bkmap[(dr, hh)]
                    ((sig, t_sig), (logf, t_logf), (kk, t_kk), (bb, t_bb), (bx, t_bx), (dd, t_dd), (d2, t_d2),
                     (E1, t_E1), (E2, t_E2), (E3, t_E3), (E4, t_E4)) = scr[li % 2]
                    q_ap, t_q = qf[hh]
                    prod, t_prod = prods[(dr * 2 + hh) % 2]
                    kof, t_kof = koutf[(dr * 2 + hh) % 2]
                    kom, t_kom = koutm[(dr * 2 + hh) % 2]
                    dec_ap, t_dec = dec[hh][dr]
                    OP("act", lambda e, li=li: e.activation(out=logf, in_=sig, func=AF.Ln, bias=lbv[:, li, 0:1], scale=lbv[:, li, 1:2]),
                       reads=[t_sig, t_lbv], writes=[t_logf])
                    OP("dve", lambda e, li=li: e.tensor_scalar(out=kk, in0=sig, scalar1=lbv[:, li, 2:3], scalar2=lbv[:, li, 1:2],
                                                               op0=ALU.mult, op1=ALU.add),
                       reads=[t_sig, t_lbv], writes=[t_kk])
                    OP("dve", lambda e: e.tensor_tensor_scan(out=bb, data0=rmask, data1=logf, initial=0.0, op0=ALU.mult, op1=ALU.add),
                       reads=[t_rmask, t_logf], writes=[t_bb])
                    OP("act", lambda e, dec_ap=dec_ap, tb=tb: e.activation(out=dec_ap[:, tb * 8:(tb + 1) * 8], in_=v3(bb)[:, :, 63], func=AF.Exp),
                       reads=[t_bb], writes=[t_dec])
                    if dr == 0:
                        OP("dve", lambda e: e.tensor_tensor(out=v3(dd), in0=v3(bb), in1=v3(bb)[:, :, 32:33].to_broadcast([128, 8, 64]), op=ALU.subtract),
                           reads=[t_bb], writes=[t_dd])
                        OP("dve", lambda e: e.tensor_tensor(out=v3(d2), in0=v3(bb), in1=v3(bb)[:, :, 63:64].to_broadcast([128, 8, 64]), op=ALU.subtract),
                           reads=[t_bb], writes=[t_d2])
                        OP("act", lambda e: e.activation(out=E1, in_=dd, func=AF.Exp), reads=[t_dd], writes=[t_E1])
                        OP("act", lambda e: e.activation(out=E2, in_=dd, func=AF.Exp, scale=-1.0), reads=[t_dd], writes=[t_E2])
                        OP("act", lambda e: e.activation(out=E3, in_=bb, func=AF.Exp), reads=[t_bb], writes=[t_E3])
                        OP("act", lambda e: e.activation(out=E4, in_=d2, func=AF.Exp, scale=-1.0), reads=[t_d2], writes=[t_E4])
                    else:
                        OP("dve", lambda e: e.tensor_tensor(out=bx, in0=bb, in1=logf, op=ALU.subtract), reads=[t_bb, t_logf], writes=[t_bx])
                        OP("dve", lambda e: e.tensor_tensor(out=v3(dd), in0=v3(bx), in1=v3(bx)[:, :, 32:33].to_broadcast([128, 8, 64]), op=ALU.subtract),
                           reads=[t_bx], writes=[t_dd])
                        OP("dve", lambda e: e.tensor_tensor(out=v3(d2), in0=v3(bx), in1=v3(bb)[:, :, 63:64].to_broadcast([128, 8, 64]), op=ALU.subtract),
                           reads=[t_bx, t_bb], writes=[t_d2])
                        OP("act", lambda e: e.activation(out=E1, in_=dd, func=AF.Exp, scale=-1.0), reads=[t_dd], writes=[t_E1])
                        OP("act", lambda e: e.activation(out=E2, in_=dd, func=AF.Exp), reads=[t_dd], writes=[t_E2])
                        OP("act", lambda e: e.activation(out=E3, in_=d2, func=AF.Exp, scale=-1.0), reads=[t_d2], writes=[t_E3])
                        OP("act", lambda e: e.activation(out=E4, in_=bx, func=AF.Exp), reads=[t_bx], writes=[t_E4])
                    OP("pool", lambda e, prod=prod, q_ap=q_ap: e.tensor_tensor(out=prod[:, 0, :], in0=q_ap, in1=E1, op=ALU.mult),
                       reads=[t_q, t_E1], writes=[t_prod])
                    OP("pool", lambda e, prod=prod: e.tensor_tensor(out=prod[:, 1, :], in0=kk, in1=E2, op=ALU.mult),
                       reads=[t_kk, t_E2], writes=[t_prod])
                    OP("dve", lambda e, prod=prod, q_ap=q_ap: e.tensor_tensor(out=prod[:, 2, :], in0=q_ap, in1=E3, op=ALU.mult),
                       reads=[t_q, t_E3], writes=[t_prod])
                    OP("pool", lambda e, kof=kof: e.tensor_tensor(out=kof, in0=kk, in1=E4, op=ALU.mult),
                       reads=[t_kk, t_E4], writes=[t_kof])
                    dma("sp", hgT[hh, dr].rearrange("a p t -> p a t")[:, :, tsl], prod, r=[t_prod], w=[t_dram["hgT"]])

                    def stageC(kof=kof, t_kof=t_kof, kom=kom, t_kom=t_kom, hh=hh, dr=dr):
                        for i in range(4):
                            OP("pe", lambda e, i=i, kof=kof: e.transpose(out=psbf(6)[:, i * 128:(i + 1) * 128], in_=kof[:, i * 128:(i + 1) * 128], identity=identb),
                               reads=[t_kof, t_identb], writes=[pst[6]])
                        OP("act", lambda e, kom=kom: e.copy(out=kom, in_=psbf(6)[:, 0:512].rearrange("p (a b) -> p a b", b=128)),
                           reads=[pst[6]], writes=[t_kom])
                        dma("sp", koutM[hh, dr, tsl, :].rearrange("(i p) d -> p i d", p=128), kom, r=[t_kom], w=[t_dram["koutM"]])
                    return stageC

                items3 = [(0, 0), (0, 1), (1, 0), (1, 1)]
                stageA(*items3[0])
                pendC = []
                for n3, it_ in enumerate(items3):
                    if n3 + 1 < 4:
                        stageA(*items3[n3 + 1])
                    if pendC:
                        pendC.pop(0)()
                    pendC.append(stageB(*it_))
                for ci in range(3):
                    cb = 6 + ci
                    bk = pcount[0] % 4
                    pcount[0] += 1
                    fm_matmul(cb, bk)
                    qb, t_qb = qbs[ci]
                    OP("act", lambda e, bk=bk, qb=qb: e.copy(out=qb, in_=psf(bk)), reads=[pst[bk]], writes=[t_qb])
                while pendC:
                    pendC.pop(0)()

                def ropeB(ci):
                    qb, t_qb = qbs[ci]
                    OP("pe", lambda e: e.matmul(psf(7), lhsT=pswap, rhs=qb, start=True, stop=True), reads=[t_pswap, t_qb], writes=[pst[7]])
                    OP("pool", lambda e: e.tensor_tensor(out=rt1, in0=qb, in1=cosT, op=ALU.mult), reads=[t_qb, t_cosT], writes=[t_rt1])
                    OP("dve", lambda e: e.tensor_tensor(out=rt2, in0=psf(7), in1=sinT, op=ALU.mult), reads=[pst[7], t_sinT], writes=[t_rt2])
                    if ci < 2:
                        dst_ap, t_dst = qTa[ci][0][:, tsl], qTa[ci][1]
                    else:
                        dst_ap, t_dst = kTa[:, 128 + tb * 512:128 + (tb + 1) * 512], t_kTa
                    OP("pool", lambda e, dst_ap=dst_ap: e.tensor_tensor(out=dst_ap, in0=rt1, in1=rt2, op=ALU.add),
                       reads=[t_rt1, t_rt2], writes=[t_dst])
                for i in range(4):
                    tmo_ap, t_tmo = tmo[i % 2]
                    gt = tb * 4 + i
                    for kc in range(KC):
                        OP("pe", lambda e, kc=kc, i=i: e.matmul(psf(4), lhsT=hblk[:, kc, i * 128:(i + 1) * 128], rhs=Wtm[:, kc, 0:512],
                                                                start=(kc == 0), stop=(kc == KC - 1)),
                           reads=[t_hblk, t_Wtm], writes=[pst[4]])
                    for kc in range(KC):
                        OP("pe", lambda e, kc=kc, i=i: e.matmul(psf(5)[:, 0:128], lhsT=hblk[:, kc, i * 128:(i + 1) * 128], rhs=Wtm[:, kc, 512:640],
                                                                start=(kc == 0), stop=(kc == KC - 1)),
                           reads=[t_hblk, t_Wtm], writes=[pst[5]])
                    OP("act", lambda e, tmo_ap=tmo_ap: e.copy(out=tmo_ap[:, 0:256], in_=psf(4)[:, 0:256]), reads=[pst[4]], writes=[t_tmo])
                    OP("act", lambda e, tmo_ap=tmo_ap: e.activation(out=tmo_ap[:, 256:512], in_=psf(4)[:, 256:512], func=AF.Silu),
                       reads=[pst[4]], writes=[t_tmo])
                    OP("dve", lambda e, gt=gt: e.tensor_copy(out=vat[:, gt + 1, :], in_=psf(5)[:, 0:128]), reads=[pst[5]], writes=[t_vat])
                    dma("sp", vg[gt * 128:(gt + 1) * 128, :], tmo_ap, r=[t_tmo], w=[t_dram["vg"]])
                    if i < 3:
                        ropeB(i)
            A.release(m3b)

            stop_here(3)
            m4 = A.mark()
            Ssb = [A.alloc([386], F32) for _ in range(4)]
            Pb = [A.alloc([386], BF16) for _ in range(4)]
            for hh_ in range(4):
                OP("pool", lambda e, hh_=hh_: e.tensor_copy(out=Ssb[hh_][0][:, 384:385], in_=sinkb[:, hh_ % 2:hh_ % 2 + 1]), reads=[t_sinkb], writes=[Ssb[hh_][1]])
            PTs = [A.alloc([384], BF16) for _ in range(2)]
            st4 = [A.alloc([8], F32) for _ in range(4)]
            obt = [A.alloc([256], BF16) for _ in range(2)]
            scale = 128 ** -0.5
            it4 = 0
            pend4 = []

            def att_stageA(i, hh, it4):
                var = 0 if i == 0 else (2 if i == 31 else 1)
                s_ap, t_s = Ssb[it4 % 4]
                p_ap, t_p = Pb[it4 % 4]
                sc4, t_sc4 = st4[it4 % 4]
                bS = it4 % 4
                q_ap, t_q = qTa[hh]
                OP("pe", lambda e, q_ap=q_ap, i=i, bS=bS: e.matmul(psf(bS)[:, 0:384], lhsT=q_ap[:, i * 128:(i + 1) * 128],
                                                                   rhs=kTa[:, i * 128:i * 128 + 384], start=True, stop=True),
                   reads=[t_q, t_kTa], writes=[pst[bS]])
                OP("dve", lambda e, s_ap=s_ap, bS=bS, var=var: e.scalar_tensor_tensor(out=s_ap[:, 0:384], in0=psf(bS)[:, 0:384], scalar=scale,
                                                                                      in1=band[:, var, :], op0=ALU.mult, op1=ALU.add),
                   reads=[pst[bS], t_band], writes=[t_s])
                OP("dve", lambda e, s_ap=s_ap, sc4=sc4: e.tensor_reduce(out=sc4[:, 0:1], in_=s_ap[:, 0:385], axis=AX.X, op=ALU.max),
                   reads=[t_s], writes=[t_sc4])
                OP("dve", lambda e, sc4=sc4: e.tensor_scalar(out=sc4[:, 2:3], in0=sc4[:, 0:1], scalar1=-1.0, scalar2=None, op0=ALU.mult),
                   reads=[t_sc4], writes=[t_sc4])
                OP("act", lambda e, s_ap=s_ap, p_ap=p_ap, sc4=sc4: e.activation(out=p_ap[:, 0:385], in_=s_ap[:, 0:385], func=AF.Exp, bias=sc4[:, 2:3], scale=1.0,
                                                                                 accum_out=sc4[:, 3:4]),
                   reads=[t_s, t_sc4], writes=[t_p, t_sc4])
                OP("dve", lambda e, sc4=sc4: e.reciprocal(out=sc4[:, 6:7], in_=sc4[:, 3:4]), reads=[t_sc4], writes=[t_sc4])

            def att_stageB(i, hh, it4):
                ob_ap, t_ob = obt[i % 2]
                p_ap, t_p = Pb[it4 % 4]
                pt_ap, t_pt = PTs[it4 % 2]
                sc4, t_sc4 = st4[it4 % 4]
                bT = 4 + it4 % 2
                bO = 6 + it4 % 2
                for kb in range(3):
                    OP("pe", lambda e, kb=kb, p_ap=p_ap, bT=bT: e.transpose(out=psbf(bT)[:, kb * 128:(kb + 1) * 128],
                                                                           in_=p_ap[:, kb * 128:(kb + 1) * 128], identity=identb),
                       reads=[t_p, t_identb], writes=[pst[bT]])
                OP("act", lambda e, pt_ap=pt_ap, bT=bT: e.copy(out=pt_ap, in_=psbf(bT)[:, 0:384]), reads=[pst[bT]], writes=[t_pt])
                for kb in range(3):
                    OP("pe", lambda e, kb=kb, pt_ap=pt_ap, bO=bO, i=i: e.matmul(psf(bO)[:, 0:128], lhsT=pt_ap[:, kb * 128:(kb + 1) * 128],
                                                                                rhs=vat[:, i + kb, :], start=(kb == 0), stop=(kb == 2)),
                       reads=[t_pt, t_vat], writes=[pst[bO]])
                OP("dve", lambda e, ob_ap=ob_ap, hh=hh, bO=bO, sc4=sc4: e.tensor_scalar(out=ob_ap[:, hh * 128:(hh + 1) * 128], in0=psf(bO)[:, 0:128],
                                                                                        scalar1=sc4[:, 6:7], scalar2=None, op0=ALU.mult),
                   reads=[pst[bO], t_sc4], writes=[t_ob])
                if hh == 1:
                    dma("sp", src_o[i * 128:(i + 1) * 128, 256:512], ob_ap, r=[t_ob], w=[t_o_ch[i // 8]])

            for i in range(32):
                for hh in range(2):
                    att_stageA(i, hh, it4)
                    while len(pend4) > 1:
                        pend4.pop(0)()
                    pend4.append(lambda i=i, hh=hh, it4=it4: att_stageB(i, hh, it4))
                    it4 += 1
            while pend4:
                pend4.pop(0)()
            if DEBUG:
                dq = nc.dram_tensor("dbg_qT", [2, 128, S], BF16, kind="ExternalOutput").ap()
                dk = nc.dram_tensor("dbg_kT", [128, S + 256], BF16, kind="ExternalOutput").ap()
                dv = nc.dram_tensor("dbg_vat", [128, 34, 128], BF16, kind="ExternalOutput").ap()
                P.final_waits.append(dma("sp", dq[0], qTa[0][0], r=[qTa[0][1]]))
                P.final_waits.append(dma("sp", dq[1], qTa[1][0], r=[qTa[1][1]]))
                P.final_waits.append(dma("sp", dk, kTa, r=[t_kTa]))
                P.final_waits.append(dma("sp", dv, vat, r=[t_vat]))
            A.release(m4)
            A.release(m3)
            A.top = m3b

            stop_here(4)
            m5 = A.mark()
            Sall = [A.alloc([64, 128], BF16) for _ in range(2)]
            Sst = [[A.alloc([128], F32) for _ in range(2)] for _ in range(2)]
            kbl = [[A.alloc([8, 128], BF16) for _ in range(2)] for _ in range(2)]
            vbl = [[A.alloc([8, 128], BF16) for _ in range(2)] for _ in range(2)]
            pbl = [[A.alloc([3, 512], BF16) for _ in range(2)] for _ in range(2)]
            vgb = [A.alloc([8, 512], BF16) for _ in range(2)]
            ATs = [[A.alloc([64], BF16) for _ in range(2)] for _ in range(2)]
            ss5s = [A.alloc([4], F32) for _ in range(2)]
            tmp5s = [A.alloc([128], F32) for _ in range(2)]
            junk5s = [A.alloc([128], BF16) for _ in range(2)]
            og5 = [A.alloc([2, 128], BF16) for _ in range(2)]
            junk5, t_junk5 = A.alloc([128], BF16)
            H = slice(0, 64)
            pend_o = []
            for hh in range(2):
                for dr in range(2):
                    OP("pool", lambda e, dr=dr: e.memset(Sst[dr][0][0], 0.0), writes=[Sst[dr][0][1]])
                pp1 = [0, 0]
                def load_p1(step, hh=hh):
                    for dr in range(2):
                        tb = step if dr == 0 else 7 - step
                        k_ap, t_k = kbl[dr][step % 2]
                        v_ap, t_v = vbl[dr][step % 2]
                        dma("sp", k_ap[H], koutM[hh, dr, tb * 512:(tb + 1) * 512, :].rearrange("(n p) d -> p n d", p=64),
                            r=[t_dram["koutM"]], w=[t_k])
                        dma("sp", v_ap[H], vg[tb * 512:(tb + 1) * 512, hh * 128:(hh + 1) * 128].rearrange("(n p) d -> p n d", p=64),
                            r=[t_dram["vg"]], w=[t_v])
                load_p1(0)
                for step in range(8):
                    if step + 1 < 8:
                        load_p1(step + 1)
                    for cstep in range(8):
                        for dr in range(2):
                            tb = step if dr == 0 else 7 - step
                            cc = cstep if dr == 0 else 7 - cstep
                            n = tb * 8 + cc
                            k_ap, t_k = kbl[dr][step % 2]
                            v_ap, t_v = vbl[dr][step % 2]
                            S_ap, t_S = Sst[dr][pp1[dr] % 2]
                            Sn_ap, t_Sn = Sst[dr][(pp1[dr] + 1) % 2]
                            pp1[dr] += 1
                            Sa_ap, t_Sa = Sall[dr]
                            dec_ap, t_dec = dec[hh][dr]
                            bk = dr * 2 + (cstep % 2)
                            OP("act", lambda e, Sa_ap=Sa_ap, S_ap=S_ap, n=n: e.copy(out=Sa_ap[:, n, :], in_=S_ap), reads=[t_S], writes=[t_Sa])
                            OP("pe", lambda e, k_ap=k_ap, v_ap=v_ap, cc=cc, bk=bk: e.matmul(psf(bk)[:, 0:128], lhsT=k_ap[H, cc, :],
                                                                                             rhs=v_ap[H, cc, :], start=True, stop=True),
                               reads=[t_k, t_v], writes=[pst[bk]])
                            OP("dve", lambda e, S_ap=S_ap, Sn_ap=Sn_ap, dec_ap=dec_ap, n=n, bk=bk: e.scalar_tensor_tensor(out=Sn_ap, in0=S_ap, scalar=dec_ap[:, n:n + 1],
                                                                                                                           in1=psf(bk)[:, 0:128], op0=ALU.mult, op1=ALU.add),
                               reads=[t_S, t_dec, pst[bk]], writes=[t_Sn])
                def load_p2(tb, hh=hh):
                    pf_ap, t_pf = pbl[0][tb % 2]
                    pb_ap, t_pb = pbl[1][tb % 2]
                    vg_ap, t_vgb = vgb[tb % 2]
                    tsl = slice(tb * 512, (tb + 1) * 512)
                    dma("sp", pf_ap, hgT[hh, 0].rearrange("a p t -> p a t")[:, :, tsl], r=[t_dram["hgT"]], w=[t_pf])
                    dma("sp", pb_ap, hgT[hh, 1].rearrange("a p t -> p a t")[:, :, tsl], r=[t_dram["hgT"]], w=[t_pb])
                    dma("sp", vg_ap[H], vg[tsl, :].rearrange("(n p) d -> p n d", p=64), r=[t_dram["vg"]], w=[t_vgb])
                load_p2(0)
                for tb in range(8):
                    if tb + 1 < 8:
                        load_p2(tb + 1)
                    pf_ap, t_pf = pbl[0][tb % 2]
                    pb_ap, t_pb = pbl[1][tb % 2]
                    vg_ap, t_vgb = vgb[tb % 2]
                    tsl = slice(tb * 512, (tb + 1) * 512)
                    def p5A(i, c, pf_ap=pf_ap, t_pf=t_pf, pb_ap=pb_ap, t_pb=t_pb):
                        cl = i * 2 + c
                        cc_ = slice(cl * 64, (cl + 1) * 64)
                        atf, t_atf = ATs[0][c]
                        atb, t_atb = ATs[1][c]
                        bA = c * 2
                        OP("pe", lambda e, pf_ap=pf_ap, cc_=cc_, bA=bA: e.matmul(psf(bA)[H, 0:64], lhsT=pf_ap[:, 1, cc_], rhs=pf_ap[:, 0, cc_], start=True, stop=True),
                           reads=[t_pf], writes=[pst[bA]])
                        OP("dve", lambda e, atf=atf, bA=bA: e.tensor_tensor(out=atf[H], in0=psf(bA)[H, 0:64], in1=maskF[H, 0:64], op=ALU.mult),
                           reads=[pst[bA], t_maskF], writes=[t_atf])
                        OP("pe", lambda e, pb_ap=pb_ap, cc_=cc_, bA=bA: e.matmul(psf(bA + 1)[H, 0:64], lhsT=pb_ap[:, 1, cc_], rhs=pb_ap[:, 0, cc_], start=True, stop=True),
                           reads=[t_pb], writes=[pst[bA + 1]])
                        OP("dve", lambda e, atb=atb, bA=bA: e.tensor_tensor(out=atb[H], in0=psf(bA + 1)[H, 0:64], in1=maskB[H, 0:64], op=ALU.mult),
                           reads=[pst[bA + 1], t_maskB], writes=[t_atb])

                    def p5B(i, c, pf_ap=pf_ap, t_pf=t_pf, pb_ap=pb_ap, t_pb=t_pb, vg_ap=vg_ap, t_vgb=t_vgb, tb=tb, hh=hh):
                        gt = tb * 4 + i
                        og_ap, t_og = og5[i % 2]
                        bO = 4 + (i % 2)
                        cl = i * 2 + c
                        n = tb * 8 + cl
                        cc_ = slice(cl * 64, (cl + 1) * 64)
                        atf, t_atf = ATs[0][c]
                        atb, t_atb = ATs[1][c]
                        ss5, t_ss5 = ss5s[c]
                        tmp5, t_tmp5 = tmp5s[c]
                        junk5, t_junk5 = junk5s[c]
                        oc = psf(bO)[H, c * 128:(c + 1) * 128]
                        vv = vg_ap[H, cl, hh * 128:(hh + 1) * 128]
                        OP("pe", lambda e, atf=atf, vv=vv, oc=oc: e.matmul(oc, lhsT=atf[H], rhs=vv, start=True, stop=False),
                           reads=[t_atf, t_vgb], writes=[pst[bO]])
                        OP("pe", lambda e, atb=atb, vv=vv, oc=oc: e.matmul(oc, lhsT=atb[H], rhs=vv, start=False, stop=False),
                           reads=[t_atb, t_vgb], writes=[pst[bO]])
                        OP("pe", lambda e, pf_ap=pf_ap, cc_=cc_, n=n, oc=oc: e.matmul(oc, lhsT=pf_ap[:, 2, cc_], rhs=Sall[0][0][:, n, :], start=False, stop=False),
                           reads=[t_pf, Sall[0][1]], writes=[pst[bO]])
                        OP("pe", lambda e, pb_ap=pb_ap, cc_=cc_, n=n, oc=oc: e.matmul(oc, lhsT=pb_ap[:, 2, cc_], rhs=Sall[1][0][:, n, :], start=False, stop=True),
                           reads=[t_pb, Sall[1][1]], writes=[pst[bO]])
                        OP("act", lambda e, oc=oc, c=c, junk5=junk5, ss5=ss5: e.activation(out=junk5[H], in_=oc, func=AF.Square, accum_out=ss5[H, c:c + 1]),
                           reads=[pst[bO]], writes=[t_junk5, t_ss5])
                        rms_rstd(None, ss5[H, c:c + 1], t_ss5, ss5[H, 2 + c:3 + c], t_ss5, 128)
                        OP("dve", lambda e, oc=oc, hh=hh, c=c, tmp5=tmp5, ss5=ss5: e.scalar_tensor_tensor(out=tmp5[H], in0=oc, scalar=ss5[H, 2 + c:3 + c],
                                                                                                      in1=hgnb[H, hh * 128:(hh + 1) * 128], op0=ALU.mult, op1=ALU.mult),
                           reads=[pst[bO], t_ss5, t_hgnb], writes=[t_tmp5])
                        OP("pool", lambda e, og_ap=og_ap, vg_ap=vg_ap, cl=cl, hh=hh, c=c, tmp5=tmp5: e.tensor_tensor(out=og_ap[H, c, :], in0=tmp5[H],
                                                                                                                    in1=vg_ap[H, cl, 256 + hh * 128:256 + (hh + 1) * 128], op=ALU.mult),
                           reads=[t_tmp5, t_vgb], writes=[t_og])
                        if c == 1:
                            dma("sp", src_o[gt * 128:(gt + 1) * 128, hh * 128:(hh + 1) * 128].rearrange("(c p) d -> p c d", p=64), og_ap[H],
                                r=[t_og], w=[t_o_ch[gt // 8]])

                    chunks = [(i, c) for i in range(4) for c in range(2)]
                    p5A(*chunks[0])
                    for n5, (i, c) in enumerate(chunks):
                        if n5 + 1 < len(chunks):
                            p5A(*chunks[n5 + 1])
                        p5B(i, c)
                    if hh == 1 and tb % 2 == 1:
                        pend_o.append(ag_start(src_o, 1024, tb // 2, t_o_ch[tb // 2], dst=dst_o))
            A.release(m5)
            A.top = m3
            t_dsto = [t for (_, t) in pend_o]

            stop_here(5)
            m7 = A.mark()
            affown, t_affown = A.alloc([8, 16], F32)
            affT, t_affT = A.alloc([1024], F32)
            posT, t_posT = A.alloc([8, 16], F32)
            selm, t_selm = A.alloc([8, 16], F32)
            sel2, t_sel2 = A.alloc([8, 16], F32)
            affm, t_affm = A.alloc([8, 16], F32)
            idxi, t_idxi = A.alloc([8, 16], I32)
            pmT, t_pmT = A.alloc([32, 4], F32)
            idxg, t_idxg = A.alloc([4, 4], I32)
            idxf, t_idxf = A.alloc([4, 4], F32)
            m7p = A.mark()
            mergedT, t_mergedT = A.alloc([KC, 1024], BF16)
            m7b = A.mark()
            hTo, t_hTo = A.alloc([KC, 1024], BF16)
            oaT, t_oaT = A.alloc([8, 1024], BF16)
            obT, t_obT = A.alloc([8, 1024], BF16)
            for ci in range(4):
                dma("sp", hTo[:, :, ci * 256:(ci + 1) * 256], src_hT[ci * 2048:(ci + 1) * 2048, :].rearrange("(k p) t -> p k t", p=128),
                    r=[t_dram["src_hT"]], w=[t_hTo])
            stop_here(50)
            ots = [A.alloc([4, 512], BF16) for _ in range(2)]
            osrc = dst_o.rearrange("(r t) c -> t r c", r=4)
            for j in range(8):
                ot, t_ot = ots[j % 2]

                for r in range(4):
                    OP("pool", lambda e, ot=ot, j=j, r=r: e.indirect_dma_start(
                        out=ot[:, r, :], out_offset=None, in_=dst_o,
                        in_offset=bass.IndirectOffsetOnAxis(ap=cidx[:, j * 4 + r:j * 4 + r + 1], axis=0)),
                       reads=t_dsto + [t_cidx], writes=[t_ot], dma=True)
                for half in range(2 if KVAR != 1 else 0):
                    bk = 6 + half
                    for rr in range(2):
                        r = half * 2 + rr
                        for w4 in range(4):
                            OP("pe", lambda e, ot=ot, r=r, w4=w4, rr=rr, bk=bk: e.transpose(out=psbf(bk)[:, (rr * 4 + w4) * 128:(rr * 4 + w4 + 1) * 128],
                                                                                           in_=ot[:, r, w4 * 128:(w4 + 1) * 128], identity=identb),
                               reads=[t_ot, t_identb], writes=[pst[bk]])
                    pv = psbf(bk).rearrange("p (a b) -> p a b", b=128)
                    for rr in range(2):
                        r = half * 2 + rr
                        OP("act", lambda e, pv=pv, r=r, rr=rr, j=j: e.copy(out=oaT[:, 2 * r:2 * r + 2, j * 128:(j + 1) * 128], in_=pv[:, rr * 4:rr * 4 + 2, :]),
                           reads=[pst[bk]], writes=[t_oaT])
                        OP("act", lambda e, pv=pv, r=r, rr=rr, j=j: e.copy(out=obT[:, 2 * r:2 * r + 2, j * 128:(j + 1) * 128], in_=pv[:, rr * 4 + 2:rr * 4 + 4, :]),
                           reads=[pst[bk]], writes=[t_obT])
            stop_here(51)
            Wsets = [dict(Wa=A.alloc([8, 256], BF16), Wb=A.alloc([8, 256], BF16), Wga=A.alloc([KC, 256], BF16), Wgb=A.alloc([KC, 256], BF16))
                     for _ in range(2)]
            sgas = [A.alloc([512], F32) for _ in range(2)]
            sgbs = [A.alloc([512], F32) for _ in range(2)]
            wba_v = w_ba.rearrange("(k p) n -> p k n", p=128)
            wbb_v = w_bb.rearrange("(k p) n -> p k n", p=128)
            wgab_v = w_gab.rearrange("(k p) n -> p k n", p=128)

            def load_w7(c8):
                ws = Wsets[c8 % 2]
                csl = slice(c8 * 256, (c8 + 1) * 256)
                dma("pool", ws["Wa"][0], wba_v[:, :, csl], w=[ws["Wa"][1]])
                dma("pool", ws["Wb"][0], wbb_v[:, :, csl], w=[ws["Wb"][1]])
                dma("pool", ws["Wga"][0], wgab_v[:, :, csl], w=[ws["Wga"][1]])
                dma("pool", ws["Wgb"][0], wgab_v[:, :, 2048 + c8 * 256:2048 + (c8 + 1) * 256], w=[ws["Wgb"][1]])
            load_w7(0)
            it7 = 0
            for c8 in range(8):
                if c8 + 1 < 8:
                    load_w7(c8 + 1)
                ws = Wsets[c8 % 2]
                (Wa, t_Wa), (Wb, t_Wb), (Wga, t_Wga), (Wgb, t_Wgb) = ws["Wa"], ws["Wb"], ws["Wga"], ws["Wgb"]
                for th in range(2):
                    tsl = slice(th * 512, (th + 1) * 512)
                    for ci in range(2):
                        cb = c8 * 2 + ci
                        wsl = slice(ci * 128, (ci + 1) * 128)
                        pb0 = (it7 % 2) * 4
                        sga, t_sga = sgas[it7 % 2]
                        sgb, t_sgb = sgbs[it7 % 2]
                        it7 += 1
                        for ch in range(8):
                            OP("pe", lambda e, ch=ch, wsl=wsl, tsl=tsl, Wa=Wa, pb0=pb0: e.matmul(psf(pb0), lhsT=Wa[:, ch, wsl], rhs=oaT[:, ch, tsl], start=(ch == 0), stop=(ch == 7)),
                               reads=[t_Wa, t_oaT], writes=[pst[pb0]])
                        for ch in range(8):
                            OP("pe", lambda e, ch=ch, wsl=wsl, tsl=tsl, Wb=Wb, pb0=pb0: e.matmul(psf(pb0 + 1), lhsT=Wb[:, ch, wsl], rhs=obT[:, ch, tsl], start=(ch == 0), stop=(ch == 7)),
                               reads=[t_Wb, t_obT], writes=[pst[pb0 + 1]])
                        for kc in range(KC):
                            OP("pe", lambda e, kc=kc, wsl=wsl, tsl=tsl, Wga=Wga, pb0=pb0: e.matmul(psf(pb0 + 2), lhsT=Wga[:, kc, wsl], rhs=hTo[:, kc, tsl], start=(kc == 0), stop=(kc == KC - 1)),
                               reads=[t_Wga, t_hTo], writes=[pst[pb0 + 2]])
                        for kc in range(KC):
                            OP("pe", lambda e, kc=kc, wsl=wsl, tsl=tsl, Wgb=Wgb, pb0=pb0: e.matmul(psf(pb0 + 3), lhsT=Wgb[:, kc, wsl], rhs=hTo[:, kc, tsl], start=(kc == 0), stop=(kc == KC - 1)),
                               reads=[t_Wgb, t_hTo], writes=[pst[pb0 + 3]])
                        OP("act", lambda e, sga=sga, pb0=pb0: e.activation(out=sga, in_=psf(pb0 + 2), func=AF.Sigmoid), reads=[pst[pb0 + 2]], writes=[t_sga])
                        OP("act", lambda e, sgb=sgb, pb0=pb0: e.activation(out=sgb, in_=psf(pb0 + 3), func=AF.Sigmoid), reads=[pst[pb0 + 3]], writes=[t_sgb])
                        OP("dve", lambda e, sga=sga, pb0=pb0: e.tensor_tensor(out=sga, in0=sga, in1=psf(pb0), op=ALU.mult), reads=[t_sga, pst[pb0]], writes=[t_sga])
                        OP("dve", lambda e, sgb=sgb, pb0=pb0: e.tensor_tensor(out=sgb, in0=sgb, in1=psf(pb0 + 1), op=ALU.mult), reads=[t_sgb, pst[pb0 + 1]], writes=[t_sgb])
                        OP("pool", lambda e, cb=cb, tsl=tsl, sga=sga, sgb=sgb: e.tensor_tensor(out=mergedT[:, cb, tsl], in0=sga, in1=sgb, op=ALU.add),
                           reads=[t_sga, t_sgb], writes=[t_mergedT])
            stop_here(52)
            A.release(m7b)
            yall, t_yall = A.alloc([8, D], F32)
            m7a = A.mark()
            Wos = [A.alloc([KC, 512], BF16) for _ in range(2)]
            wo_v = w_o.rearrange("(k p) n -> p k n", p=128)
            for cg in range(4):
                Wo, t_Wo = Wos[cg % 2]
                dma("pool", Wo, wo_v[:, :, cg * 512:(cg + 1) * 512], w=[t_Wo])
                for j in range(8):
                    bk = j % 4
                    for mc in range(KC):
                        OP("pe", lambda e, mc=mc, j=j, Wo=Wo, bk=bk: e.matmul(psf(bk), lhsT=mergedT[:, mc, j * 128:(j + 1) * 128], rhs=Wo[:, mc, :],
                                                                              start=(mc == 0), stop=(mc == KC - 1)),
                           reads=[t_mergedT, t_Wo], writes=[pst[bk]])
                    OP("act", lambda e, j=j, cg=cg, bk=bk: e.copy(out=yall[:, j, cg * 512:(cg + 1) * 512], in_=psf(bk)), reads=[pst[bk]], writes=[t_yall])

            A.release(m7a)
            stop_here(7)
            G1, t_G1 = A.alloc([D], F32)
            A2, t_A2 = A.alloc([D], F32)
            B2, t_B2 = A.alloc([D], F32)
            load_modvec(G1, t_G1, 2, 1, "g")
            load_modvec(A2, t_A2, 4, 2, "a")
            load_modvec(B2, t_B2, 3, 2, "b")
            Wr, t_Wr = A.alloc([KC, 16], F32)
            dma("sp", Wr, w_r.rearrange("(k p) n -> p k n", p=128), w=[t_Wr])
            def alias8(k):
                v = mergedT[:, 4 * k:4 * k + 4, :].rearrange("p a b -> p (a b)").bitcast(F32)
                tk = Tok()
                tk.rd = ([t_mergedT.lw] if t_mergedT.lw is not None else []) + list(t_mergedT.rd)
                return v, tk
            xt8s = [A.alloc([D], F32), alias8(0)]
            x1ts = [A.alloc([D], F32), alias8(1)]
            h2fs = [A.alloc([D], F32), alias8(2)]
            h2bs = [A.alloc([D], BF16) for _ in range(2)]
            _h2T0 = A.alloc([KC, 128], F32)
            _v, _tk = alias8(3)
            h2Ts = [_h2T0, (_v.rearrange("p (a b) -> p a b", b=128), _tk)]
            junk8, t_junk8 = A.alloc([D], BF16)
            s8s = [A.alloc([8], F32) for _ in range(2)]
            e16s = [A.alloc([16], F32) for _ in range(2)]
            pend_h2 = []
            for j in range(8):
                (xt8, t_xt8), (x1t, t_x1t), (h2f, t_h2f), (h2b, t_h2b), (h2T, t_h2T) = xt8s[j % 2], x1ts[j % 2], h2fs[j % 2], h2bs[j % 2], h2Ts[j % 2]
                s8, t_s8 = s8s[j % 2]
                e16, t_e16 = e16s[j % 2]
                dma("sp", xt8, x_own[j * 128:(j + 1) * 128, :], w=[t_xt8])
                OP("act", lambda e, j=j: e.activation(out=junk8, in_=yall[:, j, :], func=AF.Square, accum_out=s8[:, 0:1]),
                   reads=[t_yall], writes=[t_junk8, t_s8])
                rms_rstd(None, s8[:, 0:1], t_s8, s8[:, 1:2], t_s8, D)
                OP("dve", lambda e, j=j: e.scalar_tensor_tensor(out=x1t, in0=yall[:, j, :], scalar=s8[:, 1:2], in1=G1, op0=ALU.mult, op1=ALU.mult),
                   reads=[t_yall, t_s8, t_G1], writes=[t_x1t])
                OP("pool", lambda e: e.tensor_tensor(out=x1t, in0=x1t, in1=xt8, op=ALU.add), reads=[t_x1t, t_xt8], writes=[t_x1t])
                dma("sp", x1_d[j * 128:(j + 1) * 128, :], x1t, r=[t_x1t], w=[t_dram["x1_d"]])
                OP("act", lambda e: e.activation(out=junk8, in_=x1t, func=AF.Square, accum_out=s8[:, 2:3]), reads=[t_x1t], writes=[t_junk8, t_s8])
                rms_rstd(None, s8[:, 2:3], t_s8, s8[:, 3:4], t_s8, D)
                OP("dve", lambda e: e.scalar_tensor_tensor(out=h2f, in0=x1t, scalar=s8[:, 3:4], in1=A2, op0=ALU.mult, op1=ALU.mult),
                   reads=[t_x1t, t_s8, t_A2], writes=[t_h2f])
                OP("dve", lambda e: e.tensor_tensor(out=h2f, in0=h2f, in1=B2, op=ALU.add), reads=[t_h2f, t_B2], writes=[t_h2f])
                OP("act", lambda e: e.copy(out=h2b, in_=h2f), reads=[t_h2f], writes=[t_h2b])
                dma("sp", src_h2[j * 128:(j + 1) * 128, :], h2b, r=[t_h2b], w=[t_h2_ch[j // 2]])
                if j % 2 == 1:
                    pend_h2.append(ag_start(src_h2, 256, j // 2, t_h2_ch[j // 2], dst=dst_h2))
                for g in range(4):
                    bk = g
                    for k4 in range(4):
                        kc = g * 4 + k4
                        OP("pe", lambda e, kc=kc, k4=k4, bk=bk: e.transpose(out=psf(bk)[:, k4 * 128:(k4 + 1) * 128], in_=h2f[:, kc * 128:(kc + 1) * 128], identity=identf),
                           reads=[t_h2f, t_identf], writes=[pst[bk]])
                    OP("act", lambda e, g=g, bk=bk: e.copy(out=h2T[:, g * 4:g * 4 + 4, :], in_=psf(bk).rearrange("p (a b) -> p a b", b=128)),
                       reads=[pst[bk]], writes=[t_h2T])
                for kc in range(KC):
                    OP("pe", lambda e, kc=kc: e.matmul(psf(4)[:, 0:16], lhsT=h2T[:, kc, :], rhs=Wr[:, kc, :], start=(kc == 0), stop=(kc == KC - 1)),
                       reads=[t_h2T, t_Wr], writes=[pst[4]])
                OP("dve", lambda e: e.tensor_reduce(out=s8[:, 4:5], in_=psf(4)[:, 0:16], axis=AX.X, op=ALU.max), reads=[pst[4]], writes=[t_s8])
                OP("dve", lambda e: e.tensor_scalar(out=s8[:, 5:6], in0=s8[:, 4:5], scalar1=-1.0, scalar2=None, op0=ALU.mult), reads=[t_s8], writes=[t_s8])
                OP("act", lambda e: e.activation(out=e16, in_=psf(4)[:, 0:16], func=AF.Exp, bias=s8[:, 5:6], scale=1.0, accum_out=s8[:, 6:7]),
                   reads=[pst[4], t_s8], writes=[t_e16, t_s8])
                OP("dve", lambda e: e.reciprocal(out=s8[:, 7:8], in_=s8[:, 6:7]), reads=[t_s8], writes=[t_s8])
                OP("dve", lambda e, j=j: e.tensor_scalar(out=affown[:, j, :], in0=e16, scalar1=s8[:, 7:8], scalar2=None, op0=ALU.mult),
                   reads=[t_e16, t_s8], writes=[t_affown])
                OP("pe", lambda e, j=j: e.transpose(out=psf(5)[0:16, 0:128], in_=affown[:, j, :], identity=identf),
                   reads=[t_affown, t_identf], writes=[pst[5]])
                OP("act", lambda e, j=j: e.copy(out=affT[0:16, j * 128:(j + 1) * 128], in_=psf(5)[0:16, 0:128]), reads=[pst[5]], writes=[t_affT])
            dma("sp", src_aff, affT[0:16, :], r=[t_affT], w=[t_dram["src_aff"]])
            allgather(src_aff, dst_aff, G4, t_dram["src_aff"], t_dram["dst_aff"])
            t_dsth2 = [t for (_, t) in pend_h2]
            A.release(m7p)
            Wg_, t_Wg = A.alloc([KC, 1024], BF16)
            Wu_, t_Wu = A.alloc([KC, 1024], BF16)
            Wd_, t_Wd = A.alloc([8, D], BF16)

            def load_expert(k):
                wg_v = wg[k].rearrange("(k p) n -> p k n", p=128)
                wu_v = wu[k].rearrange("(k p) n -> p k n", p=128)
                wd_v = wd[k].rearrange("(k p) n -> p k n", p=128)
                for g in range(2):
                    dma("pool", Wg_[:, 8 * g:8 * g + 8, :], wg_v[:, 8 * g:8 * g + 8, :], w=[t_Wg])
                    dma("pool", Wu_[:, 8 * g:8 * g + 8, :], wu_v[:, 8 * g:8 * g + 8, :], w=[t_Wu])
                for g in range(2):
                    dma("pool", Wd_[:, 4 * g:4 * g + 4, :], wd_v[:, 4 * g:4 * g + 4, :], w=[t_Wd])
            load_expert(0)
            m8keep = A.mark()

            stop_here(8)
            affR, t_affR = A.alloc([S], F32)
            junk9, t_junk9 = A.alloc([S], F32)
            maskR, t_maskR = A.alloc([S], F32)
            r9, t_r9 = A.alloc([8], F32)
            av_ = dst_aff.rearrange("(q e) t -> e q t", q=4)
            dma("sp", affR[0:16, :].rearrange("p (q t) -> p q t", q=4), av_, r=[t_dram["dst_aff"]], w=[t_affR])
            R32 = slice(0, 16)
            OP("dve", lambda e: e.memset(r9[R32, :], 0.5), writes=[t_r9])
            NIT = 24
            for k in range(NIT):
                wk = 2.0 ** -(k + 1)
                OP("dve", lambda e: e.tensor_scalar(out=junk9[R32, :], in0=affR[R32, :], scalar1=r9[R32, 1:2], scalar2=0.0, op0=ALU.is_gt, op1=ALU.add,
                                                    accum_out=r9[R32, 2:3]),
                   reads=[t_affR, t_r9], writes=[t_junk9, t_r9])
                OP("dve", lambda e, wk=wk: e.tensor_scalar(out=r9[R32, 3:4], in0=r9[R32, 2:3], scalar1=511.5, scalar2=wk, op0=ALU.is_gt, op1=ALU.mult),
                   reads=[t_r9], writes=[t_r9])
                OP("dve", lambda e, wk=wk: e.scalar_tensor_tensor(out=r9[R32, 1:2], in0=r9[R32, 3:4], scalar=-0.5 * wk, in1=r9[R32, 1:2], op0=ALU.add, op1=ALU.add),
                   reads=[t_r9], writes=[t_r9])
            OP("dve", lambda e: e.tensor_scalar(out=r9[R32, 0:1], in0=r9[R32, 1:2], scalar1=-(2.0 ** -(NIT + 1)), scalar2=None, op0=ALU.add),
               reads=[t_r9], writes=[t_r9])
            OP("dve", lambda e: e.tensor_scalar(out=maskR[R32, :], in0=affR[R32, :], scalar1=r9[R32, 0:1], scalar2=None, op0=ALU.is_gt),
               reads=[t_affR, t_r9], writes=[t_maskR])
            OP("pool", lambda e: e.memset(junk9[R32, :], 1.0), writes=[t_junk9])
            OP("dve", lambda e: e.tensor_tensor_scan(out=affR[R32, :], data0=junk9[R32, :], data1=maskR[R32, :], initial=0.0, op0=ALU.mult, op1=ALU.add),
               reads=[t_junk9, t_maskR], writes=[t_affR])
            OP("dve", lambda e: e.tensor_tensor(out=affR[R32, :], in0=affR[R32, :], in1=maskR[R32, :], op=ALU.mult), reads=[t_affR, t_maskR], writes=[t_affR])
            OP("dve", lambda e: e.tensor_scalar(out=affR[R32, :], in0=affR[R32, :], scalar1=-1.0, scalar2=None, op0=ALU.add), reads=[t_affR], writes=[t_affR])
            dma("sp", route_d, affR[R32, :], r=[t_affR], w=[t_dram["route_d"]])
            t_rd2 = Tok()
            for qr in range(4):
                dma("sp", route_d2[qr * 16:(qr + 1) * 16, :], affR[R32, qr * 1024:(qr + 1) * 1024], r=[t_affR], w=[t_rd2])
            A.release(m8keep)
            slab, t_slab = A.alloc([1024], F32)

            OP("pool", lambda e: e.indirect_dma_start(
                out=slab[0:16, :], out_offset=None, in_=route_d2,
                in_offset=bass.IndirectOffsetOnAxis(ap=cidx[0:16, 33:34], axis=0)),
               reads=[t_rd2, t_cidx], writes=[t_slab], dma=True)
            for j in range(8):
                OP("pe", lambda e, j=j: e.transpose(out=psf(0)[:, j * 16:(j + 1) * 16], in_=slab[0:16, j * 128:(j + 1) * 128], identity=identf[0:16, 0:16]),
                   reads=[t_slab, t_identf], writes=[pst[0]])
            OP("act", lambda e: e.copy(out=posT, in_=psf(0)[:, 0:128].rearrange("p (a b) -> p a b", b=16)), reads=[pst[0]], writes=[t_posT])
            OP("dve", lambda e: e.tensor_scalar(out=selm, in0=posT, scalar1=-0.5, scalar2=None, op0=ALU.is_gt), reads=[t_posT], writes=[t_selm])
            OP("dve", lambda e: e.tensor_scalar(out=sel2, in0=posT, scalar1=511.5, scalar2=None, op0=ALU.is_lt), reads=[t_posT], writes=[t_sel2])
            OP("dve", lambda e: e.tensor_tensor(out=selm, in0=selm, in1=sel2, op=ALU.mult), reads=[t_selm, t_sel2], writes=[t_selm])
            OP("dve", lambda e: e.tensor_tensor(out=affm, in0=affown, in1=selm, op=ALU.mult), reads=[t_affown, t_selm], writes=[t_affm])
            OP("dve", lambda e: e.tensor_scalar(out=sel2, in0=posT, scalar1=255.5, scalar2=768.0, op0=ALU.is_gt, op1=ALU.mult), reads=[t_posT], writes=[t_sel2])
            OP("dve", lambda e: e.tensor_tensor(out=posT, in0=posT, in1=sel2, op=ALU.add), reads=[t_posT, t_sel2], writes=[t_posT])
            OP("dve", lambda e: e.tensor_tensor(out=posT, in0=posT, in1=rowbase.unsqueeze(1).to_broadcast([128, 8, 16]), op=ALU.add),
               reads=[t_posT, t_rowbase], writes=[t_posT])
            OP("dve", lambda e: e.tensor_tensor(out=posT, in0=posT, in1=selm, op=ALU.mult), reads=[t_posT, t_selm], writes=[t_posT])
            OP("dve", lambda e: e.tensor_copy(out=idxi, in_=posT), reads=[t_posT], writes=[t_idxi])
            pm, t_pm = A.alloc([S], F32)
            ohall, t_ohall = A.alloc([32, 512], BF16)
            OP("pool", lambda e: e.indirect_dma_start(
                out=pm[0:4, :], out_offset=None, in_=route_d,
                in_offset=bass.IndirectOffsetOnAxis(ap=cidx[0:4, 32:33], axis=0)),
               reads=[t_dram["route_d"], t_cidx], writes=[t_pm], dma=True)
            for tt in range(32):
                OP("pe", lambda e, tt=tt: e.transpose(out=psf(1)[:, tt * 4:(tt + 1) * 4], in_=pm[0:4, tt * 128:(tt + 1) * 128], identity=identf[0:4, 0:4]),
                   reads=[t_pm, t_identf], writes=[pst[1]])
            OP("act", lambda e: e.copy(out=pmT, in_=psf(1)[:, 0:128].rearrange("p (a b) -> p a b", b=4)), reads=[pst[1]], writes=[t_pmT])
            psidx = psf(2)[:, 0:32].rearrange("p (a b c) -> p a b c", b=4, c=2)
            for pp in range(4):
                for tt in range(32):
                    OP("dve", lambda e, tt=tt, pp=pp: e.tensor_scalar(out=ohall[:, tt, :], in0=iota, scalar1=pmT[:, tt, pp:pp + 1], scalar2=None, op0=ALU.is_equal),
                       reads=[t_iota, t_pmT], writes=[t_ohall])
                for s4 in range(4):
                    for tt in range(32):
                        OP("pe", lambda e, tt=tt, pp=pp, s4=s4: e.matmul(psidx[:, pp, s4, :], lhsT=ohall[:, tt, s4 * 128:(s4 + 1) * 128], rhs=tval[:, tt, :],
                                                                         start=(tt == 0), stop=(tt == 31)),
                           reads=[t_ohall, t_tval], writes=[pst[2]])
            idx2, t_idx2 = A.alloc([4, 4, 2], F32)
            OP("act", lambda e: e.copy(out=idx2, in_=psidx), reads=[pst[2]], writes=[t_idx2])
            OP("dve", lambda e: e.scalar_tensor_tensor(out=idxf, in0=idx2[:, :, :, 0], scalar=64.0, in1=idx2[:, :, :, 1], op0=ALU.mult, op1=ALU.add),
               reads=[t_idx2], writes=[t_idxf])
            OP("dve", lambda e: e.tensor_copy(out=idxg, in_=idxf), reads=[t_idxf], writes=[t_idxg])

            stop_here(9)
            A.release(m8keep)
            m10 = A.mark()
            xgs = [A.alloc([D], BF16) for _ in range(2)]
            xeTs = [A.alloc([KC, 512], BF16) for _ in range(2)]
            aT, t_aT = A.alloc([8, 512], BF16)
            sgf, t_sgf = A.alloc([512], F32)
            ysb = [A.alloc([D], BF16) for _ in range(2)]
            pend_Y = []
            dvY = dst_Y.rearrange("(r c m) x -> c r m x", r=4, c=8)

            def load_gu(k):
                wg_v = wg[k].rearrange("(k p) n -> p k n", p=128)
                wu_v = wu[k].rearrange("(k p) n -> p k n", p=128)
                for g in range(2):
                    dma("pool", Wg_[:, 8 * g:8 * g + 8, :], wg_v[:, 8 * g:8 * g + 8, :], w=[t_Wg])
                    dma("pool", Wu_[:, 8 * g:8 * g + 8, :], wu_v[:, 8 * g:8 * g + 8, :], w=[t_Wu])

            def load_d(k):
                wd_v = wd[k].rearrange("(k p) n -> p k n", p=128)
                for g in range(2):
                    dma("pool", Wd_[:, 4 * g:4 * g + 4, :], wd_v[:, 4 * g:4 * g + 4, :], w=[t_Wd])

            def gather_x(k):
                xeT, t_xeT = xeTs[k % 2]
                for s4 in range(4):
                    xg, t_xg = xgs[s4 % 2]
                    OP("pool", lambda e, xg=xg, k=k, s4=s4: e.indirect_dma_start(
                        out=xg, out_offset=None, in_=dst_h2,
                        in_offset=bass.IndirectOffsetOnAxis(ap=idxg[:, k, s4:s4 + 1], axis=0),
                        ),
                       reads=t_dsth2 + [t_idxg], writes=[t_xg], dma=True)
                    for half in range(2):
                        bk = 6 + half
                        for k8 in range(8):
                            kc = half * 8 + k8
                            OP("pe", lambda e, xg=xg, kc=kc, k8=k8, bk=bk: e.transpose(out=psbf(bk)[:, k8 * 128:(k8 + 1) * 128],
                                                                                       in_=xg[:, kc * 128:(kc + 1) * 128], identity=identb),
                               reads=[t_xg, t_identb], writes=[pst[bk]])
                        OP("act", lambda e, half=half, bk=bk, s4=s4, xeT=xeT: e.copy(out=xeT[:, half * 8:half * 8 + 8, s4 * 128:(s4 + 1) * 128],
                                                                                  in_=psbf(bk).rearrange("p (a b) -> p a b", b=128)),
                           reads=[pst[bk]], writes=[t_xeT])

            def gateup(k):
                xeT, t_xeT = xeTs[k % 2]
                for ft in range(8):
                    fsl = slice(ft * 128, (ft + 1) * 128)
                    bg = (ft % 2) * 2
                    for kc in range(KC):
                        OP("pe", lambda e, kc=kc, fsl=fsl, bg=bg, xeT=xeT: e.matmul(psf(bg), lhsT=Wg_[:, kc, fsl], rhs=xeT[:, kc, :], start=(kc == 0), stop=(kc == KC - 1)),
                           reads=[t_Wg, t_xeT], writes=[pst[bg]])
                    for kc in range(KC):
                        OP("pe", lambda e, kc=kc, fsl=fsl, bg=bg, xeT=xeT: e.matmul(psf(bg + 1), lhsT=Wu_[:, kc, fsl], rhs=xeT[:, kc, :], start=(kc == 0), stop=(kc == KC - 1)),
                           reads=[t_Wu, t_xeT], writes=[pst[bg + 1]])
                    OP("act", lambda e, bg=bg: e.activation(out=sgf, in_=psf(bg), func=AF.Silu), reads=[pst[bg]], writes=[t_sgf])
                    OP("dve", lambda e, bg=bg, ft=ft: e.tensor_tensor(out=aT[:, ft, :], in0=sgf, in1=psf(bg + 1), op=ALU.mult),
                       reads=[t_sgf, pst[bg + 1]], writes=[t_aT])

            def down(k):
                pp = k
                for s4 in range(4):
                    y_ap, t_y = ysb[s4 % 2]
                    for cg in range(4):
                        bk = 4 + (cg % 2)
                        for ft in range(8):
                            OP("pe", lambda e, ft=ft, s4=s4, cg=cg, bk=bk: e.matmul(psf(bk), lhsT=aT[:, ft, s4 * 128:(s4 + 1) * 128],
                                                                                    rhs=Wd_[:, ft, cg * 512:(cg + 1) * 512], start=(ft == 0), stop=(ft == 7)),
                               reads=[t_aT, t_Wd], writes=[pst[bk]])
                        OP("act", lambda e, y_ap=y_ap, cg=cg, bk=bk: e.copy(out=y_ap[:, cg * 512:(cg + 1) * 512], in_=psf(bk)), reads=[pst[bk]], writes=[t_y])
                    dma("sp", src_Y[pp * 512 + s4 * 128:pp * 512 + (s4 + 1) * 128, :], y_ap, r=[t_y], w=[t_Y_ch[pp * 2 + s4 // 2]])

            def finish_Y(k):
                for ci in (2 * k, 2 * k + 1):
                    tmp, ttmp = pend_Y[ci]
                    dma("sp", dvY[ci], tmp.rearrange("(r m) x -> r m x", r=4), r=[ttmp], w=[t_dram["dst_Y"]])

            gather_x(0)
            for k in range(4):
                gateup(k)
                if k + 1 < 4:
                    load_gu(k + 1)
                    gather_x(k + 1)
                down(k)
                if k + 1 < 4:
                    load_d(k + 1)
                for ci in (2 * k, 2 * k + 1):
                    pend_Y.append(ag_start(src_Y, 256, ci, t_Y_ch[ci], dst=dst_Y))
            t_dstY = [t for (_, t) in pend_Y]
            A.release(m10)

            stop_here(10)
            G2, t_G2 = A.alloc([D], F32)
            load_modvec(G2, t_G2, 5, 3, "g")
            gbs = [A.alloc([D], BF16) for _ in range(4)]
            accs = [A.alloc([D], F32) for _ in range(2)]
            x1rs = [A.alloc([D], F32) for _ in range(2)]
            dgs = [A.alloc([128], BF16) for _ in range(2)]
            junk11, t_junk11 = A.alloc([D], BF16)
            s11s = [A.alloc([4], F32) for _ in range(2)]
            for gb_, t_gb in gbs:
                OP("pool", lambda e, gb_=gb_: e.memset(gb_, 0.0), writes=[t_gb])
            gi = 0
            finals = []
            for j in range(8):
                acc, t_acc = accs[j % 2]
                x1r, t_x1r = x1rs[j % 2]
                s11, t_s11 = s11s[j % 2]
                pb0 = (j % 2) * 4
                dma("sp", x1r, x1_d[j * 128:(j + 1) * 128, :], r=[t_dram["x1_d"]], w=[t_x1r])
                for ex in range(16):
                    gb_, t_gb = gbs[gi % 4]
                    dg, t_dg = dgs[gi % 2]
                    gi += 1
                    OP("pool", lambda e, gb_=gb_, j=j, ex=ex: e.indirect_dma_start(
                        out=gb_, out_offset=None, in_=dst_Y,
                        in_offset=bass.IndirectOffsetOnAxis(ap=idxi[:, j, ex:ex + 1], axis=0),
                        ),
                       reads=t_dstY + [t_idxi, t_gb], writes=[t_gb], dma=True)
                    OP("dve", lambda e, dg=dg, j=j, ex=ex: e.tensor_scalar(out=dg, in0=identb, scalar1=affm[:, j, ex:ex + 1], scalar2=None, op0=ALU.mult),
                       reads=[t_identb, t_affm], writes=[t_dg])
                    for cg in range(4):
                        OP("pe", lambda e, dg=dg, gb_=gb_, cg=cg, ex=ex, pb0=pb0: e.matmul(psf(pb0 + cg), lhsT=dg, rhs=gb_[:, cg * 512:(cg + 1) * 512],
                                                                                          start=(ex == 0), stop=(ex == 15)),
                           reads=[t_dg, t_gb], writes=[pst[pb0 + cg]])
                for cg in range(4):
                    OP("act", lambda e, acc=acc, cg=cg, pb0=pb0: e.copy(out=acc[:, cg * 512:(cg + 1) * 512], in_=psf(pb0 + cg)), reads=[pst[pb0 + cg]], writes=[t_acc])
                OP("act", lambda e, acc=acc, s11=s11: e.activation(out=junk11, in_=acc, func=AF.Square, accum_out=s11[:, 0:1]), reads=[t_acc], writes=[t_junk11, t_s11])
                rms_rstd(None, s11[:, 0:1], t_s11, s11[:, 1:2], t_s11, D)
                OP("dve", lambda e, acc=acc, s11=s11: e.scalar_tensor_tensor(out=acc, in0=acc, scalar=s11[:, 1:2], in1=G2, op0=ALU.mult, op1=ALU.mult),
                   reads=[t_acc, t_s11, t_G2], writes=[t_acc])
                OP("dve", lambda e, acc=acc, x1r=x1r: e.tensor_tensor(out=acc, in0=acc, in1=x1r, op=ALU.add), reads=[t_acc, t_x1r], writes=[t_acc])
                finals.append(dma("sp", out[j * 128:(j + 1) * 128, :], acc, r=[t_acc]))
            P.final_waits.extend(finals)
        except _Stop:
            pass
        if DEBUG:
            for nm, src, shp, dt in (("dbg_o", src_o, [S, 512], BF16), ("dbg_x1", x1_d, [1024, D], F32),
                                     ("dbg_route", route_d, [16, S], F32), ("dbg_aff", dst_aff, [64, 1024], F32),
                                     ("dbg_mod", dst_mod, [4, 3072], F32), ("dbg_hT", src_hT, [4 * KC * 128, 256], BF16),
                                     ("dbg_Y", src_Y, [2048, D], BF16), ("dbg_h2", src_h2, [1024, D], BF16),
                                     ("dbg_vg", vg, [S, 512], BF16), ("dbg_hgT", hgT.rearrange("a b c p t -> (a b c p) t"), [12 * 128, S], BF16),
                                     ("dbg_koutM", koutM.rearrange("a b t d -> (a b t) d"), [4 * S, 128], BF16),
                                     ):
                dd_ = nc.dram_tensor(nm, shp, dt, kind="ExternalOutput").ap()
                key = {"dbg_o": "src_o", "dbg_x1": "x1_d", "dbg_route": "route_d", "dbg_aff": "dst_aff", "dbg_mod": "dst_mod",
                       "dbg_hT": "src_hT", "dbg_Y": "src_Y", "dbg_h2": "src_h2", "dbg_vg": "vg", "dbg_hgT": "hgT", "dbg_koutM": "koutM", "dbg_dsthT": "dst_hT"}[nm]
                P.final_waits.append(dma("sp", dd_, src, r=[t_dram[key]]))
        P.emit(nc, st)
    return nc


def _consts():
    bf = ml_dtypes.bfloat16
    c = {}
    c["c_identb"] = np.eye(128, dtype=np.float32).astype(bf)
    c["c_identf"] = np.eye(128, dtype=np.float32)
    m = np.arange(128)[:, None]
    l = np.arange(128)[None, :]
    same = (m // 64) == (l // 64)
    c["c_maskF"] = (same & (l >= m)).astype(np.float32)
    c["c_maskB"] = (same & (m >= l)).astype(np.float32)
    ps = np.zeros((128, 128), np.float32)
    for mm_ in range(128):
        ps[(mm_ + 64) % 128, mm_] = 1.0
    c["c_pswap"] = ps.astype(bf)
    half = 64
    inv = (10000.0 ** (-np.arange(half, dtype=np.float32) / half)).astype(np.float32)
    sm = np.zeros((128, 4), np.float32)
    sm[:, 0] = np.concatenate([inv, inv])
    sm[:, 1] = np.concatenate([-np.ones(64), np.ones(64)])
    sm[:, 2] = -math.pi * sm[:, 1]
    sm[:, 3] = -math.pi
    c["c_small"] = sm
    qi = np.arange(128)[:, None]
    kj = np.arange(384)[None, :]
    bandok = np.abs(kj - 128 - qi) <= 128
    bm = np.zeros((128, 3, 384), np.float32)
    for var in range(3):
        ok = bandok.copy()
        if var == 0:
            ok &= (kj >= 128)
        if var == 2:
            ok &= (kj < 256)
        bm[:, var, :] = np.where(ok, 0.0, -30000.0)
    c["c_band"] = bm
    rm = np.ones((128, 512), np.float32)
    rm[:, ::64] = 0.0
    c["c_rmask"] = rm
    c["c_iota"] = np.broadcast_to(np.arange(512, dtype=np.float32)[None, :], (128, 512)).copy()
    t = np.arange(32)[None, :] * 128 + np.arange(128)[:, None]
    trow = ((t // 256) % 4) * 1024 + (t // 1024) * 256 + (t % 256)
    c["c_tval"] = np.stack([trow // 64, trow % 64], axis=-1).astype(np.float32).astype(bf)
    return c


_NC_CACHE = {}


def kernel(x, c, positions, w_ada, b_ada, g_pre_mix, g_post_mix, g_pre_ffn, g_post_ffn,
           w_in, hg_lb_logits, hg_out_norm, attn_sink, w_branch_a, w_branch_b, w_out,
           w_router, w_exp_gate, w_exp_up, w_exp_down):
    f = lambda a: np.ascontiguousarray(np.asarray(a))
    x, c, positions = f(x), f(c), f(positions)
    w_ada, b_ada, w_in = f(w_ada)[0], f(b_ada)[0], f(w_in)[0]
    lbl_all = f(hg_lb_logits)
    hgn_all = f(hg_out_norm)[0]
    sink_all = f(attn_sink)[0]
    w_ba, w_bb, w_o, w_r = f(w_branch_a)[0], f(w_branch_b)[0], f(w_out)[0], f(w_router)[0]
    wg_all, wu_all, wd_all = f(w_exp_gate)[0], f(w_exp_up)[0], f(w_exp_down)[0]
    gv = np.stack([f(g_pre_mix)[0], f(g_post_mix)[0], f(g_pre_ffn)[0], f(g_post_ffn)[0]], axis=0)
    consts = _consts()
    if "nc" not in _NC_CACHE:
        _NC_CACHE["nc"] = build_program()
    nc = _NC_CACHE["nc"]
    O = {"hq": 0, "ff": 1024, "fb": 2048, "hi": 3072, "hg": 4096, "aq": 5120, "ak": 6144, "av": 6400, "ga": 6656, "gb": 8704}
    w_gab = np.ascontiguousarray(w_in[:, O["ga"]:O["ga"] + 4096])
    in_maps = []
    for core in range(8):
        b, q = core // 4, core % 4
        hs = [2 * q, 2 * q + 1]
        kvh = q // 2
        cols_fm = []
        for base in ("hq", "ff", "fb", "aq"):
            for h in hs:
                cols_fm.append(np.arange(O[base] + h * 128, O[base] + (h + 1) * 128))
        cols_fm.append(np.arange(O["ak"] + kvh * 128, O["ak"] + (kvh + 1) * 128))
        cols_fm = np.concatenate(cols_fm)
        cols_tm = np.concatenate([np.arange(O["hi"] + h * 128, O["hi"] + (h + 1) * 128) for h in hs] +
                                 [np.arange(O["hg"] + h * 128, O["hg"] + (h + 1) * 128) for h in hs] +
                                 [np.arange(O["av"] + kvh * 128, O["av"] + (kvh + 1) * 128)])
        lbl = np.zeros((128, 4, 2), np.float32)
        for dr in range(2):
            for hh in range(2):
                h = hs[hh]
                lbl[:, dr * 2 + hh, :] = lbl_all[dr, :, h * 128:(h + 1) * 128].T
        cidx_h = np.zeros((128, 40), np.int32)
        pp_ = np.arange(128)
        for j_ in range(8):
            for r_ in range(4):
                cidx_h[:, j_ * 4 + r_] = q * 4096 + r_ * 1024 + j_ * 128 + pp_
        cidx_h[:, 32] = np.minimum(4 * q + pp_, 15)
        cidx_h[:, 33] = np.minimum(16 * q + pp_, 63)
        rowbase = np.array([(e % 4) * 2048 + (e // 4) * 256 for e in range(16)], np.float32)
        m = {
            "x_own": x[b, q * 1024:(q + 1) * 1024, :],
            "c_b": np.ascontiguousarray(c[b].reshape(KC, 128).T),
            "pos_b": positions[b:b + 1, :].astype(np.int32),
            "w_ada_q": np.ascontiguousarray(w_ada[:, q * 3072:(q + 1) * 3072]),
            "b_ada_q": b_ada[None, q * 3072:(q + 1) * 3072],
            "gvecs": gv,
            "w_fm": np.ascontiguousarray(w_in[:, cols_fm]),
            "w_tm": np.ascontiguousarray(w_in[:, cols_tm]),
            "w_gab": w_gab,
            "lbl": lbl,
            "hgn": np.ascontiguousarray(hgn_all[hs].reshape(1, 256)),
            "sink": np.ascontiguousarray(sink_all[hs].reshape(1, 2)),
            "w_ba": w_ba, "w_bb": w_bb, "w_o": w_o, "w_r": w_r,
            "wg": wg_all[4 * q:4 * q + 4], "wu": wu_all[4 * q:4 * q + 4], "wd": wd_all[4 * q:4 * q + 4],
            "c_rowbase": np.broadcast_to(rowbase[None, :], (128, 16)).copy(),
            "c_idx": cidx_h,
        }
        m.update(consts)
        in_maps.append({k: np.ascontiguousarray(v) for k, v in m.items()})
    res = run_bass_kernel_spmd(nc, in_maps, core_ids=list(range(8)))
    outp = np.zeros((2, S, D), np.float32)
    for core in range(8):
        b, q = core // 4, core % 4
        outp[b, q * 1024:(q + 1) * 1024, :] = res.results[core]["out"]
    if DEBUG:
        kernel.debug = res.results
    return outp
```
